# Optimizing a Trainium2 kernel written in Bass

```python
import math
import jax
import jax.numpy as jnp
from jax import lax
import numpy as np

D_MODEL = 1024
BATCH = 8
SEQ = 8192
DEPTH = 2

GRID_W = 64
CTX_LEN = 256
N_MIXERS = 2
N_HEADS = 8
HEAD_DIM = D_MODEL // N_HEADS
CHUNK = 64
N_GROUPS = 4
EXPERTS_PER_GROUP = 8
N_EXPERTS = N_GROUPS * EXPERTS_PER_GROUP
TOP_K = 2
D_EXPERT = D_MODEL // 2
MOE_BLOCK = 128
N_MOD = 6
EPS = 1e-6

kernel_name = 'hybrid_deltanet_shortconv_hmoe_dit'


def rmsnorm(x, g):
    xf = x.astype(jnp.float32)
    y = xf * lax.rsqrt(jnp.mean(xf * xf, axis=-1, keepdims=True) + EPS)
    return (y * g.astype(jnp.float32)).astype(x.dtype)


def modulate(x, g, shift, scale):
    return rmsnorm(x, g) * (1 + scale) + shift


def ada_params(cond, w, b):
    m = jax.nn.silu(cond) @ w + b
    return jnp.split(m[..., None, :], N_MOD, axis=-1)


def conv3_seq(x, w):
    xp = jnp.pad(x, ((0, 0), (1, 1), (0, 0)))
    return xp[:, :-2] * w[0] + xp[:, 1:-1] * w[1] + xp[:, 2:] * w[2]


def conv3_grid(x, w, rows):
    b, s, ch = x.shape
    half = ch // 2
    g = x.reshape(b, rows, GRID_W, ch)
    wh, wv = w[:, :half], w[:, half:]
    ph = jnp.pad(g[..., :half], ((0, 0), (0, 0), (1, 1), (0, 0)))
    yh = ph[:, :, :-2] * wh[0] + ph[:, :, 1:-1] * wh[1] + ph[:, :, 2:] * wh[2]
    pv = jnp.pad(g[..., half:], ((0, 0), (1, 1), (0, 0), (0, 0)))
    yv = pv[:, :-2] * wv[0] + pv[:, 1:-1] * wv[1] + pv[:, 2:] * wv[2]
    return jnp.concatenate([yh, yv], axis=-1).reshape(b, s, ch)


def l2norm(t):
    return t * lax.rsqrt(jnp.sum(t * t, axis=-1, keepdims=True) + EPS)


def to_chunks(t):
    b, l, h = t.shape[:3]
    t = t.reshape(b, l // CHUNK, CHUNK, h, *t.shape[3:])
    return jnp.moveaxis(t, (1, 3), (0, 2))


def from_chunks(t):
    t = jnp.moveaxis(t, (0, 2), (1, 3))
    b, n, c, h, d = t.shape
    return t.reshape(b, n * c, h, d)


def gated_delta_scan(q, k, v, g, beta, s0, with_output):
    qc, kc, vc = to_chunks(q), to_chunks(k), to_chunks(v)
    gc = jnp.cumsum(to_chunks(g), axis=-1)
    bc = to_chunks(beta)
    pos = jnp.arange(CHUNK)
    incl = pos[:, None] >= pos[None, :]
    decay = jnp.exp(jnp.where(incl, gc[..., :, None] - gc[..., None, :], -jnp.inf))
    kb = kc * bc[..., None]
    lmat = jnp.where(pos[:, None] > pos[None, :],
                     jnp.einsum('nbhid,nbhjd->nbhij', kb, kc) * decay, 0.0)
    rhs = jnp.concatenate([vc * bc[..., None], kb * jnp.exp(gc)[..., None]], axis=-1)
    sol = lax.linalg.triangular_solve(lmat, rhs, left_side=True, lower=True, unit_diagonal=True)
    dv = vc.shape[-1]
    u, w = sol[..., :dv], sol[..., dv:]
    k_dec = kc * jnp.exp(gc[..., -1:] - gc)[..., None]
    g_last = jnp.exp(gc[..., -1])
    xs = (u, w, k_dec, g_last)
    if with_output:
        xs = xs + (qc * jnp.exp(gc)[..., None], jnp.einsum('nbhid,nbhjd->nbhij', qc, kc) * decay)

    def step(s, inp):
        u_i, w_i, kd_i, gl_i = inp[:4]
        v_new = u_i - jnp.einsum('bhcd,bhde->bhce', w_i, s)
        s_next = s * gl_i[..., None, None] + jnp.einsum('bhcd,bhce->bhde', kd_i, v_new)
        if with_output:
            qd_i, qk_i = inp[4:]
            o = jnp.einsum('bhcd,bhde->bhce', qd_i, s) + jnp.einsum('bhij,bhje->bhie', qk_i, v_new)
            return s_next, o
        return s_next, None

    s_final, o = lax.scan(step, s0, xs)
    return s_final, (from_chunks(o) if with_output else None)


def deltanet_mixer(h_lat, h_ctx, w_in, conv_w, a_log, dt_bias, onorm, w_out, ctx_out):
    d, nh, f32 = D_MODEL, N_HEADS, jnp.float32

    def project(h):
        b, l, _ = h.shape
        p = h @ w_in
        qkv = jax.nn.silu(conv3_seq(p[..., :3 * d], conv_w))
        q, k, v = (t.reshape(b, l, nh, HEAD_DIM).astype(f32) for t in jnp.split(qkv, 3, axis=-1))
        q = l2norm(q) * HEAD_DIM ** -0.5
        k = l2norm(k)
        z = p[..., 3 * d:4 * d]
        beta = jax.nn.sigmoid(p[..., 4 * d:4 * d + 2 * nh].astype(f32)).reshape(b, l, 2, nh)
        a_in = p[..., 4 * d + 2 * nh:].astype(f32).reshape(b, l, 2, nh)
        g = -jnp.exp(a_log.astype(f32)) * jax.nn.softplus(a_in + dt_bias.astype(f32))
        return q, k, v, z, beta, g

    def bidir(q, k, v, beta, g, s_f, s_b, with_output):
        flip = lambda t: t[:, ::-1]
        rf = gated_delta_scan(q, k, v, g[:, :, 0], beta[:, :, 0], s_f, with_output)
        rb = gated_delta_scan(flip(q), flip(k), flip(v), flip(g[:, :, 1]), flip(beta[:, :, 1]),
                              s_b, with_output)
        return rf, rb

    def finish(o, z):
        b, l = z.shape[:2]
        o = o * lax.rsqrt(jnp.mean(o * o, axis=-1, keepdims=True) + EPS) * onorm.astype(f32)
        y = o.astype(z.dtype) * jax.nn.silu(z).reshape(b, l, nh, HEAD_DIM)
        return y.reshape(b, l, d) @ w_out

    qc, kc, vc, zc, bc, gc = project(h_ctx)
    s_zero = jnp.zeros((h_ctx.shape[0], nh, HEAD_DIM, HEAD_DIM), f32)
    (sf, ocf), (sb, ocb) = bidir(qc, kc, vc, bc, gc, s_zero, s_zero, ctx_out)
    y_ctx = finish(ocf + ocb[:, ::-1], zc) if ctx_out else None
    ql, kl, vl, zl, bl, gl = project(h_lat)
    (_, olf), (_, olb) = bidir(ql, kl, vl, bl, gl, sf, sb, True)
    y_lat = finish(olf + olb[:, ::-1], zl)
    return y_lat, y_ctx


def shortconv_mixer(h, w_in, conv_w, w_out, rows):
    gate_b, gate_c, hx = jnp.split(h @ w_in, 3, axis=-1)
    u = gate_c * hx
    y = conv3_grid(u, conv_w, rows) if rows is not None else conv3_seq(u, conv_w)
    return (gate_b * y) @ w_out


def hier_moe(h, router_g, router_e, w1, w3, w2):
    f32 = jnp.float32
    t, d = h.shape
    hf = h.astype(f32)
    pg = jax.nn.softmax(hf @ router_g.astype(f32), axis=-1)
    gi = jnp.argmax(pg, axis=-1).astype(jnp.int32)
    pg_sel = jnp.take_along_axis(pg, gi[:, None], axis=-1)
    le = (hf @ router_e.astype(f32)).reshape(t, N_GROUPS, EXPERTS_PER_GROUP)
    le_sel = jnp.take_along_axis(le, gi[:, None, None], axis=1)[:, 0]
    pk, ik = lax.top_k(jax.nn.softmax(le_sel, axis=-1), TOP_K)
    wts = pg_sel * pk / jnp.sum(pk, axis=-1, keepdims=True)
    eid = gi[:, None] * EXPERTS_PER_GROUP + ik.astype(jnp.int32)

    n = t * TOP_K
    n_blocks = -(-n // MOE_BLOCK) + N_EXPERTS
    cap = n_blocks * MOE_BLOCK
    flat_e = eid.reshape(-1)
    flat_tok = jnp.repeat(jnp.arange(t, dtype=jnp.int32), TOP_K)
    flat_w = wts.reshape(-1)
    order = jnp.argsort(flat_e)
    se = flat_e[order]
    counts = jnp.zeros((N_EXPERTS,), jnp.int32).at[flat_e].add(1)
    padded = (counts + MOE_BLOCK - 1) // MOE_BLOCK * MOE_BLOCK
    ends_pad = jnp.cumsum(padded)
    start_pad = ends_pad - padded
    start_raw = jnp.cumsum(counts) - counts
    dest = start_pad[se] + jnp.arange(n, dtype=jnp.int32) - start_raw[se]
    buf_tok = jnp.full((cap,), t, jnp.int32).at[dest].set(flat_tok[order])
    buf_w = jnp.zeros((cap,), f32).at[dest].set(flat_w[order])
    block_e = jnp.minimum(
        jnp.searchsorted(ends_pad, jnp.arange(n_blocks, dtype=jnp.int32) * MOE_BLOCK, side='right'),
        N_EXPERTS - 1).astype(jnp.int32)
    h_pad = jnp.concatenate([h, jnp.zeros((1, d), h.dtype)], axis=0)
    xb = h_pad[buf_tok].reshape(n_blocks, MOE_BLOCK, d)

    def expert_block(args):
        xi, e = args
        return (jax.nn.silu(xi @ w1[e]) * (xi @ w3[e])) @ w2[e]

    yb = lax.map(expert_block, (xb, block_e)).reshape(cap, d)
    y = jax.ops.segment_sum(yb * buf_w[:, None].astype(yb.dtype), buf_tok, num_segments=t + 1)
    return y[:t]


def setup_inputs(seed: int = 0) -> dict:
    key = jax.random.key(seed)
    ks = jax.random.split(key, 24)
    d, nh, f32 = D_MODEL, N_HEADS, jnp.float32
    n_a = (DEPTH + N_MIXERS - 1) // N_MIXERS
    n_b = DEPTH // N_MIXERS

    def nrm(k, shape, scale):
        return jax.random.normal(k, shape, f32) * scale

    dt = jnp.exp(jax.random.uniform(ks[10], (n_a, 2, nh), f32, math.log(1e-3), math.log(1e-1)))
    return {
        'x': nrm(ks[0], (BATCH, SEQ, d), 1.0),
        'c': nrm(ks[1], (BATCH, d), 1.0),
        'ctx': nrm(ks[2], (BATCH, CTX_LEN, d), 1.0),
        'c_ctx': nrm(ks[3], (d,), 1.0),
        'ada_w': nrm(ks[4], (DEPTH, d, N_MOD * d), 0.5 * d ** -0.5),
        'ada_b': nrm(ks[5], (DEPTH, N_MOD * d), 0.02),
        'norm_mix': 1.0 + nrm(ks[6], (DEPTH, d), 0.05),
        'norm_ffn': 1.0 + nrm(ks[7], (DEPTH, d), 0.05),
        'w_in_a': nrm(ks[8], (n_a, d, 4 * d + 4 * nh), d ** -0.5),
        'conv_a': nrm(ks[9], (n_a, 3, 3 * d), 3 ** -0.5),
        'a_log_a': jnp.log(jax.random.uniform(ks[11], (n_a, 2, nh), f32, 1.0, 16.0)),
        'dt_bias_a': dt + jnp.log(-jnp.expm1(-dt)),
        'onorm_a': 1.0 + nrm(ks[12], (n_a, HEAD_DIM), 0.05),
        'w_out_a': nrm(ks[13], (n_a, d, d), d ** -0.5),
        'w_in_b': nrm(ks[14], (n_b, d, 3 * d), d ** -0.5),
        'conv_b': nrm(ks[15], (n_b, 3, d), 3 ** -0.5),
        'w_out_b': nrm(ks[16], (n_b, d, d), d ** -0.5),
        'router_g': nrm(ks[17], (DEPTH, d, N_GROUPS), d ** -0.5),
        'router_e': nrm(ks[18], (DEPTH, d, N_EXPERTS), d ** -0.5),
        'w1': nrm(ks[19], (DEPTH, N_EXPERTS, d, D_EXPERT), d ** -0.5),
        'w3': nrm(ks[20], (DEPTH, N_EXPERTS, d, D_EXPERT), d ** -0.5),
        'w2': nrm(ks[21], (DEPTH, N_EXPERTS, D_EXPERT, d), D_EXPERT ** -0.5),
        'final_norm': 1.0 + nrm(ks[22], (d,), 0.05),
    }


def reference(x, c, ctx, c_ctx, ada_w, ada_b, norm_mix, norm_ffn,
              w_in_a, conv_a, a_log_a, dt_bias_a, onorm_a, w_out_a,
              w_in_b, conv_b, w_out_b, router_g, router_e, w1, w3, w2, final_norm):
    b, s, d = x.shape
    rows = s // GRID_W
    kinds = [i % N_MIXERS for i in range(DEPTH)]
    for i in range(DEPTH):
        kind, j = kinds[i], i // N_MIXERS
        ctx_live = 0 in kinds[i + 1:]
        sh_m, sc_m, gt_m, sh_f, sc_f, gt_f = ada_params(c, ada_w[i], ada_b[i])
        if kind == 0 or ctx_live:
            csh_m, csc_m, cgt_m, csh_f, csc_f, cgt_f = ada_params(c_ctx, ada_w[i], ada_b[i])
            hc = modulate(ctx, norm_mix[i], csh_m, csc_m)
        h = modulate(x, norm_mix[i], sh_m, sc_m)
        if kind == 0:
            y, yc = deltanet_mixer(h, hc, w_in_a[j], conv_a[j], a_log_a[j], dt_bias_a[j],
                                   onorm_a[j], w_out_a[j], ctx_live)
        else:
            y = shortconv_mixer(h, w_in_b[j], conv_b[j], w_out_b[j], rows)
            yc = shortconv_mixer(hc, w_in_b[j], conv_b[j], w_out_b[j], None) if ctx_live else None
        x = x + gt_m * y
        hf = modulate(x, norm_ffn[i], sh_f, sc_f).reshape(b * s, d)
        if ctx_live:
            ctx = ctx + cgt_m * yc
            hcf = modulate(ctx, norm_ffn[i], csh_f, csc_f).reshape(-1, d)
            out = hier_moe(jnp.concatenate([hf, hcf], axis=0), router_g[i], router_e[i],
                           w1[i], w3[i], w2[i])
            x = x + gt_f * out[:b * s].reshape(b, s, d)
            ctx = ctx + cgt_f * out[b * s:].reshape(ctx.shape)
        else:
            x = x + gt_f * hier_moe(hf, router_g[i], router_e[i], w1[i], w3[i], w2[i]).reshape(b, s, d)
    return rmsnorm(x, final_norm)
```

```python
import numpy as np
from contextlib import ExitStack, contextmanager
import concourse.bass as bass
import concourse.mybir as mybir
from concourse.bass_utils import run_bass_kernel_spmd

F32 = mybir.dt.float32
BF16 = mybir.dt.bfloat16
I32 = mybir.dt.int32
AF = mybir.ActivationFunctionType
ALU = mybir.AluOpType
AX = mybir.AxisListType

D = 1024
NH = 8
HD = 128
NE = 32
DE = 512
EPS = 1e-6
BIG = 30000.0
GRID_W = 64


class Op:
    __slots__ = ("eng", "fn", "deps", "need", "sem", "val", "isdma", "idx")


class Prog:
    def __init__(self, nc, ndma=32):
        self.nc = nc
        self.es = ExitStack()
        self.engs = ["pe", "dve", "act", "pool", "sp"]
        self.q = {k: [] for k in self.engs}
        self.esem = {k: self.es.enter_context(nc.semaphore("s_" + k)) for k in self.engs}
        self.dsem = [self.es.enter_context(nc.semaphore("d%d" % i)) for i in range(ndma)]
        self.dlast = [None] * ndma
        self.dcount = [0] * ndma
        self.dnext = 0
        self.lastw = {}
        self.readers = {}
        self.nops = 0

    @staticmethod
    def key(x):
        if isinstance(x, (str, tuple)):
            return x
        return x.name

    def op(self, eng, fn, r=(), w=(), dma=False):
        o = Op()
        o.eng = eng
        o.fn = fn
        o.need = False
        o.isdma = dma
        o.sem = None
        o.val = None
        o.idx = self.nops
        self.nops += 1
        deps = {}
        rk = [self.key(x) for x in r]
        wk = [self.key(x) for x in w]

        def add(d):
            if d is None:
                return
            if d.isdma:
                deps[("d", id(d))] = d
            else:
                if d.eng == eng and eng == "pe" and not dma:
                    return
                cur = deps.get(("e", d.eng))
                if cur is None or cur.idx < d.idx:
                    deps[("e", d.eng)] = d

        for k in rk:
            add(self.lastw.get(k))
        for k in wk:
            add(self.lastw.get(k))
            for rd in self.readers.get(k, ()):
                add(rd)
        if dma:
            i = self.dnext
            self.dnext = (self.dnext + 1) % len(self.dsem)
            add(self.dlast[i])
            self.dcount[i] += 16
            o.sem = self.dsem[i]
            o.val = self.dcount[i]
            self.dlast[i] = o
        o.deps = list(deps.values())
        for d in o.deps:
            d.need = True
        for k in rk:
            self.readers.setdefault(k, []).append(o)
        for k in wk:
            self.lastw[k] = o
            self.readers[k] = []
        self.q[eng].append(o)
        return o

    def dma(self, eng, out, in_, r=None, w=None, **kw):
        r = [in_] if r is None else r
        w = [out] if w is None else w
        return self.op(eng, lambda e: e.dma_start(out=out, in_=in_, **kw), r=r, w=w, dma=True)

    def mm(self, out, lhsT, rhs, start=True, stop=True, r=None, w=None):
        r = [lhsT, rhs] if r is None else r
        w = [out] if w is None else w
        return self.op("pe", lambda e: e.matmul(out, lhsT, rhs, start=start, stop=stop), r=r, w=w)

    def tr(self, out, in_, ident, r=None, w=None):
        r = [in_, ident] if r is None else r
        w = [out] if w is None else w
        return self.op("pe", lambda e: e.transpose(out, in_, ident), r=r, w=w)

    def act(self, out, in_, func, bias=None, scale=None, r=None, w=None):
        rr = [in_] + ([bias] if bias is not None and not isinstance(bias, (int, float)) else [])
        r = rr if r is None else r
        w = [out] if w is None else w
        kw = {}
        if bias is not None:
            kw["bias"] = bias
        if scale is not None:
            kw["scale"] = scale
        return self.op("act", lambda e: e.activation(out=out, in_=in_, func=func, **kw), r=r, w=w)

    def tt(self, out, in0, in1, op, eng="dve", r=None, w=None):
        r = [in0, in1] if r is None else r
        w = [out] if w is None else w
        return self.op(eng, lambda e: e.tensor_tensor(out=out, in0=in0, in1=in1, op=op), r=r, w=w)

    def ts(self, out, in0, s1, op0, s2=None, op1=None, eng="dve", r=None, w=None):
        rr = [in0] + [s for s in (s1, s2) if s is not None and not isinstance(s, (int, float))]
        r = rr if r is None else r
        w = [out] if w is None else w
        if op1 is None:
            return self.op(eng, lambda e: e.tensor_scalar(out=out, in0=in0, scalar1=s1, scalar2=None, op0=op0), r=r, w=w)
        return self.op(eng, lambda e: e.tensor_scalar(out=out, in0=in0, scalar1=s1, scalar2=s2, op0=op0, op1=op1), r=r, w=w)

    def stt(self, out, in0, scalar, in1, op0, op1, r=None, w=None):
        rr = [in0, in1] + ([scalar] if not isinstance(scalar, (int, float)) else [])
        r = rr if r is None else r
        w = [out] if w is None else w
        return self.op("dve", lambda e: e.scalar_tensor_tensor(out=out, in0=in0, scalar=scalar, in1=in1, op0=op0, op1=op1), r=r, w=w)

    def copy(self, out, in_, eng="dve", r=None, w=None):
        r = [in_] if r is None else r
        w = [out] if w is None else w
        if eng == "act":
            return self.op("act", lambda e: e.copy(out=out, in_=in_), r=r, w=w)
        return self.op(eng, lambda e: e.tensor_copy(out=out, in_=in_), r=r, w=w)

    def red(self, out, in_, op, axis=AX.X, r=None, w=None):
        r = [in_] if r is None else r
        w = [out] if w is None else w
        return self.op("dve", lambda e: e.tensor_reduce(out=out, in_=in_, axis=axis, op=op), r=r, w=w)

    def memset(self, ap, val, eng="dve"):
        return self.op(eng, lambda e: e.memset(ap, val), r=[], w=[ap])

    def recip(self, out, in_, r=None, w=None):
        r = [in_] if r is None else r
        w = [out] if w is None else w
        return self.op("dve", lambda e: e.reciprocal(out=out, in_=in_), r=r, w=w)

    def barrier(self):
        lasts = [d for d in self.dlast if d is not None]
        for e in self.engs:
            for o in reversed(self.q[e]):
                if not o.isdma and o.fn is not None:
                    lasts.append(o)
                    break
        for d in lasts:
            d.need = True
        for e in self.engs:
            b = Op()
            b.eng = e
            b.fn = None
            b.need = False
            b.isdma = False
            b.sem = None
            b.val = None
            b.idx = self.nops
            self.nops += 1
            b.deps = list(lasts)
            self.q[e].append(b)
        self.lastw = {}
        self.readers = {}

    def finish(self):
        fin = Op()
        fin.eng = "sp"
        fin.fn = None
        fin.need = False
        fin.isdma = False
        fin.idx = self.nops
        deps = [d for d in self.dlast if d is not None]
        for e in self.engs:
            for o in reversed(self.q[e]):
                if not o.isdma and o.fn is not None:
                    o.need = True
                    deps.append(o)
                    break
        fin.deps = deps
        self.q["sp"].append(fin)
        for e in self.engs:
            c = 0
            for o in self.q[e]:
                if o.isdma or o.fn is None:
                    continue
                if o.need:
                    c += 1
                    o.val = c
                    o.sem = self.esem[e]
        self.counts = {e: len(self.q[e]) for e in self.engs}

        def replay(name, e):
            waited = {}
            for o in self.q[name]:
                for d in o.deps:
                    sk = id(d.sem)
                    if waited.get(sk, 0) < d.val:
                        e.wait_ge(d.sem, d.val)
                        waited[sk] = d.val
                if o.fn is None:
                    continue
                ins = o.fn(e)
                if o.isdma:
                    ins.then_inc(o.sem, 16)
                elif o.need:
                    ins.then_inc(o.sem, 1)

        nc = self.nc
        with nc.Block() as block:
            @block.tensor
            def _(e):
                replay("pe", e)

            @block.vector
            def _(e):
                replay("dve", e)

            @block.scalar
            def _(e):
                replay("act", e)

            @block.gpsimd
            def _(e):
                replay("pool", e)

            @block.sync
            def _(e):
                replay("sp", e)
        self.es.close()


class Builder:
    def __init__(self, S, CTXL, phases=99, debug=()):
        self.S = S
        self.CTXL = CTXL
        self.NT = S // 128
        self.NC = CTXL // 128
        self.T = S
        self.NBLK = (2 * S) // 128 + NE
        self.phases = phases
        self.debug = set(debug)
        nc = self.nc = bass.Bass("TRN2", target_bir_lowering=False)
        self.P = Prog(nc)
        self.top = ExitStack()
        self._n = 0
        self.regcache = {}
        self.pfi = [0]

        def din(name, shape, dt=F32):
            return nc.dram_tensor(name, list(shape), dt, kind="ExternalInput").ap()

        self.x = din("x", [S, D])
        self.c = din("c", [D])
        self.ctx = din("ctx", [CTXL, D])
        self.c_ctx = din("c_ctx", [D])
        self.ada_w = din("ada_w", [2, D, 6 * D])
        self.ada_b = din("ada_b", [2, 6 * D])
        self.norm_mix = din("norm_mix", [2, D])
        self.norm_ffn = din("norm_ffn", [2, D])
        self.w_in_a = din("w_in_a", [D, 4 * D + 32])
        self.conv_a = din("conv_a", [3, 3 * D])
        self.a_log = din("a_log", [2, NH])
        self.dt_bias = din("dt_bias", [2, NH])
        self.onorm = din("onorm", [HD])
        self.w_out_a = din("w_out_a", [D, D])
        self.w_in_b = din("w_in_b", [D, 3 * D])
        self.conv_b = din("conv_b", [3, D])
        self.w_out_b = din("w_out_b", [D, D])
        self.router = din("router", [2, D, 36])
        if phases >= 4:
            self.wall = [din("wall%d" % l, [NE * 128, 3 * 4096]) for l in range(2)]
        self.final_norm = din("final_norm", [D])
        self.out = nc.dram_tensor("out", [S, D], F32, kind="ExternalOutput").ap()

        TT = S + CTXL
        self.MODP = self.scr("modp", [16, 128, D])
        self.P0 = self.scr("p0", [TT, 4 * D + 32])
        self.OF = self.scr("of", [S, D])
        self.OB = self.scr("ob", [S, D])
        self.QKV = self.scr("qkv", [TT, 3 * D], BF16)
        self.X1 = self.scr("x1", [S, D])
        self.X2 = self.scr("x2", [S, D])
        self.X3 = self.scr("x3", [S, D])
        self.HF = self.scr("hfb", [S, D], BF16)
        self.XB = self.scr("xb", [self.NBLK * 128, D], BF16)
        self.YB = self.scr("yb", [self.NBLK * 128, D])
        self.U1 = self.scr("u1", [S, D])
        self.GB = self.scr("gb", [S, D])

    @contextmanager
    def scope(self):
        with ExitStack() as es:
            yield es
            self.P.barrier()

    def run2(self, n, body, lag=0):
        def stream(par):
            for i in range(par, n, 2):
                yield from body(i)
        gens = [stream(0), stream(1)]
        for _ in range(lag):
            next(gens[0])
        while gens:
            for g in list(gens):
                try:
                    next(g)
                except StopIteration:
                    gens.remove(g)

    def scr(self, name, shape, dt=F32):
        kind = "ExternalOutput" if name in self.debug else "Internal"
        return self.nc.dram_tensor(name, list(shape), dt, kind=kind).ap()

    def sb(self, es, name, shape, dt=F32):
        self._n += 1
        return es.enter_context(self.nc.sbuf_tensor("%s_%d" % (name, self._n), list(shape), dt))

    def build(self):
        nc, P = self.nc, self.P
        top = self.top
        self.pf = [top.enter_context(nc.psum_tensor("pf%d" % i, [128, 512], F32)) for i in range(6)]
        self.pt = [top.enter_context(nc.psum_tensor("pt%d" % i, [128, 8, 128], BF16)) for i in range(2)]
        self.consts()
        self.phase_ada()
        if self.phases >= 1:
            self.phase_projA()
        if self.phases >= 2:
            self.phase_qkv()
        if self.phases >= 3:
            self.phase_delta_both()
            self.phase_finish()
        if self.phases >= 4:
            self.phase_moe(0, self.X1, self.X2, final=False)
        if self.phases >= 5:
            self.phase_convD()
        if self.phases >= 6:
            self.phase_convE()
        if self.phases >= 7:
            self.phase_moe(1, self.X3, self.out, final=True)
        P.finish()
        top.close()
        return nc

    def consts(self):
        nc, P, top = self.nc, self.P, self.top
        sb = lambda n, s, d=F32: self.sb(top, n, s, d)
        rowi = sb("rowi", [128, 128])
        coli = sb("coli", [128, 128])
        P.op("pool", lambda e: e.iota(rowi[:], pattern=[[0, 128]], base=0, channel_multiplier=1,
                                       allow_small_or_imprecise_dtypes=True), w=[rowi])
        P.op("pool", lambda e: e.iota(coli[:], pattern=[[1, 128]], base=0, channel_multiplier=0,
                                       allow_small_or_imprecise_dtypes=True), w=[coli])
        self.rowi, self.coli = rowi, coli
        self.ident_f = sb("ident_f", [128, 128])
        self.ident_b = sb("ident_b", [128, 128], BF16)
        self.ones_f = sb("ones_f", [128, 128])
        self.ones_b = sb("ones_b", [128, 128], BF16)
        P.tt(self.ident_f[:], rowi[:], coli[:], ALU.is_equal)
        P.copy(self.ident_b[:], self.ident_f[:])
        P.memset(self.ones_f[:], 1.0)
        P.memset(self.ones_b[:], 1.0)
        self.U = [sb("Uf", [128, 128]), sb("Ub", [128, 128])]
        P.tt(self.U[0][:], rowi[:], coli[:], ALU.is_le)
        P.tt(self.U[1][:], rowi[:], coli[:], ALU.is_ge)
        self.SM = [sb("SMf", [128, 128]), sb("SMb", [128, 128])]
        P.tt(self.SM[0][:], rowi[:], coli[:], ALU.is_gt)
        P.tt(self.SM[1][:], rowi[:], coli[:], ALU.is_lt)
        self.BM = [sb("BMf", [128, 4, 128]), sb("BMb", [128, 4, 128])]
        for d in range(2):
            for h in range(4):
                P.ts(self.BM[d][:, h, :], self.SM[1 - d][:], BIG, ALU.mult)
        self.Us_b = sb("Us_b", [128, 128], BF16)
        P.copy(self.Us_b[:], self.SM[1][:])
        bd = {}
        colb = sb("colb", [128, 128]); rowb = sb("rowb", [128, 128])
        for sz in (8, 16, 32, 64):
            P.op("pool", lambda e, sz=sz: e.iota(colb[:], pattern=[[1, 128 // sz], [0, sz]], base=0, channel_multiplier=0,
                                                  allow_small_or_imprecise_dtypes=True), w=[colb])
            P.tr(self.pf[0][:, 0:128], colb[:], self.ident_f[:])
            P.copy(rowb[:], self.pf[0][:, 0:128])
            bd[sz] = sb("bd%d" % sz, [128, 128])
            P.tt(bd[sz][:], rowb[:], colb[:], ALU.is_equal)
        self.BD8 = sb("BD8b", [128, 128], BF16)
        P.copy(self.BD8[:], bd[8][:])
        self.MS = {}
        for sz in (8, 16, 32, 64):
            self.MS[sz] = sb("MS%d" % sz, [128, 128], BF16)
            if sz < 64:
                P.tt(self.MS[sz][:], bd[2 * sz][:], bd[sz][:], ALU.subtract)
            else:
                P.ts(self.MS[sz][:], bd[sz][:], -1.0, ALU.mult, 1.0, ALU.add)
        self.epsc = sb("epsc", [128, 1])
        P.memset(self.epsc[:], EPS)

    def evac(self, out, in_, i):
        if i % 2 == 0:
            return self.P.copy(out, in_, eng="dve")
        return self.P.copy(out, in_, eng="act")

    def load_cast(self, es, name, src_ap, ncols, chunk=512, kparts=8, qeng="sp"):
        P = self.P
        wb = self.sb(es, name, [128, kparts, ncols], BF16)
        with self.scope() as tmp:
            st = [self.sb(tmp, name + "_st", [128, kparts, chunk]) for _ in range(2)]
            src = src_ap.rearrange("(kc p) n -> p kc n", p=128)
            i = 0
            for c0 in range(0, ncols, chunk):
                cw = min(chunk, ncols - c0)
                s = st[i % 2]
                P.dma(qeng, s[:, :, 0:cw], src[:, :, c0:c0 + cw])
                if i % 2 == 0:
                    P.copy(wb[:, :, c0:c0 + cw], s[:, :, 0:cw], eng="dve")
                else:
                    P.copy(wb[:, :, c0:c0 + cw], s[:, :, 0:cw], eng="pool")
                i += 1
        return wb

    def bcast_load(self, es, name, vec_ap, n):
        t = self.sb(es, name, [128, n])
        self.P.dma("sp", t[:], vec_ap.partition_broadcast(128))
        return t

    def modulate(self, xt, A, B, sq, ss, hf, hb):
        P = self.P
        P.act(sq[:], xt[:], AF.Square)
        P.red(ss[:, 0:1], sq[:], ALU.add)
        P.ts(ss[:, 1:2], ss[:, 0:1], 1.0 / D, ALU.mult, EPS, ALU.add)
        P.act(ss[:, 2:3], ss[:, 1:2], AF.Sqrt)
        P.recip(ss[:, 3:4], ss[:, 2:3])
        P.stt(hf[:], xt[:], ss[:, 3:4], A[:], ALU.mult, ALU.mult)
        if hb is not None:
            P.tt(hb[:], hf[:], B[:], ALU.add)
        else:
            P.tt(hf[:], hf[:], B[:], ALU.add)

    def transpose8(self, dstT, src_bf, ptile, i=0, n=8):
        P = self.P
        for kc in range(n):
            P.tr(ptile[:, kc, :], src_bf[:, kc * 128:(kc + 1) * 128], self.ident_b[:])
        self.evac(dstT[:, 0:n, :], ptile[:, 0:n, :], i)

    def phase_ada(self):
        nc, P = self.nc, self.P
        with self.scope() as es:
            sb = lambda n, s, d=F32: self.sb(es, n, s, d)
            craw = sb("craw", [128, 2, 8])
            P.dma("sp", craw[:, 0, :], self.c.rearrange("(kc p) -> p kc", p=128), allow_slow_non_contiguous=True)
            P.dma("sp", craw[:, 1, :], self.c_ctx.rearrange("(kc p) -> p kc", p=128), allow_slow_non_contiguous=True)
            csil = sb("csil", [128, 2, 8])
            P.act(csil[:], craw[:], AF.Silu)
            cb = sb("cb", [128, 2, 8, 128])
            for j in range(2):
                P.copy(cb[:, j, :, :], csil[:, j, :].unsqueeze(2).to_broadcast([128, 8, 128]))
            wst = [sb("adaw", [128, 8, 512]) for _ in range(2)]
            mb = sb("mb", [128, 6 * D])
            bb = sb("bb", [128, 6 * D])
            nm = sb("nm", [128, D])
            res = [sb("res", [128, D]) for _ in range(2)]
            cnt = 0
            for (layer, j) in ((0, 0), (0, 1), (1, 0)):
                P.dma("sp", bb[:], self.ada_b[layer].partition_broadcast(128))
                for n in range(12):
                    w = wst[cnt % 2]
                    P.dma("sp", w[:], self.ada_w[layer].rearrange("(kc p) n -> p kc n", p=128)[:, :, n * 512:(n + 1) * 512])
                    ps = self.pf[cnt % 2]
                    for kc in range(8):
                        P.mm(ps[:], cb[:, j, kc, :], w[:, kc, :], start=(kc == 0), stop=(kc == 7))
                    P.tt(mb[:, n * 512:(n + 1) * 512], ps[:], bb[:, n * 512:(n + 1) * 512], ALU.add)
                    cnt += 1
                outs = []
                for sub, (nrm, base) in enumerate(((self.norm_mix, 0), (self.norm_ffn, 3))):
                    if j == 1 and sub == 1:
                        continue
                    P.dma("sp", nm[:], nrm[layer].partition_broadcast(128))
                    r0 = res[0]
                    P.stt(r0[:], mb[:, (base + 1) * D:(base + 2) * D], 1.0, nm[:], ALU.add, ALU.mult)
                    ka = (12 if j == 1 else 6 * layer + base)
                    P.dma("sp", self.MODP[ka], r0[:], w=[("modp", ka)])
                    P.dma("sp", self.MODP[ka + 1], mb[:, base * D:(base + 1) * D], w=[("modp", ka + 1)])
                    if j == 0:
                        P.dma("sp", self.MODP[ka + 2], mb[:, (base + 2) * D:(base + 3) * D], w=[("modp", ka + 2)])

    def load_mod(self, es, name, k):
        t = self.sb(es, name, [128, D])
        self.P.dma("sp", t[:], self.MODP[k], r=[("modp", k)])
        return t

    def phase_projA(self):
        nc, P = self.nc, self.P
        NCOL = 4 * D + 32
        with self.scope() as es:
            sb = lambda n, s, d=F32: self.sb(es, n, s, d)
            wb = self.load_cast(es, "wina", self.w_in_a, NCOL)
            A = [self.load_mod(es, "A", 0), self.load_mod(es, "Ac", 12)]
            B = [self.load_mod(es, "B", 1), self.load_mod(es, "Bc", 13)]
            xt = [sb("xt", [128, D]) for _ in range(2)]
            sq = sb("sq", [128, D])
            ss = sb("ss", [128, 4])
            hf = sb("hf", [128, D])
            hb = sb("hb", [128, D], BF16)
            hT = [sb("hT", [128, 8, 128], BF16) for _ in range(2)]
            po = [sb("po", [128, NCOL]) for _ in range(2)]
            tiles = [("c", i) for i in range(self.NC)] + [("l", i) for i in range(self.NT)]
            for ti, (kind, i) in enumerate(tiles):
                src = self.ctx if kind == "c" else self.x
                j = 1 if kind == "c" else 0
                row0 = i * 128 + (0 if kind == "c" else self.CTXL)
                x_ = xt[ti % 2]
                P.dma("sp", x_[:], src[i * 128:(i + 1) * 128, :])
                self.modulate(x_, A[j], B[j], sq, ss, hf, hb)
                hT_ = hT[ti % 2]
                self.transpose8(hT_, hb, self.pt[ti % 2], ti)
                po_ = po[ti % 2]
                g = 0
                for c0 in range(0, NCOL, 512):
                    cw = min(512, NCOL - c0)
                    ps = self.pf[g % 4]
                    for kc in range(8):
                        P.mm(ps[:, 0:cw], hT_[:, kc, :], wb[:, kc, c0:c0 + cw], start=(kc == 0), stop=(kc == 7))
                    self.evac(po_[:, c0:c0 + cw], ps[:, 0:cw], g)
                    g += 1
                P.dma("pool", self.P0[row0:row0 + 128, :], po_[:], w=[("p0", row0 // 128)])

    def phase_qkv(self):
        nc, P = self.nc, self.P
        CT = self.CTXL
        with self.scope() as es:
            sb = lambda n, s, dt=F32: self.sb(es, n, s, dt)
            cw = [self.bcast_load(es, "cw%d" % s, self.conv_a[s], 3 * D) for s in range(3)]
            pm_ = [sb("pm", [128, 3 * D]) for _ in range(2)]
            pc_ = [sb("pc", [128, 3 * D]) for _ in range(2)]
            pp_ = [sb("pp", [128, 3 * D]) for _ in range(2)]
            qo_ = [sb("qo", [128, 3 * D], BF16) for _ in range(2)]
            so_ = [sb("so", [128, 3 * D]) for _ in range(2)]
            ssq_ = [sb("ssq", [128, 64]) for _ in range(2)]
            bc = lambda ap: ap.unsqueeze(2).to_broadcast([128, ap.shape[1], HD])
            tiles = [("c", i) for i in range(self.NC)] + [("l", i) for i in range(self.NT)]

            def loads(ti):
                if ti >= len(tiles):
                    return
                kind, i = tiles[ti]
                pm, pc, pp = pm_[ti % 2], pc_[ti % 2], pp_[ti % 2]
                ntile = self.NC if kind == "c" else self.NT
                row0 = (0 if kind == "c" else CT) + i * 128
                P.dma("sp", pc[:], self.P0[row0:row0 + 128, 0:3 * D])
                if i == 0:
                    P.memset(pm[:], 0.0, eng="pool")
                    P.dma("sp", pm[1:128, :], self.P0[row0:row0 + 127, 0:3 * D], w=[pm])
                else:
                    P.dma("sp", pm[:], self.P0[row0 - 1:row0 + 127, 0:3 * D])
                if i == ntile - 1:
                    P.memset(pp[:], 0.0, eng="pool")
                    P.dma("sp", pp[0:127, :], self.P0[row0 + 1:row0 + 128, 0:3 * D], w=[pp])
                else:
                    P.dma("sp", pp[:], self.P0[row0 + 1:row0 + 129, 0:3 * D])

            loads(0)
            loads(1)

            def qbody(ti):
                kind, i = tiles[ti]
                pm, pc, pp, qo, ssq = pm_[ti % 2], pc_[ti % 2], pp_[ti % 2], qo_[ti % 2], ssq_[ti % 2]
                so = so_[ti % 2]
                row0 = (0 if kind == "c" else CT) + i * 128
                P.tt(pm[:], pm[:], cw[0][:], ALU.mult, eng="pool")
                P.tt(pc[:], pc[:], cw[1][:], ALU.mult)
                P.tt(pp[:], pp[:], cw[2][:], ALU.mult, eng="pool")
                yield
                for c in range(6):
                    ps = self.pf[c]
                    cs = slice(c * 512, (c + 1) * 512)
                    P.mm(ps[:], self.ident_f[:], pm[:, cs], start=True, stop=False)
                    P.mm(ps[:], self.ident_f[:], pc[:, cs], start=False, stop=False)
                    P.mm(ps[:], self.ident_f[:], pp[:, cs], start=False, stop=True)
                    P.act(so[:, cs], ps[:], AF.Silu)
                    if c % 2 == 1:
                        yield
                P.act(pm[:, 0:2 * D], so[:, 0:2 * D], AF.Square)
                yield
                P.red(ssq[:, 0:16], pm[:, 0:2 * D].rearrange("p (h e) -> p h e", e=HD), ALU.add)
                P.ts(ssq[:, 16:32], ssq[:, 0:16], EPS, ALU.add)
                P.act(ssq[:, 32:48], ssq[:, 16:32], AF.Sqrt)
                P.recip(ssq[:, 48:64], ssq[:, 32:48])
                P.ts(ssq[:, 48:56], ssq[:, 48:56], HD ** -0.5, ALU.mult)
                P.tt(qo[:, 0:D].rearrange("p (h e) -> p h e", e=HD), so[:, 0:D].rearrange("p (h e) -> p h e", e=HD),
                     bc(ssq[:, 48:56]), ALU.mult)
                P.tt(qo[:, D:2 * D].rearrange("p (h e) -> p h e", e=HD), so[:, D:2 * D].rearrange("p (h e) -> p h e", e=HD),
                     bc(ssq[:, 56:64]), ALU.mult, eng="pool")
                P.copy(qo[:, 2 * D:3 * D], so[:, 2 * D:3 * D], eng="act")
                yield
                loads(ti + 2)
                P.dma("pool", self.QKV[row0:row0 + 128, :], qo[:])
                yield
            self.run2(len(tiles), qbody)

    def phase_delta_both(self):
        with self.scope() as es:
            gens = [self.delta_gen(es, 0), self.delta_gen(es, 1)]
            while gens:
                for g in list(gens):
                    try:
                        next(g)
                    except StopIteration:
                        gens.remove(g)

    def delta_gen(self, es, d):
        nc, P = self.nc, self.P
        CT = self.CTXL
        sb = lambda n, s, dt=F32: self.sb(es, n, s, dt)
        nA = self.bcast_load(es, "nA", self.a_log[d], NH)
        P.act(nA[:], nA[:], AF.Exp)
        P.ts(nA[:], nA[:], -1.0, ALU.mult)
        dtb = self.bcast_load(es, "dtb", self.dt_bias[d], NH)
        qv = sb("qv", [128, 3 * D], BF16)
        gts = sb("gts", [128, 32])
        sml = sb("sml", [128, 16 * NH])
        kT = sb("kT", [128, NH, HD], BF16)
        qT = sb("qT", [128, NH, HD], BF16)
        qd_b = sb("qd_b", [128, NH, HD], BF16)
        qdT = sb("qdT", [128, NH, HD], BF16)
        vb = sb("vb", [128, NH, HD], BF16)
        kbe = sb("kbe", [128, NH, HD], BF16)
        kdec = sb("kdec", [128, NH, HD], BF16)
        rhsR = sb("rhsR", [128, NH, HD])
        Dm = sb("Dm", [128, NH, HD])
        As = sb("As", [128, NH, HD], BF16)
        qk_b = sb("qk_b", [128, NH, HD], BF16)
        qkT = sb("qkT", [128, NH, HD], BF16)
        L_b = sb("L_b", [128, NH, HD], BF16); N_b = sb("N_b", [128, NH, HD], BF16)
        Ld = sb("Ld", [128, NH, HD], BF16); Nd = sb("Nd", [128, NH, HD], BF16)
        P1 = sb("P1", [128, NH, HD], BF16); Q1 = sb("Q1", [128, NH, HD], BF16)
        P2 = sb("P2", [128, NH, HD], BF16); Q2 = sb("Q2", [128, NH, HD], BF16)
        Tw = [sb("Tw", [128, NH, HD], BF16) for _ in range(2)]
        Xw = [sb("Xw", [128, NH, HD], BF16) for _ in range(2)]
        u32 = sb("u32", [128, NH, HD])
        wT = sb("wT", [128, NH, HD], BF16)
        vnew = sb("vnew", [128, NH, HD], BF16)
        S32 = sb("S32", [128, NH, HD])
        Sb = sb("Sb", [128, NH, HD], BF16)
        Stmp = sb("Stmp", [128, NH, HD])
        o32 = sb("o32", [128, NH, HD])
        P.memset(S32[:], 0.0)
        P.memset(Sb[:], 0.0, eng="pool")
        OUT = self.OF if d == 0 else self.OB
        ptd = self.pt[d]
        if d == 0:
            order = [("c", i) for i in range(self.NC)] + [("l", i) for i in range(self.NT)]
        else:
            order = [("c", i) for i in reversed(range(self.NC))] + [("l", i) for i in reversed(range(self.NT))]
        bc = lambda ap: ap.unsqueeze(2).to_broadcast([128, ap.shape[1], HD])
        mb = lambda m: m[:].unsqueeze(1).to_broadcast([128, NH, HD])
        v4 = lambda ps: ps[:].rearrange("p (h e) -> p h e", e=HD)
        pfi = self.pfi

        def nextpf():
            pfi[0] += 1
            return self.pf[pfi[0] % 6]

        def mm4(lhs, rhs, hg):
            ps = nextpf()
            for hh in range(4):
                h = hg * 4 + hh
                P.mm(ps[:, hh * 128:(hh + 1) * 128], lhs[:, h, :], rhs[:, h, :])
            return ps

        for ti, (kind, i) in enumerate(order):
            row0 = (0 if kind == "c" else CT) + i * 128
            P.dma("sp", qv[:], self.QKV[row0:row0 + 128, :])
            P.dma("sp", gts[:], self.P0[row0:row0 + 128, 4 * D:4 * D + 32])
            qn_b = qv[:, 0:D].rearrange("p (h e) -> p h e", e=HD)
            kn_b = qv[:, D:2 * D].rearrange("p (h e) -> p h e", e=HD)
            v_b = qv[:, 2 * D:3 * D].rearrange("p (h e) -> p h e", e=HD)
            yield
            beta = sml[:, 0:8]
            P.act(beta, gts[:, d * 8:d * 8 + 8], AF.Sigmoid)
            xg = sml[:, 8:16]
            P.tt(xg, gts[:, 16 + d * 8:16 + d * 8 + 8], dtb[:], ALU.add)
            P.stt(sml[:, 16:24], xg, -1.0, xg, ALU.mult, ALU.max)
            P.act(sml[:, 24:32], sml[:, 16:24], AF.Exp, scale=-1.0)
            P.act(sml[:, 32:40], sml[:, 24:32], AF.Ln, bias=1.0)
            P.stt(sml[:, 40:48], xg, 0.0, sml[:, 32:40], ALU.max, ALU.add)
            g = sml[:, 48:56]
            P.tt(g, sml[:, 40:48], nA[:], ALU.mult)
            yield
            ps = nextpf()
            P.mm(ps[:, 0:8], self.U[d][:], g)
            P.mm(ps[:, 8:16], self.ones_f[:], g)
            gc = sml[:, 56:64]
            gl = sml[:, 64:72]
            P.copy(sml[:, 56:72], ps[:, 0:16])
            for (src, dst) in ((kn_b, kT), (qn_b, qT)):
                for h in range(NH):
                    P.tr(ptd[:, h, :], src[:, h, :], self.ident_b[:])
                self.evac(dst[:], ptd[:], d)
                yield
            egc = sml[:, 72:80]
            P.act(egc, gc, AF.Exp)
            egl = sml[:, 80:88]
            P.act(egl, gl, AF.Exp)
            kdc = sml[:, 88:96]
            P.tt(kdc, gl, gc, ALU.subtract)
            P.act(kdc, kdc, AF.Exp)
            bq = sml[:, 96:104]
            P.tt(bq, beta, egc, ALU.mult)
            yield
            P.tt(vb[:], v_b, bc(beta), ALU.mult)
            P.tt(kbe[:], kn_b, bc(bq), ALU.mult, eng="pool")
            P.tt(kdec[:], kn_b, bc(kdc), ALU.mult, eng="pool")
            P.tt(qd_b[:], qn_b, bc(egc), ALU.mult)
            P.tt(rhsR[:], self.U[d][:].unsqueeze(1).to_broadcast([128, NH, HD]), bc(g), ALU.mult, eng="pool")
            yield
            for h in range(NH):
                P.tr(ptd[:, h, :], qd_b[:, h, :], self.ident_b[:])
            self.evac(qdT[:], ptd[:], d + 1)
            yield
            for hg in range(2):
                ps = nextpf()
                P.mm(ps[:], self.ones_f[:], rhsR[:, hg * 4:(hg + 1) * 4, :].rearrange("p h e -> p (h e)"), start=True, stop=False)
                P.mm(ps[:], self.ident_f[:], self.BM[d][:].rearrange("p h e -> p (h e)"), start=False, stop=True)
                for hh in range(4):
                    h = hg * 4 + hh
                    P.act(Dm[:, h, :], ps[:, hh * 128:(hh + 1) * 128], AF.Exp, bias=gc[:, h:h + 1], scale=-1.0)
                yield
            for hg in range(2):
                hs = slice(hg * 4, (hg + 1) * 4)
                P.tt(As[:, hs, :], v4(mm4(kT, kT, hg)), self.SM[d][:].unsqueeze(1).to_broadcast([128, 4, HD]), ALU.mult)
                P.tt(qk_b[:, hs, :], v4(mm4(qT, kT, hg)), Dm[:, hs, :], ALU.mult)
                yield
            for h in range(NH):
                P.stt(L_b[:, h, :], As[:, h, :], beta[:, h:h + 1], Dm[:, h, :], ALU.mult, ALU.mult)
            yield
            for (src, dst) in ((L_b, N_b), (qk_b, qkT)):
                for h in range(NH):
                    P.tr(ptd[:, h, :], src[:, h, :], self.ident_b[:])
                self.evac(dst[:], ptd[:], d)
                yield
            P.tt(Ld[:], L_b[:], mb(self.BD8), ALU.mult)
            P.tt(Nd[:], N_b[:], mb(self.BD8), ALU.mult, eng="pool")
            P.tt(Tw[0][:], mb(self.ident_b), Ld[:], ALU.subtract)
            P.tt(Xw[0][:], mb(self.ident_b), Nd[:], ALU.subtract, eng="pool")
            yield
            cur = 0
            for (Pa, Qa, Pprev, Qprev) in ((P1, Q1, Ld, Nd), (P2, Q2, P1, Q1)):
                for hg in range(2):
                    hs = slice(hg * 4, (hg + 1) * 4)
                    self.evac(Pa[:, hs, :], v4(mm4(Qprev, Pprev, hg)), 1)
                    self.evac(Qa[:, hs, :], v4(mm4(Pprev, Qprev, hg)), 1)
                yield
                nxt = 1 - cur
                for hg in range(2):
                    hs = slice(hg * 4, (hg + 1) * 4)
                    P.tt(Tw[nxt][:, hs, :], Tw[cur][:, hs, :], v4(mm4(Qa, Tw[cur], hg)), ALU.add)
                    P.tt(Xw[nxt][:, hs, :], Xw[cur][:, hs, :], v4(mm4(Pa, Xw[cur], hg)), ALU.add)
                yield
                cur = nxt
            Cs, CTs, Zb, Zpb = Ld, Nd, P1, Q1
            for sz in (8, 16, 32, 64):
                P.tt(Cs[:], L_b[:], mb(self.MS[sz]), ALU.mult)
                P.tt(CTs[:], N_b[:], mb(self.MS[sz]), ALU.mult, eng="pool")
                for hg in range(2):
                    hs = slice(hg * 4, (hg + 1) * 4)
                    self.evac(Zb[:, hs, :], v4(mm4(Cs, Xw[cur], hg)), 1)
                    if sz < 64:
                        self.evac(Zpb[:, hs, :], v4(mm4(CTs, Tw[cur], hg)), 1)
                yield
                nxt = 1 - cur
                for hg in range(2):
                    hs = slice(hg * 4, (hg + 1) * 4)
                    P.tt(Xw[nxt][:, hs, :], Xw[cur][:, hs, :], v4(mm4(Tw[cur], Zb, hg)), ALU.subtract)
                    if sz < 64:
                        P.tt(Tw[nxt][:, hs, :], Tw[cur][:, hs, :], v4(mm4(Xw[cur], Zpb, hg)), ALU.subtract)
                yield
                cur = nxt
            TTm = Xw[cur]
            for hg in range(2):
                hs = slice(hg * 4, (hg + 1) * 4)
                self.evac(u32[:, hs, :], v4(mm4(TTm, vb, hg)), hg)
                self.evac(wT[:, hs, :], v4(mm4(kbe, TTm, hg)), hg + 1)
            yield
            for hg in range(2):
                hs = slice(hg * 4, (hg + 1) * 4)
                P.tt(vnew[:, hs, :], u32[:, hs, :], v4(mm4(wT, Sb, hg)), ALU.subtract)
                yield
                if kind == "l":
                    ps2 = nextpf()
                    for hh in range(4):
                        h = hg * 4 + hh
                        o_ = ps2[:, hh * 128:(hh + 1) * 128]
                        P.mm(o_, qdT[:, h, :], Sb[:, h, :], start=True, stop=False)
                        P.mm(o_, qkT[:, h, :], vnew[:, h, :], start=False, stop=True)
                    self.evac(o32[:, hs, :], v4(ps2), hg)
                ps3 = mm4(kdec, vnew, hg)
                P.tt(Stmp[:, hs, :], S32[:, hs, :], bc(egl[:, hs]), ALU.mult, eng="pool")
                P.tt(S32[:, hs, :], Stmp[:, hs, :], v4(ps3), ALU.add)
                P.copy(Sb[:, hs, :], S32[:, hs, :], eng="act")
                yield
            if kind == "l":
                P.dma("pool", OUT[i * 128:(i + 1) * 128, :], o32[:].rearrange("p h e -> p (h e)"))
            yield

    def phase_finish(self):
        nc, P = self.nc, self.P
        CT = self.CTXL
        with self.scope() as es:
            sb = lambda n, s, dt=F32: self.sb(es, n, s, dt)
            onw = self.bcast_load(es, "onw", self.onorm, HD)
            wo = self.load_cast(es, "wouta", self.w_out_a, D)
            Gm = self.load_mod(es, "Gm", 2)
            of_ = [sb("of_", [128, NH, HD]) for _ in range(2)]
            ob_ = [sb("ob_", [128, NH, HD]) for _ in range(2)]
            zt_ = [sb("zt", [128, D]) for _ in range(2)]
            xt_ = [sb("xt", [128, D]) for _ in range(2)]
            osq_ = [sb("osq", [128, NH, HD]) for _ in range(2)]
            orn_ = [sb("orn", [128, 4 * NH]) for _ in range(2)]
            yg_ = [sb("yg", [128, D], BF16) for _ in range(2)]
            ygT = [sb("ygT", [128, 8, 128], BF16) for _ in range(2)]
            x1t = [sb("x1t", [128, D]) for _ in range(2)]
            bc = lambda ap: ap.unsqueeze(2).to_broadcast([128, ap.shape[1], HD])

            def loads(i):
                if i >= self.NT:
                    return
                sl = i % 2
                P.dma("sp", of_[sl][:].rearrange("p h e -> p (h e)"), self.OF[i * 128:(i + 1) * 128, :])
                P.dma("sp", ob_[sl][:].rearrange("p h e -> p (h e)"), self.OB[i * 128:(i + 1) * 128, :])
                P.dma("sp", zt_[sl][:], self.P0[CT + i * 128:CT + (i + 1) * 128, 3 * D:4 * D])
                P.dma("sp", xt_[sl][:], self.x[i * 128:(i + 1) * 128, :])

            loads(0)
            loads(1)

            def fbody(i):
                sl = i % 2
                o32, ob, zt, xt = of_[sl], ob_[sl], zt_[sl], xt_[sl]
                osq, orn, yg = osq_[sl], orn_[sl], yg_[sl]
                P.tt(o32[:], o32[:], ob[:], ALU.add, eng="pool")
                yield
                P.act(osq[:], o32[:], AF.Square)
                P.red(orn[:, 0:8], osq[:], ALU.add)
                P.ts(orn[:, 8:16], orn[:, 0:8], 1.0 / HD, ALU.mult, EPS, ALU.add)
                P.act(orn[:, 16:24], orn[:, 8:16], AF.Sqrt)
                P.recip(orn[:, 24:32], orn[:, 16:24])
                yield
                P.tt(osq[:], o32[:], bc(orn[:, 24:32]), ALU.mult)
                P.tt(osq[:], osq[:], onw[:].unsqueeze(1).to_broadcast([128, NH, HD]), ALU.mult, eng="pool")
                P.act(zt[:], zt[:], AF.Silu)
                P.tt(yg[:], osq[:].rearrange("p h e -> p (h e)"), zt[:], ALU.mult)
                yield
                self.transpose8(ygT[sl], yg, self.pt[sl], i)
                yield
                x1 = x1t[sl]
                for half in range(2):
                    ps = self.pf[(2 * i + half) % 6]
                    for kc in range(8):
                        P.mm(ps[:], ygT[sl][:, kc, :], wo[:, kc, half * 512:(half + 1) * 512], start=(kc == 0), stop=(kc == 7))
                    cs = slice(half * 512, (half + 1) * 512)
                    P.tt(x1[:, cs], ps[:], Gm[:, cs], ALU.mult)
                    P.tt(x1[:, cs], x1[:, cs], xt[:, cs], ALU.add, eng="pool")
                loads(i + 2)
                yield
                P.dma("pool", self.X1[i * 128:(i + 1) * 128, :], x1[:], w=[("x1", i)])
                yield
            self.run2(self.NT, fbody)

    def phase_moe(self, layer, XIN, XOUT, final):
        nc, P = self.nc, self.P
        NT, NB = self.NT, self.NBLK
        xin_key = "x1" if layer == 0 else "x3"
        with self.scope() as es:
            sb = lambda n, s, dt=F32: self.sb(es, n, s, dt)
            A = self.load_mod(es, "Af", 6 * layer + 3)
            B = self.load_mod(es, "Bf", 6 * layer + 4)
            G = self.load_mod(es, "Gf", 6 * layer + 5)
            rt = sb("rt", [128, 8, 36])
            P.dma("sp", rt[:], self.router[layer].rearrange("(kc p) n -> p kc n", p=128))
            E1 = sb("E1", [128, NT]); E2 = sb("E2", [128, NT])
            R1 = sb("R1", [128, NT]); R2 = sb("R2", [128, NT])
            W1 = sb("W1", [128, NT]); W2 = sb("W2", [128, NT])
            carry = sb("carry", [128, NE])
            P.memset(carry[:], 0.0)
            iotaE = sb("iotaE", [128, NE])
            P.copy(iotaE[:], self.coli[:, 0:NE])
            with self.scope() as e1:
                s1 = lambda n, s, dt=F32: self.sb(e1, n, s, dt)
                xt = [s1("xt", [128, D]) for _ in range(2)]
                bufs = []
                for _ in range(2):
                    bufs.append(dict(
                        sq=s1("sq", [128, D]), ss=s1("ss", [128, 4]), hf=s1("hf", [128, D]), hb=s1("hb", [128, D], BF16),
                        hT=s1("hT", [128, 8, 128]), lg=s1("lg", [128, 36]), sm=s1("sm", [128, 64]),
                        lem=s1("lem", [128, NE]), top8=s1("top8", [128, 8]), m1=s1("m1", [128, NE]),
                        m12=s1("m12", [128, NE]), m2=s1("m2", [128, NE]), m12b=s1("m12b", [128, NE], BF16),
                        pos=s1("pos", [128, NE]), tmp=s1("tmp", [128, NE])))

                def rt_body(i):
                    bb = bufs[i % 2]
                    sq, ss, hf, hb, hT, lg, sm = bb["sq"], bb["ss"], bb["hf"], bb["hb"], bb["hT"], bb["lg"], bb["sm"]
                    lem, top8, m1, m12, m2, m12b, pos, tmp = bb["lem"], bb["top8"], bb["m1"], bb["m12"], bb["m2"], bb["m12b"], bb["pos"], bb["tmp"]
                    x_ = xt[i % 2]
                    P.dma("sp", x_[:], XIN[i * 128:(i + 1) * 128, :], r=[(xin_key, i)])
                    self.modulate(x_, A, B, sq, ss, hf, None)
                    P.copy(hb[:], hf[:], eng="act")
                    P.dma("pool", self.HF[i * 128:(i + 1) * 128, :], hb[:], w=[("hf", i)])
                    yield
                    for half in range(2):
                        ps = self.pf[half]
                        for kk in range(4):
                            kc = half * 4 + kk
                            P.tr(ps[:, kk * 128:(kk + 1) * 128], hf[:, kc * 128:(kc + 1) * 128], self.ident_f[:])
                        self.evac(hT[:, half * 4:(half + 1) * 4, :], ps[:].rearrange("p (k e) -> p k e", e=128), half)
                    yield
                    ps = self.pf[2 + 2 * (i % 2)]
                    for kc in range(8):
                        P.mm(ps[:, 0:36], hT[:, kc, :], rt[:, kc, :], start=(kc == 0), stop=(kc == 7))
                    P.copy(lg[:], ps[:, 0:36])
                    yield
                    P.red(sm[:, 0:1], lg[:, 0:4], ALU.max)
                    P.ts(sm[:, 1:2], sm[:, 0:1], -1.0, ALU.mult)
                    P.act(sm[:, 4:8], lg[:, 0:4], AF.Exp, bias=sm[:, 1:2])
                    P.red(sm[:, 2:3], sm[:, 4:8], ALU.add)
                    P.recip(sm[:, 3:4], sm[:, 2:3])
                    P.ts(sm[:, 8:12], lg[:, 0:4], sm[:, 0:1], ALU.is_ge)
                    P.ts(sm[:, 12:16], sm[:, 8:12], BIG, ALU.mult, -BIG, ALU.add)
                    P.tt(lem[:].rearrange("p (g e) -> p g e", e=8), lg[:, 4:36].rearrange("p (g e) -> p g e", e=8),
                         sm[:, 12:16].unsqueeze(2).to_broadcast([128, 4, 8]), ALU.add)
                    yield
                    P.op("dve", lambda e, o=top8, i_=lem: e.max(out=o[:], in_=i_[:]), r=[lem], w=[top8])
                    P.ts(m1[:], lem[:], top8[:, 0:1], ALU.is_ge)
                    P.ts(m12[:], lem[:], top8[:, 1:2], ALU.is_ge)
                    P.tt(m2[:], m12[:], m1[:], ALU.subtract)
                    P.copy(m12b[:], m12[:], eng="act")
                    yield
                    P.tt(sm[:, 16:17], top8[:, 0:1], top8[:, 1:2], ALU.subtract)
                    P.act(sm[:, 17:18], sm[:, 16:17], AF.Sigmoid)
                    P.tt(W1[:, i:i + 1], sm[:, 17:18], sm[:, 3:4], ALU.mult)
                    P.tt(W2[:, i:i + 1], sm[:, 3:4], W1[:, i:i + 1], ALU.subtract)
                    yield
                    ps = self.pf[3 + 2 * (i % 2)]
                    P.mm(ps[:, 0:NE], self.Us_b[:], m12b[:])
                    P.mm(ps[:, NE:2 * NE], self.ones_b[:], m12b[:])
                    P.tt(pos[:], ps[:, 0:NE], carry[:], ALU.add)
                    P.tt(carry[:], carry[:], ps[:, NE:2 * NE], ALU.add)
                    for (mk, Ed, Rd) in ((m1, E1, R1), (m2, E2, R2)):
                        P.tt(tmp[:], mk[:], iotaE[:], ALU.mult)
                        P.red(Ed[:, i:i + 1], tmp[:], ALU.add)
                        P.tt(tmp[:], mk[:], pos[:], ALU.mult)
                        P.red(Rd[:, i:i + 1], tmp[:], ALU.add)
                    yield
                self.run2(NT, rt_body)
            D1i = sb("D1i", [128, NT], I32); D2i = sb("D2i", [128, NT], I32)
            WIi = sb("WIi", [128, NB], I32)
            with self.scope() as e2:
                s2 = lambda n, s, dt=F32: self.sb(e2, n, s, dt)
                NTH = 2 * NT + 1
                thr = s2("thr", [128, NTH])
                P.op("pool", lambda e: e.iota(thr[:], pattern=[[128, NTH]], base=0, channel_multiplier=0,
                                               allow_small_or_imprecise_dtypes=True), w=[thr])
                cmp = s2("cmp", [128, NE, NTH])
                P.tt(cmp[:], carry[:].unsqueeze(2).to_broadcast([128, NE, NTH]),
                     thr[:].unsqueeze(1).to_broadcast([128, NE, NTH]), ALU.is_gt)
                padded = s2("padded", [128, NE])
                P.red(padded[:], cmp[:], ALU.add)
                P.ts(padded[:], padded[:], 128.0, ALU.mult)
                le = s2("le", [128, NE, NE])
                P.tt(le[:], self.coli[:, 0:NE].unsqueeze(2).to_broadcast([128, NE, NE]),
                     self.coli[:, 0:NE].unsqueeze(1).to_broadcast([128, NE, NE]), ALU.is_ge)
                P.tt(le[:], le[:], padded[:].unsqueeze(1).to_broadcast([128, NE, NE]), ALU.mult)
                ends = s2("ends", [128, NE]); starts = s2("starts", [128, NE])
                P.red(ends[:], le[:], ALU.add)
                P.tt(starts[:], ends[:], padded[:], ALU.subtract)
                oh = s2("oh", [128, NT, NE])
                dd = s2("dd", [128, NT])
                for (Ed, Rd, Di) in ((E1, R1, D1i), (E2, R2, D2i)):
                    P.tt(oh[:], Ed[:].unsqueeze(2).to_broadcast([128, NT, NE]),
                         iotaE[:].unsqueeze(1).to_broadcast([128, NT, NE]), ALU.is_equal)
                    P.tt(oh[:], oh[:], starts[:].unsqueeze(1).to_broadcast([128, NT, NE]), ALU.mult)
                    P.red(dd[:], oh[:], ALU.add)
                    P.tt(dd[:], dd[:], Rd[:], ALU.add)
                    P.copy(Di[:], dd[:])
                bthr = s2("bthr", [128, NB])
                P.op("pool", lambda e: e.iota(bthr[:], pattern=[[128, NB]], base=0, channel_multiplier=0,
                                               allow_small_or_imprecise_dtypes=True), w=[bthr])
                cb = s2("cb", [128, NB, NE])
                P.tt(cb[:], ends[:].unsqueeze(1).to_broadcast([128, NB, NE]),
                     bthr[:].unsqueeze(2).to_broadcast([128, NB, NE]), ALU.is_le)
                be = s2("be", [128, NB + 2])
                P.memset(be[:, 0:2], -1.0)
                P.red(be[:, 2:NB + 2], cb[:], ALU.add)
                P.ts(be[:, 2:NB + 2], be[:, 2:NB + 2], float(NE - 1), ALU.min)
                need = s2("need", [128, NB])
                P.tt(need[:], be[:, 2:NB + 2], be[:, 1:NB + 1], ALU.not_equal)
                P.memset(need[:, NB // 2:NB // 2 + 1], 1.0)
                wi = s2("wi", [128, NB])
                P.ts(wi[:], be[:, 2:NB + 2], 128.0, ALU.mult, self.rowi[:, 0:1], ALU.add)
                P.ts(wi[:], wi[:], -1.0e6, ALU.add)
                P.tt(wi[:], wi[:], need[:], ALU.mult)
                P.ts(wi[:], wi[:], 1.0e6, ALU.add)
                P.copy(WIi[:], wi[:])
            with self.scope() as e3:
                s3 = lambda n, s, dt=F32: self.sb(e3, n, s, dt)
                hbt = [s3("hbt", [128, D], BF16) for _ in range(2)]
                for i in range(NT):
                    h_ = hbt[i % 2]
                    P.dma("sp", h_[:], self.HF[i * 128:(i + 1) * 128, :], r=[("hf", i)])
                    for Di in (D1i, D2i):
                        def sc(e, Di=Di, h_=h_, i=i):
                            if "bx" not in self.regcache:
                                self.regcache["bx"] = e.to_reg(self.NBLK * 128 - 1)
                            return e.indirect_dma_start(
                                out=self.XB[:, :], out_offset=bass.IndirectOffsetOnAxis(ap=Di[:, i:i + 1], axis=0),
                                in_=h_[:, :], in_offset=None, bounds_check=self.regcache["bx"], oob_is_err=False)
                        P.op("pool", sc, r=[h_, Di], w=[], dma=True)
            with self.scope() as e4:
                s4 = lambda n, s, dt=F32: self.sb(e4, n, s, dt)
                stg = [s4("stg", [128, 3 * 4096]) for _ in range(2)]
                w1b_ = [s4("w1b", [128, 8, DE], BF16) for _ in range(2)]
                w3b_ = [s4("w3b", [128, 8, DE], BF16) for _ in range(2)]
                w2b_ = [s4("w2b", [128, 4, D], BF16) for _ in range(2)]
                xbt = [s4("xbt", [128, D], BF16) for _ in range(2)]
                xT_ = [s4("xT", [128, 8, 128], BF16) for _ in range(2)]
                h1_ = [s4("h1", [128, DE]) for _ in range(2)]
                gb_ = [s4("gb", [128, DE], BF16) for _ in range(2)]
                gT_ = [s4("gT", [128, 4, 128], BF16) for _ in range(2)]
                yo = [s4("yo", [128, D]) for _ in range(2)]
                HB = NB // 2
                lane = lambda b: 0 if b < HB else 1

                def gather(b, ln):
                    if b >= NB or lane(b) != ln:
                        return
                    sl = ln

                    def gw(e, b=b, sl=sl):
                        if "bc" not in self.regcache:
                            self.regcache["bc"] = e.to_reg(NE * 128 - 1)
                        return e.indirect_dma_start(
                            out=stg[sl][:, :], out_offset=None, in_=self.wall[layer][:, :],
                            in_offset=bass.IndirectOffsetOnAxis(ap=WIi[:, b:b + 1], axis=0),
                            bounds_check=self.regcache["bc"], oob_is_err=False)
                    P.op("pool", gw, r=[WIi], w=[stg[sl]], dma=True)

                def xload(b, ln):
                    if b >= NB or lane(b) != ln:
                        return
                    P.dma("sp", xbt[ln][:], self.XB[b * 128:(b + 1) * 128, :], r=[], w=[xbt[ln]])

                gather(0, 0)
                gather(HB, 1)
                xload(0, 0)
                xload(HB, 1)
                def blk(b):
                    sl = lane(b)
                    w1b, w3b, w2b, xT, h1, gb, gT = w1b_[sl], w3b_[sl], w2b_[sl], xT_[sl], h1_[sl], gb_[sl], gT_[sl]
                    P.copy(w1b[:].rearrange("p k n -> p (k n)"), stg[sl][:, 0:4096], eng="dve")
                    P.copy(w3b[:].rearrange("p k n -> p (k n)"), stg[sl][:, 4096:8192], eng="act")
                    P.copy(w2b[:, 0:2, :].rearrange("p k n -> p (k n)"), stg[sl][:, 8192:10240], eng="act")
                    P.copy(w2b[:, 2:4, :].rearrange("p k n -> p (k n)"), stg[sl][:, 10240:12288], eng="dve")
                    gather(b + 1, sl)
                    yield
                    self.transpose8(xT, xbt[sl], self.pt[sl], b)
                    xload(b + 1, sl)
                    yield
                    p1, p3 = self.pf[2 * sl], self.pf[2 * sl + 1]
                    for kc in range(8):
                        P.mm(p1[:], xT[:, kc, :], w1b[:, kc, :], start=(kc == 0), stop=(kc == 7))
                    for kc in range(8):
                        P.mm(p3[:], xT[:, kc, :], w3b[:, kc, :], start=(kc == 0), stop=(kc == 7))
                    yield
                    P.act(h1[:], p1[:], AF.Silu)
                    P.tt(gb[:], h1[:], p3[:], ALU.mult)
                    yield
                    pt_ = self.pt[1 - sl]
                    for fc in range(4):
                        P.tr(pt_[:, fc, :], gb[:, fc * 128:(fc + 1) * 128], self.ident_b[:])
                    self.evac(gT[:], pt_[:, 0:4, :], b + 1)
                    yield
                    y_ = yo[sl]
                    for half in range(2):
                        ps = self.pf[4 + half]
                        for fc in range(4):
                            P.mm(ps[:], gT[:, fc, :], w2b[:, fc, half * 512:(half + 1) * 512], start=(fc == 0), stop=(fc == 3))
                        self.evac(y_[:, half * 512:(half + 1) * 512], ps[:], half)
                    yield
                    P.dma("pool", self.YB[b * 128:(b + 1) * 128, :], y_[:], w=["yb_all"])
                    yield
                def lane_stream(ln):
                    for b in (range(0, HB) if ln == 0 else range(HB, NB)):
                        yield from blk(b)
                gens = [lane_stream(0), lane_stream(1)]
                while gens:
                    for g_ in list(gens):
                        try:
                            next(g_)
                        except StopIteration:
                            gens.remove(g_)
            with self.scope() as e5:
                s5 = lambda n, s, dt=F32: self.sb(e5, n, s, dt)
                y1 = [s5("y1", [128, D]) for _ in range(2)]
                y2 = [s5("y2", [128, D]) for _ in range(2)]
                xt = [s5("xt", [128, D]) for _ in range(2)]
                acc_ = [s5("acc", [128, D]) for _ in range(2)]
                xo_ = [s5("xo", [128, D]) for _ in range(2)]
                if final:
                    fn = self.bcast_load(e5, "fn", self.final_norm, D)
                    sq_ = [s5("sq", [128, D]) for _ in range(2)]; ss_ = [s5("ss", [128, 4]) for _ in range(2)]
                def cload(i):
                    if i >= NT:
                        return
                    sl = i % 2
                    for (yy, Di) in ((y1, D1i), (y2, D2i)):
                        def gy(e, yy=yy, Di=Di, i=i, sl=sl):
                            if "bx" not in self.regcache:
                                self.regcache["bx"] = e.to_reg(self.NBLK * 128 - 1)
                            return e.indirect_dma_start(
                                out=yy[sl][:, :], out_offset=None, in_=self.YB[:, :],
                                in_offset=bass.IndirectOffsetOnAxis(ap=Di[:, i:i + 1], axis=0),
                                bounds_check=self.regcache["bx"], oob_is_err=False)
                        P.op("pool", gy, r=[Di], w=[yy[sl]], dma=True)
                    P.dma("sp", xt[sl][:], XIN[i * 128:(i + 1) * 128, :], r=[(xin_key, i)])

                cload(0)
                cload(1)

                def cmb(i):
                    sl = i % 2
                    acc = acc_[sl]
                    if final:
                        sq, ss = sq_[sl], ss_[sl]
                    P.ts(acc[:], y1[sl][:], W1[:, i:i + 1], ALU.mult)
                    P.stt(acc[:], y2[sl][:], W2[:, i:i + 1], acc[:], ALU.mult, ALU.add)
                    P.tt(acc[:], acc[:], G[:], ALU.mult, eng="pool")
                    yield
                    xo = xo_[sl]
                    P.tt(xo[:], acc[:], xt[sl][:], ALU.add)
                    cload(i + 2)
                    yield
                    if not final:
                        P.dma("act", XOUT[i * 128:(i + 1) * 128, :], xo[:], w=[("x2", i)])
                    else:
                        P.act(sq[:], xo[:], AF.Square)
                        P.red(ss[:, 0:1], sq[:], ALU.add)
                        P.ts(ss[:, 1:2], ss[:, 0:1], 1.0 / D, ALU.mult, EPS, ALU.add)
                        P.act(ss[:, 2:3], ss[:, 1:2], AF.Sqrt)
                        P.recip(ss[:, 3:4], ss[:, 2:3])
                        P.stt(sq[:], xo[:], ss[:, 3:4], fn[:], ALU.mult, ALU.mult)
                        P.dma("act", XOUT[i * 128:(i + 1) * 128, :], sq[:], w=[("out", i)])
                    yield
                self.run2(NT, cmb)

    def phase_convD(self):
        nc, P = self.nc, self.P
        with self.scope() as es:
            sb = lambda n, s, dt=F32: self.sb(es, n, s, dt)
            wb = self.load_cast(es, "winb", self.w_in_b, 3 * D)
            A = self.load_mod(es, "A1", 6)
            B = self.load_mod(es, "B1", 7)
            xt = [sb("xt", [128, D]) for _ in range(2)]
            sq_ = [sb("sq", [128, D]) for _ in range(2)]; ss_ = [sb("ss", [128, 4]) for _ in range(2)]
            hf_ = [sb("hf", [128, D]) for _ in range(2)]; hb_ = [sb("hb", [128, D], BF16) for _ in range(2)]
            hT = [sb("hT", [128, 8, 128], BF16) for _ in range(2)]
            gbt = [sb("gbt", [128, D]) for _ in range(2)]
            gcc_ = [sb("gc_", [128, D]) for _ in range(2)]
            ut = [sb("ut", [128, D]) for _ in range(2)]
            def dbody(i):
                x_ = xt[i % 2]
                sq, ss, hf, hb, gc_ = sq_[i % 2], ss_[i % 2], hf_[i % 2], hb_[i % 2], gcc_[i % 2]
                P.dma("sp", x_[:], self.X2[i * 128:(i + 1) * 128, :], r=[("x2", i)])
                self.modulate(x_, A, B, sq, ss, hf, hb)
                yield
                hT_ = hT[i % 2]
                self.transpose8(hT_, hb, self.pt[i % 2], i)
                yield
                g_, u_ = gbt[i % 2], ut[i % 2]
                for g in range(6):
                    ps = self.pf[(g + 3 * (i % 2)) % 6]
                    for kc in range(8):
                        P.mm(ps[:], hT_[:, kc, :], wb[:, kc, g * 512:(g + 1) * 512], start=(kc == 0), stop=(kc == 7))
                    cs = slice((g % 2) * 512, (g % 2 + 1) * 512)
                    if g < 2:
                        self.evac(g_[:, cs], ps[:], g)
                    elif g < 4:
                        self.evac(gc_[:, cs], ps[:], g)
                    else:
                        P.tt(u_[:, cs], ps[:], gc_[:, cs], ALU.mult)
                    if g % 2 == 1:
                        yield
                P.dma("pool", self.GB[i * 128:(i + 1) * 128, :], g_[:], w=[("gb", i)])
                P.dma("pool", self.U1[i * 128:(i + 1) * 128, :], u_[:], w=[("u1", i)])
                yield
            self.run2(self.NT, dbody)

    def phase_convE(self):
        nc, P = self.nc, self.P
        NT = self.NT
        H = D // 2
        with self.scope() as es:
            sb = lambda n, s, dt=F32: self.sb(es, n, s, dt)
            wo = self.load_cast(es, "woutb", self.w_out_b, D)
            cb = [self.bcast_load(es, "cb%d" % s, self.conv_b[s], D) for s in range(3)]
            Gm = self.load_mod(es, "Gm1", 8)
            mm_ = sb("mm_", [128, 4])
            P.ts(mm_[:, 0:1], self.rowi[:, 0:1], 64.0, ALU.is_equal)
            P.ts(mm_[:, 1:2], self.rowi[:, 0:1], 0.0, ALU.is_equal)
            P.tt(mm_[:, 0:1], mm_[:, 0:1], mm_[:, 1:2], ALU.add)
            P.ts(mm_[:, 0:1], mm_[:, 0:1], -1.0, ALU.mult, 1.0, ALU.add)
            P.ts(mm_[:, 2:3], self.rowi[:, 0:1], 63.0, ALU.is_equal)
            P.ts(mm_[:, 3:4], self.rowi[:, 0:1], 127.0, ALU.is_equal)
            P.tt(mm_[:, 2:3], mm_[:, 2:3], mm_[:, 3:4], ALU.add)
            P.ts(mm_[:, 2:3], mm_[:, 2:3], -1.0, ALU.mult, 1.0, ALU.add)
            um = [sb("um", [128, D]) for _ in range(2)]
            up = [sb("up", [128, D]) for _ in range(2)]
            uc = [sb("uc", [128, D]) for _ in range(2)]
            gbt = [sb("gbt", [128, D]) for _ in range(2)]
            xt = [sb("xt", [128, D]) for _ in range(2)]
            yc_ = [sb("yc", [128, D]) for _ in range(2)]
            vbb_ = [sb("vb_", [128, D], BF16) for _ in range(2)]
            vT_ = [sb("vT", [128, 8, 128], BF16) for _ in range(2)]
            x3t = [sb("x3t", [128, D]) for _ in range(2)]
            for t in um + up:
                P.memset(t[:], 0.0, eng="pool")
            def ebody(i):
                sl = i % 2
                r0 = i * 128
                yc, vb_, vT = yc_[sl], vbb_[sl], vT_[sl]
                um_, up_, uc_ = um[sl], up[sl], uc[sl]
                P.dma("sp", uc_[:], self.U1[r0:r0 + 128, :], r=[("u1", i)])
                P.dma("sp", gbt[sl][:], self.GB[r0:r0 + 128, :], r=[("gb", i)])
                P.dma("sp", xt[sl][:], self.X2[r0:r0 + 128, :], r=[("x2", i)])
                if i == 0:
                    P.dma("sp", um_[1:128, 0:H], self.U1[0:127, 0:H], r=[("u1", 0)], w=[um_])
                else:
                    P.dma("sp", um_[:, 0:H], self.U1[r0 - 1:r0 + 127, 0:H], r=[("u1", i), ("u1", i - 1)], w=[um_])
                if i == NT - 1:
                    P.dma("sp", up_[0:127, 0:H], self.U1[r0 + 1:r0 + 128, 0:H], r=[("u1", i)], w=[up_])
                else:
                    P.dma("sp", up_[:, 0:H], self.U1[r0 + 1:r0 + 129, 0:H], r=[("u1", i), ("u1", i + 1)], w=[up_])
                if i == 0:
                    P.memset(um_[0:64, H:D], 0.0, eng="pool")
                    P.dma("sp", um_[64:128, H:D], self.U1[0:64, H:D], r=[("u1", 0)], w=[um_])
                else:
                    P.dma("sp", um_[:, H:D], self.U1[r0 - 64:r0 + 64, H:D], r=[("u1", i), ("u1", i - 1)], w=[um_])
                if i == NT - 1:
                    P.memset(up_[64:128, H:D], 0.0, eng="pool")
                    P.dma("sp", up_[0:64, H:D], self.U1[r0 + 64:r0 + 128, H:D], r=[("u1", i)], w=[up_])
                else:
                    P.dma("sp", up_[:, H:D], self.U1[r0 + 64:r0 + 192, H:D], r=[("u1", i), ("u1", i + 1)], w=[up_])
                yield
                P.ts(um_[:, 0:H], um_[:, 0:H], mm_[:, 0:1], ALU.mult)
                P.ts(up_[:, 0:H], up_[:, 0:H], mm_[:, 2:3], ALU.mult, eng="pool")
                P.tt(um_[:], um_[:], cb[0][:], ALU.mult)
                P.tt(up_[:], up_[:], cb[2][:], ALU.mult, eng="pool")
                yield
                P.tt(yc[:], uc_[:], cb[1][:], ALU.mult)
                P.tt(yc[:], yc[:], um_[:], ALU.add)
                P.tt(yc[:], yc[:], up_[:], ALU.add, eng="pool")
                P.tt(vb_[:], yc[:], gbt[sl][:], ALU.mult)
                yield
                self.transpose8(vT, vb_, self.pt[sl], i)
                yield
                x3 = x3t[sl]
                for half in range(2):
                    ps = self.pf[half + 2 * sl]
                    for kc in range(8):
                        P.mm(ps[:], vT[:, kc, :], wo[:, kc, half * 512:(half + 1) * 512], start=(kc == 0), stop=(kc == 7))
                    cs = slice(half * 512, (half + 1) * 512)
                    P.tt(x3[:, cs], ps[:], Gm[:, cs], ALU.mult)
                    P.tt(x3[:, cs], x3[:, cs], xt[sl][:, cs], ALU.add, eng="pool")
                P.dma("pool", self.X3[r0:r0 + 128, :], x3[:], w=[("x3", i)])
                yield
            self.run2(NT, ebody)


def make_in_maps(inputs, S, CTXL, n_cores, phases=99):
    f = lambda a: np.ascontiguousarray(np.asarray(a, dtype=np.float32))
    moe = {}
    if phases >= 4:
        w1 = f(inputs["w1"]); w3 = f(inputs["w3"]); w2 = f(inputs["w2"])
        for l in range(2):
            wall = np.empty((NE, 128, 3, 4096), np.float32)
            wall[:, :, 0, :] = w1[l].reshape(NE, 8, 128, DE).transpose(0, 2, 1, 3).reshape(NE, 128, 4096)
            wall[:, :, 1, :] = w3[l].reshape(NE, 8, 128, DE).transpose(0, 2, 1, 3).reshape(NE, 128, 4096)
            wall[:, :, 2, :] = w2[l].reshape(NE, 4, 128, D).transpose(0, 2, 1, 3).reshape(NE, 128, 4096)
            moe["wall%d" % l] = wall.reshape(NE * 128, 3 * 4096)
    router = np.ascontiguousarray(np.concatenate([f(inputs["router_g"]), f(inputs["router_e"])], axis=-1))
    shared = {
        "c_ctx": f(inputs["c_ctx"]), "ada_w": f(inputs["ada_w"]), "ada_b": f(inputs["ada_b"]),
        "norm_mix": f(inputs["norm_mix"]), "norm_ffn": f(inputs["norm_ffn"]),
        "w_in_a": f(inputs["w_in_a"])[0], "conv_a": f(inputs["conv_a"])[0], "a_log": f(inputs["a_log_a"])[0],
        "dt_bias": f(inputs["dt_bias_a"])[0], "onorm": f(inputs["onorm_a"])[0], "w_out_a": f(inputs["w_out_a"])[0],
        "w_in_b": f(inputs["w_in_b"])[0], "conv_b": f(inputs["conv_b"])[0], "w_out_b": f(inputs["w_out_b"])[0],
        "router": router, "final_norm": f(inputs["final_norm"]),
    }
    shared.update(moe)
    x = f(inputs["x"]); c = f(inputs["c"]); ctx = f(inputs["ctx"])
    maps = []
    for b in range(n_cores):
        m = dict(shared)
        m["x"] = np.ascontiguousarray(x[b, :S])
        m["c"] = np.ascontiguousarray(c[b])
        m["ctx"] = np.ascontiguousarray(ctx[b, :CTXL])
        maps.append(m)
    return maps


def run(inputs, S, CTXL, n_cores, phases=99, debug=(), trace=False):
    bld = Builder(S, CTXL, phases=phases, debug=debug)
    nc = bld.build()
    import time as _t
    t0 = _t.time()
    maps = make_in_maps(inputs, S, CTXL, n_cores, phases)
    print("in_maps", _t.time() - t0, flush=True)
    res = run_bass_kernel_spmd(nc, maps, core_ids=list(range(n_cores)))
    print("spmd", _t.time() - t0, flush=True)
    return res, bld


def kernel(**inputs):
    res, _ = run(inputs, 8192, 256, 8)
    return np.stack([np.asarray(r["out"], dtype=np.float32) for r in res.results], axis=0)
```

```python
import numpy as np
from contextlib import ExitStack, contextmanager
import concourse.bass as bass
import concourse.mybir as mybir
from concourse.bass_utils import run_bass_kernel_spmd

F32 = mybir.dt.float32
BF16 = mybir.dt.bfloat16
I32 = mybir.dt.int32
AF = mybir.ActivationFunctionType
ALU = mybir.AluOpType
AX = mybir.AxisListType

D = 1024
NH = 8
HD = 128
NE = 32
DE = 512
EPS = 1e-6
BIG = 30000.0
GRID_W = 64


class Op:
    __slots__ = ("eng", "fn", "deps", "need", "sem", "val", "isdma", "idx")


class Prog:
    def __init__(self, nc, ndma=32):
        self.nc = nc
        self.es = ExitStack()
        self.engs = ["pe", "dve", "act", "pool", "sp"]
        self.q = {k: [] for k in self.engs}
        self.esem = {k: self.es.enter_context(nc.semaphore("s_" + k)) for k in self.engs}
        self.dsem = [self.es.enter_context(nc.semaphore("d%d" % i)) for i in range(ndma)]
        self.dlast = [None] * ndma
        self.dcount = [0] * ndma
        self.dnext = 0
        self.lastw = {}
        self.readers = {}
        self.nops = 0

    @staticmethod
    def key(x):
        if isinstance(x, (str, tuple)):
            return x
        return x.name

    def op(self, eng, fn, r=(), w=(), dma=False):
        o = Op()
        o.eng = eng
        o.fn = fn
        o.need = False
        o.isdma = dma
        o.sem = None
        o.val = None
        o.idx = self.nops
        self.nops += 1
        deps = {}
        rk = [self.key(x) for x in r]
        wk = [self.key(x) for x in w]

        def add(d):
            if d is None:
                return
            if d.isdma:
                deps[("d", id(d))] = d
            else:
                if d.eng == eng and eng == "pe" and not dma:
                    return
                cur = deps.get(("e", d.eng))
                if cur is None or cur.idx < d.idx:
                    deps[("e", d.eng)] = d

        for k in rk:
            add(self.lastw.get(k))
        for k in wk:
            add(self.lastw.get(k))
            for rd in self.readers.get(k, ()):
                add(rd)
        if dma:
            i = self.dnext
            self.dnext = (self.dnext + 1) % len(self.dsem)
            add(self.dlast[i])
            self.dcount[i] += 16
            o.sem = self.dsem[i]
            o.val = self.dcount[i]
            self.dlast[i] = o
        o.deps = list(deps.values())
        for d in o.deps:
            d.need = True
        for k in rk:
            self.readers.setdefault(k, []).append(o)
        for k in wk:
            self.lastw[k] = o
            self.readers[k] = []
        self.q[eng].append(o)
        return o

    def dma(self, eng, out, in_, r=None, w=None, **kw):
        r = [in_] if r is None else r
        w = [out] if w is None else w
        return self.op(eng, lambda e: e.dma_start(out=out, in_=in_, **kw), r=r, w=w, dma=True)

    def mm(self, out, lhsT, rhs, start=True, stop=True, r=None, w=None):
        r = [lhsT, rhs] if r is None else r
        w = [out] if w is None else w
        return self.op("pe", lambda e: e.matmul(out, lhsT, rhs, start=start, stop=stop), r=r, w=w)

    def tr(self, out, in_, ident, r=None, w=None):
        r = [in_, ident] if r is None else r
        w = [out] if w is None else w
        return self.op("pe", lambda e: e.transpose(out, in_, ident), r=r, w=w)

    def act(self, out, in_, func, bias=None, scale=None, r=None, w=None):
        rr = [in_] + ([bias] if bias is not None and not isinstance(bias, (int, float)) else [])
        r = rr if r is None else r
        w = [out] if w is None else w
        kw = {}
        if bias is not None:
            kw["bias"] = bias
        if scale is not None:
            kw["scale"] = scale
        return self.op("act", lambda e: e.activation(out=out, in_=in_, func=func, **kw), r=r, w=w)

    def tt(self, out, in0, in1, op, eng="dve", r=None, w=None):
        r = [in0, in1] if r is None else r
        w = [out] if w is None else w
        return self.op(eng, lambda e: e.tensor_tensor(out=out, in0=in0, in1=in1, op=op), r=r, w=w)

    def ts(self, out, in0, s1, op0, s2=None, op1=None, eng="dve", r=None, w=None):
        rr = [in0] + [s for s in (s1, s2) if s is not None and not isinstance(s, (int, float))]
        r = rr if r is None else r
        w = [out] if w is None else w
        if op1 is None:
            return self.op(eng, lambda e: e.tensor_scalar(out=out, in0=in0, scalar1=s1, scalar2=None, op0=op0), r=r, w=w)
        return self.op(eng, lambda e: e.tensor_scalar(out=out, in0=in0, scalar1=s1, scalar2=s2, op0=op0, op1=op1), r=r, w=w)

    def stt(self, out, in0, scalar, in1, op0, op1, r=None, w=None):
        rr = [in0, in1] + ([scalar] if not isinstance(scalar, (int, float)) else [])
        r = rr if r is None else r
        w = [out] if w is None else w
        return self.op("dve", lambda e: e.scalar_tensor_tensor(out=out, in0=in0, scalar=scalar, in1=in1, op0=op0, op1=op1), r=r, w=w)

    def copy(self, out, in_, eng="dve", r=None, w=None):
        r = [in_] if r is None else r
        w = [out] if w is None else w
        if eng == "act":
            return self.op("act", lambda e: e.copy(out=out, in_=in_), r=r, w=w)
        return self.op(eng, lambda e: e.tensor_copy(out=out, in_=in_), r=r, w=w)

    def red(self, out, in_, op, axis=AX.X, r=None, w=None):
        r = [in_] if r is None else r
        w = [out] if w is None else w
        return self.op("dve", lambda e: e.tensor_reduce(out=out, in_=in_, axis=axis, op=op), r=r, w=w)

    def memset(self, ap, val, eng="dve"):
        return self.op(eng, lambda e: e.memset(ap, val), r=[], w=[ap])

    def recip(self, out, in_, r=None, w=None):
        r = [in_] if r is None else r
        w = [out] if w is None else w
        return self.op("dve", lambda e: e.reciprocal(out=out, in_=in_), r=r, w=w)

    def barrier(self):
        lasts = [d for d in self.dlast if d is not None]
        for e in self.engs:
            for o in reversed(self.q[e]):
                if not o.isdma and o.fn is not None:
                    lasts.append(o)
                    break
        for d in lasts:
            d.need = True
        for e in self.engs:
            b = Op()
            b.eng = e
            b.fn = None
            b.need = False
            b.isdma = False
            b.sem = None
            b.val = None
            b.idx = self.nops
            self.nops += 1
            b.deps = list(lasts)
            self.q[e].append(b)
        self.lastw = {}
        self.readers = {}

    def finish(self):
        fin = Op()
        fin.eng = "sp"
        fin.fn = None
        fin.need = False
        fin.isdma = False
        fin.idx = self.nops
        deps = [d for d in self.dlast if d is not None]
        for e in self.engs:
            for o in reversed(self.q[e]):
                if not o.isdma and o.fn is not None:
                    o.need = True
                    deps.append(o)
                    break
        fin.deps = deps
        self.q["sp"].append(fin)
        for e in self.engs:
            c = 0
            for o in self.q[e]:
                if o.isdma or o.fn is None:
                    continue
                if o.need:
                    c += 1
                    o.val = c
                    o.sem = self.esem[e]
        self.counts = {e: len(self.q[e]) for e in self.engs}

        def replay(name, e):
            waited = {}
            for o in self.q[name]:
                for d in o.deps:
                    sk = id(d.sem)
                    if waited.get(sk, 0) < d.val:
                        e.wait_ge(d.sem, d.val)
                        waited[sk] = d.val
                if o.fn is None:
                    continue
                ins = o.fn(e)
                if o.isdma:
                    ins.then_inc(o.sem, 16)
                elif o.need:
                    ins.then_inc(o.sem, 1)

        nc = self.nc
        with nc.Block() as block:
            @block.tensor
            def _(e):
                replay("pe", e)

            @block.vector
            def _(e):
                replay("dve", e)

            @block.scalar
            def _(e):
                replay("act", e)

            @block.gpsimd
            def _(e):
                replay("pool", e)

            @block.sync
            def _(e):
                replay("sp", e)
        self.es.close()


class Builder:
    def __init__(self, S, CTXL, phases=99, debug=()):
        self.S = S
        self.CTXL = CTXL
        self.NT = S // 128
        self.NC = CTXL // 128
        self.T = S
        self.NBLK = (2 * S) // 128 + NE
        self.phases = phases
        self.debug = set(debug)
        nc = self.nc = bass.Bass("TRN2", target_bir_lowering=False)
        self.P = Prog(nc)
        self.top = ExitStack()
        self._n = 0
        self.regcache = {}
        self.pfi = [0]

        def din(name, shape, dt=F32):
            return nc.dram_tensor(name, list(shape), dt, kind="ExternalInput").ap()

        self.x = din("x", [S, D])
        self.c = din("c", [D])
        self.ctx = din("ctx", [CTXL, D])
        self.c_ctx = din("c_ctx", [D])
        self.ada_w = din("ada_w", [2, D, 6 * D])
        self.ada_b = din("ada_b", [2, 6 * D])
        self.norm_mix = din("norm_mix", [2, D])
        self.norm_ffn = din("norm_ffn", [2, D])
        self.w_in_a = din("w_in_a", [D, 4 * D + 32])
        self.conv_a = din("conv_a", [3, 3 * D])
        self.a_log = din("a_log", [2, NH])
        self.dt_bias = din("dt_bias", [2, NH])
        self.onorm = din("onorm", [HD])
        self.w_out_a = din("w_out_a", [D, D])
        self.w_in_b = din("w_in_b", [D, 3 * D])
        self.conv_b = din("conv_b", [3, D])
        self.w_out_b = din("w_out_b", [D, D])
        self.router = din("router", [2, D, 36])
        if phases >= 4:
            self.wall = [din("wall%d" % l, [NE * 128, 3 * 4096]) for l in range(2)]
        self.final_norm = din("final_norm", [D])
        self.out = nc.dram_tensor("out", [S, D], F32, kind="ExternalOutput").ap()

        TT = S + CTXL
        self.MODP = self.scr("modp", [16, 128, D])
        self.P0 = self.scr("p0", [TT, 4 * D + 32])
        self.OF = self.scr("of", [S, D])
        self.OB = self.scr("ob", [S, D])
        self.QKV = self.scr("qkv", [TT, 3 * D], BF16)
        self.X1 = self.scr("x1", [S, D])
        self.X2 = self.scr("x2", [S, D])
        self.X3 = self.scr("x3", [S, D])
        self.HF = self.scr("hfb", [S, D], BF16)
        self.XB = self.scr("xb", [self.NBLK * 128, D], BF16)
        self.YB = self.scr("yb", [self.NBLK * 128, D])
        self.U1 = self.scr("u1", [S, D])
        self.GB = self.scr("gb", [S, D])

    @contextmanager
    def scope(self):
        with ExitStack() as es:
            yield es
            self.P.barrier()

    def run2(self, n, body, lag=0):
        def stream(par):
            for i in range(par, n, 2):
                yield from body(i)
        gens = [stream(0), stream(1)]
        for _ in range(lag):
            next(gens[0])
        while gens:
            for g in list(gens):
                try:
                    next(g)
                except StopIteration:
                    gens.remove(g)

    def scr(self, name, shape, dt=F32):
        kind = "ExternalOutput" if name in self.debug else "Internal"
        return self.nc.dram_tensor(name, list(shape), dt, kind=kind).ap()

    def sb(self, es, name, shape, dt=F32):
        self._n += 1
        return es.enter_context(self.nc.sbuf_tensor("%s_%d" % (name, self._n), list(shape), dt))

    def build(self):
        nc, P = self.nc, self.P
        top = self.top
        self.pf = [top.enter_context(nc.psum_tensor("pf%d" % i, [128, 512], F32)) for i in range(6)]
        self.pt = [top.enter_context(nc.psum_tensor("pt%d" % i, [128, 8, 128], BF16)) for i in range(2)]
        self.consts()
        self.phase_ada()
        if self.phases >= 1:
            self.phase_projA()
        if self.phases >= 2:
            self.phase_qkv()
        if self.phases >= 3:
            self.phase_delta_both()
            self.phase_finish()
        if self.phases >= 4:
            self.phase_moe(0, self.X1, self.X2, final=False)
        if self.phases >= 5:
            self.phase_convD()
        if self.phases >= 6:
            self.phase_convE()
        if self.phases >= 7:
            self.phase_moe(1, self.X3, self.out, final=True)
        P.finish()
        top.close()
        return nc

    def consts(self):
        nc, P, top = self.nc, self.P, self.top
        sb = lambda n, s, d=F32: self.sb(top, n, s, d)
        rowi = sb("rowi", [128, 128])
        coli = sb("coli", [128, 128])
        P.op("pool", lambda e: e.iota(rowi[:], pattern=[[0, 128]], base=0, channel_multiplier=1,
                                       allow_small_or_imprecise_dtypes=True), w=[rowi])
        P.op("pool", lambda e: e.iota(coli[:], pattern=[[1, 128]], base=0, channel_multiplier=0,
                                       allow_small_or_imprecise_dtypes=True), w=[coli])
        self.rowi, self.coli = rowi, coli
        self.ident_f = sb("ident_f", [128, 128])
        self.ident_b = sb("ident_b", [128, 128], BF16)
        self.ones_f = sb("ones_f", [128, 128])
        self.ones_b = sb("ones_b", [128, 128], BF16)
        P.tt(self.ident_f[:], rowi[:], coli[:], ALU.is_equal)
        P.copy(self.ident_b[:], self.ident_f[:])
        P.memset(self.ones_f[:], 1.0)
        P.memset(self.ones_b[:], 1.0)
        self.U = [sb("Uf", [128, 128]), sb("Ub", [128, 128])]
        P.tt(self.U[0][:], rowi[:], coli[:], ALU.is_le)
        P.tt(self.U[1][:], rowi[:], coli[:], ALU.is_ge)
        self.SM = [sb("SMf", [128, 128]), sb("SMb", [128, 128])]
        P.tt(self.SM[0][:], rowi[:], coli[:], ALU.is_gt)
        P.tt(self.SM[1][:], rowi[:], coli[:], ALU.is_lt)
        self.BM = [sb("BMf", [128, 4, 128]), sb("BMb", [128, 4, 128])]
        for d in range(2):
            for h in range(4):
                P.ts(self.BM[d][:, h, :], self.SM[1 - d][:], BIG, ALU.mult)
        self.Us_b = sb("Us_b", [128, 128], BF16)
        P.copy(self.Us_b[:], self.SM[1][:])
        bd = {}
        colb = sb("colb", [128, 128]); rowb = sb("rowb", [128, 128])
        for sz in (8, 16, 32, 64):
            P.op("pool", lambda e, sz=sz: e.iota(colb[:], pattern=[[1, 128 // sz], [0, sz]], base=0, channel_multiplier=0,
                                                  allow_small_or_imprecise_dtypes=True), w=[colb])
            P.tr(self.pf[0][:, 0:128], colb[:], self.ident_f[:])
            P.copy(rowb[:], self.pf[0][:, 0:128])
            bd[sz] = sb("bd%d" % sz, [128, 128])
            P.tt(bd[sz][:], rowb[:], colb[:], ALU.is_equal)
        self.BD8 = sb("BD8b", [128, 128], BF16)
        P.copy(self.BD8[:], bd[8][:])
        self.MS = {}
        for sz in (8, 16, 32, 64):
            self.MS[sz] = sb("MS%d" % sz, [128, 128], BF16)
            if sz < 64:
                P.tt(self.MS[sz][:], bd[2 * sz][:], bd[sz][:], ALU.subtract)
            else:
                P.ts(self.MS[sz][:], bd[sz][:], -1.0, ALU.mult, 1.0, ALU.add)
        self.epsc = sb("epsc", [128, 1])
        P.memset(self.epsc[:], EPS)

    def evac(self, out, in_, i):
        if i % 2 == 0:
            return self.P.copy(out, in_, eng="dve")
        return self.P.copy(out, in_, eng="act")

    def load_cast(self, es, name, src_ap, ncols, chunk=512, kparts=8, qeng="sp"):
        P = self.P
        wb = self.sb(es, name, [128, kparts, ncols], BF16)
        with self.scope() as tmp:
            st = [self.sb(tmp, name + "_st", [128, kparts, chunk]) for _ in range(2)]
            src = src_ap.rearrange("(kc p) n -> p kc n", p=128)
            i = 0
            for c0 in range(0, ncols, chunk):
                cw = min(chunk, ncols - c0)
                s = st[i % 2]
                P.dma(qeng, s[:, :, 0:cw], src[:, :, c0:c0 + cw])
                if i % 2 == 0:
                    P.copy(wb[:, :, c0:c0 + cw], s[:, :, 0:cw], eng="dve")
                else:
                    P.copy(wb[:, :, c0:c0 + cw], s[:, :, 0:cw], eng="pool")
                i += 1
        return wb

    def bcast_load(self, es, name, vec_ap, n):
        t = self.sb(es, name, [128, n])
        self.P.dma("sp", t[:], vec_ap.partition_broadcast(128))
        return t

    def modulate(self, xt, A, B, sq, ss, hf, hb):
        P = self.P
        P.act(sq[:], xt[:], AF.Square)
        P.red(ss[:, 0:1], sq[:], ALU.add)
        P.ts(ss[:, 1:2], ss[:, 0:1], 1.0 / D, ALU.mult, EPS, ALU.add)
        P.act(ss[:, 2:3], ss[:, 1:2], AF.Sqrt)
        P.recip(ss[:, 3:4], ss[:, 2:3])
        P.stt(hf[:], xt[:], ss[:, 3:4], A[:], ALU.mult, ALU.mult)
        if hb is not None:
            P.tt(hb[:], hf[:], B[:], ALU.add)
        else:
            P.tt(hf[:], hf[:], B[:], ALU.add)

    def transpose8(self, dstT, src_bf, ptile, i=0, n=8):
        P = self.P
        for kc in range(n):
            P.tr(ptile[:, kc, :], src_bf[:, kc * 128:(kc + 1) * 128], self.ident_b[:])
        self.evac(dstT[:, 0:n, :], ptile[:, 0:n, :], i)

    def phase_ada(self):
        nc, P = self.nc, self.P
        with self.scope() as es:
            sb = lambda n, s, d=F32: self.sb(es, n, s, d)
            craw = sb("craw", [128, 2, 8])
            P.dma("sp", craw[:, 0, :], self.c.rearrange("(kc p) -> p kc", p=128), allow_slow_non_contiguous=True)
            P.dma("sp", craw[:, 1, :], self.c_ctx.rearrange("(kc p) -> p kc", p=128), allow_slow_non_contiguous=True)
            csil = sb("csil", [128, 2, 8])
            P.act(csil[:], craw[:], AF.Silu)
            cb = sb("cb", [128, 2, 8, 128])
            for j in range(2):
                P.copy(cb[:, j, :, :], csil[:, j, :].unsqueeze(2).to_broadcast([128, 8, 128]))
            wst = [sb("adaw", [128, 8, 512]) for _ in range(2)]
            mb = sb("mb", [128, 6 * D])
            bb = sb("bb", [128, 6 * D])
            nm = sb("nm", [128, D])
            res = [sb("res", [128, D]) for _ in range(2)]
            cnt = 0
            for (layer, j) in ((0, 0), (0, 1), (1, 0)):
                P.dma("sp", bb[:], self.ada_b[layer].partition_broadcast(128))
                for n in range(12):
                    w = wst[cnt % 2]
                    P.dma("sp", w[:], self.ada_w[layer].rearrange("(kc p) n -> p kc n", p=128)[:, :, n * 512:(n + 1) * 512])
                    ps = self.pf[cnt % 2]
                    for kc in range(8):
                        P.mm(ps[:], cb[:, j, kc, :], w[:, kc, :], start=(kc == 0), stop=(kc == 7))
                    P.tt(mb[:, n * 512:(n + 1) * 512], ps[:], bb[:, n * 512:(n + 1) * 512], ALU.add)
                    cnt += 1
                outs = []
                for sub, (nrm, base) in enumerate(((self.norm_mix, 0), (self.norm_ffn, 3))):
                    if j == 1 and sub == 1:
                        continue
                    P.dma("sp", nm[:], nrm[layer].partition_broadcast(128))
                    r0 = res[0]
                    P.stt(r0[:], mb[:, (base + 1) * D:(base + 2) * D], 1.0, nm[:], ALU.add, ALU.mult)
                    ka = (12 if j == 1 else 6 * layer + base)
                    P.dma("sp", self.MODP[ka], r0[:], w=[("modp", ka)])
                    P.dma("sp", self.MODP[ka + 1], mb[:, base * D:(base + 1) * D], w=[("modp", ka + 1)])
                    if j == 0:
                        P.dma("sp", self.MODP[ka + 2], mb[:, (base + 2) * D:(base + 3) * D], w=[("modp", ka + 2)])

    def load_mod(self, es, name, k):
        t = self.sb(es, name, [128, D])
        self.P.dma("sp", t[:], self.MODP[k], r=[("modp", k)])
        return t

    def phase_projA(self):
        nc, P = self.nc, self.P
        NCOL = 4 * D + 32
        with self.scope() as es:
            sb = lambda n, s, d=F32: self.sb(es, n, s, d)
            wb = self.load_cast(es, "wina", self.w_in_a, NCOL)
            A = [self.load_mod(es, "A", 0), self.load_mod(es, "Ac", 12)]
            B = [self.load_mod(es, "B", 1), self.load_mod(es, "Bc", 13)]
            xt = [sb("xt", [128, D]) for _ in range(2)]
            sq = sb("sq", [128, D])
            ss = sb("ss", [128, 4])
            hf = sb("hf", [128, D])
            hb = sb("hb", [128, D], BF16)
            hT = [sb("hT", [128, 8, 128], BF16) for _ in range(2)]
            po = [sb("po", [128, NCOL]) for _ in range(2)]
            tiles = [("c", i) for i in range(self.NC)] + [("l", i) for i in range(self.NT)]
            for ti, (kind, i) in enumerate(tiles):
                src = self.ctx if kind == "c" else self.x
                j = 1 if kind == "c" else 0
                row0 = i * 128 + (0 if kind == "c" else self.CTXL)
                x_ = xt[ti % 2]
                P.dma("sp", x_[:], src[i * 128:(i + 1) * 128, :])
                self.modulate(x_, A[j], B[j], sq, ss, hf, hb)
                hT_ = hT[ti % 2]
                self.transpose8(hT_, hb, self.pt[ti % 2], ti)
                po_ = po[ti % 2]
                g = 0
                for c0 in range(0, NCOL, 512):
                    cw = min(512, NCOL - c0)
                    ps = self.pf[g % 4]
                    for kc in range(8):
                        P.mm(ps[:, 0:cw], hT_[:, kc, :], wb[:, kc, c0:c0 + cw], start=(kc == 0), stop=(kc == 7))
                    self.evac(po_[:, c0:c0 + cw], ps[:, 0:cw], g)
                    g += 1
                P.dma("pool", self.P0[row0:row0 + 128, :], po_[:], w=[("p0", row0 // 128)])

    def phase_qkv(self):
        nc, P = self.nc, self.P
        CT = self.CTXL
        with self.scope() as es:
            sb = lambda n, s, dt=F32: self.sb(es, n, s, dt)
            cw = [self.bcast_load(es, "cw%d" % s, self.conv_a[s], 3 * D) for s in range(3)]
            pm_ = [sb("pm", [128, 3 * D]) for _ in range(2)]
            pc_ = [sb("pc", [128, 3 * D]) for _ in range(2)]
            pp_ = [sb("pp", [128, 3 * D]) for _ in range(2)]
            qo_ = [sb("qo", [128, 3 * D], BF16) for _ in range(2)]
            so_ = [sb("so", [128, 3 * D]) for _ in range(2)]
            ssq_ = [sb("ssq", [128, 64]) for _ in range(2)]
            bc = lambda ap: ap.unsqueeze(2).to_broadcast([128, ap.shape[1], HD])
            tiles = [("c", i) for i in range(self.NC)] + [("l", i) for i in range(self.NT)]

            def loads(ti):
                if ti >= len(tiles):
                    return
                kind, i = tiles[ti]
                pm, pc, pp = pm_[ti % 2], pc_[ti % 2], pp_[ti % 2]
                ntile = self.NC if kind == "c" else self.NT
                row0 = (0 if kind == "c" else CT) + i * 128
                P.dma("sp", pc[:], self.P0[row0:row0 + 128, 0:3 * D])
                if i == 0:
                    P.memset(pm[:], 0.0, eng="pool")
                    P.dma("sp", pm[1:128, :], self.P0[row0:row0 + 127, 0:3 * D], w=[pm])
                else:
                    P.dma("sp", pm[:], self.P0[row0 - 1:row0 + 127, 0:3 * D])
                if i == ntile - 1:
                    P.memset(pp[:], 0.0, eng="pool")
                    P.dma("sp", pp[0:127, :], self.P0[row0 + 1:row0 + 128, 0:3 * D], w=[pp])
                else:
                    P.dma("sp", pp[:], self.P0[row0 + 1:row0 + 129, 0:3 * D])

            loads(0)
            loads(1)

            def qbody(ti):
                kind, i = tiles[ti]
                pm, pc, pp, qo, ssq = pm_[ti % 2], pc_[ti % 2], pp_[ti % 2], qo_[ti % 2], ssq_[ti % 2]
                so = so_[ti % 2]
                row0 = (0 if kind == "c" else CT) + i * 128
                P.tt(pm[:], pm[:], cw[0][:], ALU.mult)
                P.tt(pc[:], pc[:], cw[1][:], ALU.mult)
                P.tt(pp[:], pp[:], cw[2][:], ALU.mult)
                yield
                for c in range(6):
                    ps = self.pf[c]
                    cs = slice(c * 512, (c + 1) * 512)
                    P.mm(ps[:], self.ident_f[:], pm[:, cs], start=True, stop=False)
                    P.mm(ps[:], self.ident_f[:], pc[:, cs], start=False, stop=False)
                    P.mm(ps[:], self.ident_f[:], pp[:, cs], start=False, stop=True)
                    P.act(so[:, cs], ps[:], AF.Silu)
                    if c % 2 == 1:
                        yield
                P.act(pm[:, 0:2 * D], so[:, 0:2 * D], AF.Square)
                yield
                P.red(ssq[:, 0:16], pm[:, 0:2 * D].rearrange("p (h e) -> p h e", e=HD), ALU.add)
                P.ts(ssq[:, 16:32], ssq[:, 0:16], EPS, ALU.add)
                P.act(ssq[:, 32:48], ssq[:, 16:32], AF.Sqrt)
                P.recip(ssq[:, 48:64], ssq[:, 32:48])
                P.ts(ssq[:, 48:56], ssq[:, 48:56], HD ** -0.5, ALU.mult)
                P.tt(qo[:, 0:D].rearrange("p (h e) -> p h e", e=HD), so[:, 0:D].rearrange("p (h e) -> p h e", e=HD),
                     bc(ssq[:, 48:56]), ALU.mult)
                P.tt(qo[:, D:2 * D].rearrange("p (h e) -> p h e", e=HD), so[:, D:2 * D].rearrange("p (h e) -> p h e", e=HD),
                     bc(ssq[:, 56:64]), ALU.mult, eng="pool")
                P.copy(qo[:, 2 * D:3 * D], so[:, 2 * D:3 * D], eng="act")
                yield
                loads(ti + 2)
                P.dma("pool", self.QKV[row0:row0 + 128, :], qo[:])
                yield
            self.run2(len(tiles), qbody)

    def phase_delta_both(self):
        with self.scope() as es:
            gens = [self.delta_gen(es, 0), self.delta_gen(es, 1)]
            while gens:
                for g in list(gens):
                    try:
                        next(g)
                    except StopIteration:
                        gens.remove(g)

    def delta_gen(self, es, d):
        nc, P = self.nc, self.P
        CT = self.CTXL
        sb = lambda n, s, dt=F32: self.sb(es, n, s, dt)
        nA = self.bcast_load(es, "nA", self.a_log[d], NH)
        P.act(nA[:], nA[:], AF.Exp)
        P.ts(nA[:], nA[:], -1.0, ALU.mult)
        dtb = self.bcast_load(es, "dtb", self.dt_bias[d], NH)
        qv = sb("qv", [128, 3 * D], BF16)
        gts = sb("gts", [128, 32])
        sml = sb("sml", [128, 16 * NH])
        kT = sb("kT", [128, NH, HD], BF16)
        qT = sb("qT", [128, NH, HD], BF16)
        qd_b = sb("qd_b", [128, NH, HD], BF16)
        qdT = sb("qdT", [128, NH, HD], BF16)
        vb = sb("vb", [128, NH, HD], BF16)
        kbe = sb("kbe", [128, NH, HD], BF16)
        kdec = sb("kdec", [128, NH, HD], BF16)
        rhsR = sb("rhsR", [128, NH, HD])
        Dm = sb("Dm", [128, NH, HD])
        As = sb("As", [128, NH, HD], BF16)
        qk_b = sb("qk_b", [128, NH, HD], BF16)
        qkT = sb("qkT", [128, NH, HD], BF16)
        L_b = sb("L_b", [128, NH, HD], BF16); N_b = sb("N_b", [128, NH, HD], BF16)
        Ld = sb("Ld", [128, NH, HD], BF16); Nd = sb("Nd", [128, NH, HD], BF16)
        P1 = sb("P1", [128, NH, HD], BF16); Q1 = sb("Q1", [128, NH, HD], BF16)
        P2 = sb("P2", [128, NH, HD], BF16); Q2 = sb("Q2", [128, NH, HD], BF16)
        Tw = [sb("Tw", [128, NH, HD], BF16) for _ in range(2)]
        Xw = [sb("Xw", [128, NH, HD], BF16) for _ in range(2)]
        u32 = sb("u32", [128, NH, HD])
        wT = sb("wT", [128, NH, HD], BF16)
        vnew = sb("vnew", [128, NH, HD], BF16)
        S32 = sb("S32", [128, NH, HD])
        Sb = sb("Sb", [128, NH, HD], BF16)
        Stmp = sb("Stmp", [128, NH, HD])
        o32 = sb("o32", [128, NH, HD])
        P.memset(S32[:], 0.0)
        P.memset(Sb[:], 0.0, eng="pool")
        OUT = self.OF if d == 0 else self.OB
        ptd = self.pt[d]
        if d == 0:
            order = [("c", i) for i in range(self.NC)] + [("l", i) for i in range(self.NT)]
        else:
            order = [("c", i) for i in reversed(range(self.NC))] + [("l", i) for i in reversed(range(self.NT))]
        bc = lambda ap: ap.unsqueeze(2).to_broadcast([128, ap.shape[1], HD])
        mb = lambda m: m[:].unsqueeze(1).to_broadcast([128, NH, HD])
        v4 = lambda ps: ps[:].rearrange("p (h e) -> p h e", e=HD)
        pfi = self.pfi

        def nextpf():
            pfi[0] += 1
            return self.pf[pfi[0] % 6]

        def mm4(lhs, rhs, hg):
            ps = nextpf()
            for hh in range(4):
                h = hg * 4 + hh
                P.mm(ps[:, hh * 128:(hh + 1) * 128], lhs[:, h, :], rhs[:, h, :])
            return ps

        for ti, (kind, i) in enumerate(order):
            row0 = (0 if kind == "c" else CT) + i * 128
            P.dma("sp", qv[:], self.QKV[row0:row0 + 128, :])
            P.dma("sp", gts[:], self.P0[row0:row0 + 128, 4 * D:4 * D + 32])
            qn_b = qv[:, 0:D].rearrange("p (h e) -> p h e", e=HD)
            kn_b = qv[:, D:2 * D].rearrange("p (h e) -> p h e", e=HD)
            v_b = qv[:, 2 * D:3 * D].rearrange("p (h e) -> p h e", e=HD)
            yield
            beta = sml[:, 0:8]
            P.act(beta, gts[:, d * 8:d * 8 + 8], AF.Sigmoid)
            xg = sml[:, 8:16]
            P.tt(xg, gts[:, 16 + d * 8:16 + d * 8 + 8], dtb[:], ALU.add)
            P.stt(sml[:, 16:24], xg, -1.0, xg, ALU.mult, ALU.max)
            P.act(sml[:, 24:32], sml[:, 16:24], AF.Exp, scale=-1.0)
            P.act(sml[:, 32:40], sml[:, 24:32], AF.Ln, bias=1.0)
            P.stt(sml[:, 40:48], xg, 0.0, sml[:, 32:40], ALU.max, ALU.add)
            g = sml[:, 48:56]
            P.tt(g, sml[:, 40:48], nA[:], ALU.mult)
            yield
            ps = nextpf()
            P.mm(ps[:, 0:8], self.U[d][:], g)
            P.mm(ps[:, 8:16], self.ones_f[:], g)
            gc = sml[:, 56:64]
            gl = sml[:, 64:72]
            P.copy(sml[:, 56:72], ps[:, 0:16])
            for (src, dst) in ((kn_b, kT), (qn_b, qT)):
                for h in range(NH):
                    P.tr(ptd[:, h, :], src[:, h, :], self.ident_b[:])
                self.evac(dst[:], ptd[:], d)
                yield
            egc = sml[:, 72:80]
            P.act(egc, gc, AF.Exp)
            egl = sml[:, 80:88]
            P.act(egl, gl, AF.Exp)
            kdc = sml[:, 88:96]
            P.tt(kdc, gl, gc, ALU.subtract)
            P.act(kdc, kdc, AF.Exp)
            bq = sml[:, 96:104]
            P.tt(bq, beta, egc, ALU.mult)
            yield
            P.tt(vb[:], v_b, bc(beta), ALU.mult)
            P.tt(kbe[:], kn_b, bc(bq), ALU.mult, eng="pool")
            P.tt(kdec[:], kn_b, bc(kdc), ALU.mult, eng="pool")
            P.tt(qd_b[:], qn_b, bc(egc), ALU.mult)
            P.tt(rhsR[:], self.U[d][:].unsqueeze(1).to_broadcast([128, NH, HD]), bc(g), ALU.mult, eng="pool")
            yield
            for h in range(NH):
                P.tr(ptd[:, h, :], qd_b[:, h, :], self.ident_b[:])
            self.evac(qdT[:], ptd[:], d + 1)
            yield
            for hg in range(2):
                ps = nextpf()
                P.mm(ps[:], self.ones_f[:], rhsR[:, hg * 4:(hg + 1) * 4, :].rearrange("p h e -> p (h e)"), start=True, stop=False)
                P.mm(ps[:], self.ident_f[:], self.BM[d][:].rearrange("p h e -> p (h e)"), start=False, stop=True)
                for hh in range(4):
                    h = hg * 4 + hh
                    P.act(Dm[:, h, :], ps[:, hh * 128:(hh + 1) * 128], AF.Exp, bias=gc[:, h:h + 1], scale=-1.0)
                yield
            for hg in range(2):
                hs = slice(hg * 4, (hg + 1) * 4)
                P.tt(As[:, hs, :], v4(mm4(kT, kT, hg)), self.SM[d][:].unsqueeze(1).to_broadcast([128, 4, HD]), ALU.mult)
                P.tt(qk_b[:, hs, :], v4(mm4(qT, kT, hg)), Dm[:, hs, :], ALU.mult)
                yield
            for h in range(NH):
                P.stt(L_b[:, h, :], As[:, h, :], beta[:, h:h + 1], Dm[:, h, :], ALU.mult, ALU.mult)
            yield
            for (src, dst) in ((L_b, N_b), (qk_b, qkT)):
                for h in range(NH):
                    P.tr(ptd[:, h, :], src[:, h, :], self.ident_b[:])
                self.evac(dst[:], ptd[:], d)
                yield
            P.tt(Ld[:], L_b[:], mb(self.BD8), ALU.mult)
            P.tt(Nd[:], N_b[:], mb(self.BD8), ALU.mult, eng="pool")
            P.tt(Tw[0][:], mb(self.ident_b), Ld[:], ALU.subtract)
            P.tt(Xw[0][:], mb(self.ident_b), Nd[:], ALU.subtract, eng="pool")
            yield
            cur = 0
            for (Pa, Qa, Pprev, Qprev) in ((P1, Q1, Ld, Nd), (P2, Q2, P1, Q1)):
                for hg in range(2):
                    hs = slice(hg * 4, (hg + 1) * 4)
                    self.evac(Pa[:, hs, :], v4(mm4(Qprev, Pprev, hg)), 1)
                    self.evac(Qa[:, hs, :], v4(mm4(Pprev, Qprev, hg)), 1)
                yield
                nxt = 1 - cur
                for hg in range(2):
                    hs = slice(hg * 4, (hg + 1) * 4)
                    P.tt(Tw[nxt][:, hs, :], Tw[cur][:, hs, :], v4(mm4(Qa, Tw[cur], hg)), ALU.add)
                    P.tt(Xw[nxt][:, hs, :], Xw[cur][:, hs, :], v4(mm4(Pa, Xw[cur], hg)), ALU.add)
                yield
                cur = nxt
            Cs, CTs, Zb, Zpb = Ld, Nd, P1, Q1
            for sz in (8, 16, 32, 64):
                P.tt(Cs[:], L_b[:], mb(self.MS[sz]), ALU.mult)
                P.tt(CTs[:], N_b[:], mb(self.MS[sz]), ALU.mult, eng="pool")
                for hg in range(2):
                    hs = slice(hg * 4, (hg + 1) * 4)
                    self.evac(Zb[:, hs, :], v4(mm4(Cs, Xw[cur], hg)), 1)
                    if sz < 64:
                        self.evac(Zpb[:, hs, :], v4(mm4(CTs, Tw[cur], hg)), 1)
                yield
                nxt = 1 - cur
                for hg in range(2):
                    hs = slice(hg * 4, (hg + 1) * 4)
                    P.tt(Xw[nxt][:, hs, :], Xw[cur][:, hs, :], v4(mm4(Tw[cur], Zb, hg)), ALU.subtract)
                    if sz < 64:
                        P.tt(Tw[nxt][:, hs, :], Tw[cur][:, hs, :], v4(mm4(Xw[cur], Zpb, hg)), ALU.subtract)
                yield
                cur = nxt
            TTm = Xw[cur]
            for hg in range(2):
                hs = slice(hg * 4, (hg + 1) * 4)
                self.evac(u32[:, hs, :], v4(mm4(TTm, vb, hg)), hg)
                self.evac(wT[:, hs, :], v4(mm4(kbe, TTm, hg)), hg + 1)
            yield
            for hg in range(2):
                hs = slice(hg * 4, (hg + 1) * 4)
                P.tt(vnew[:, hs, :], u32[:, hs, :], v4(mm4(wT, Sb, hg)), ALU.subtract)
                yield
                if kind == "l":
                    ps2 = nextpf()
                    for hh in range(4):
                        h = hg * 4 + hh
                        o_ = ps2[:, hh * 128:(hh + 1) * 128]
                        P.mm(o_, qdT[:, h, :], Sb[:, h, :], start=True, stop=False)
                        P.mm(o_, qkT[:, h, :], vnew[:, h, :], start=False, stop=True)
                    self.evac(o32[:, hs, :], v4(ps2), hg)
                ps3 = mm4(kdec, vnew, hg)
                P.tt(Stmp[:, hs, :], S32[:, hs, :], bc(egl[:, hs]), ALU.mult, eng="pool")
                P.tt(S32[:, hs, :], Stmp[:, hs, :], v4(ps3), ALU.add)
                P.copy(Sb[:, hs, :], S32[:, hs, :], eng="act")
                yield
            if kind == "l":
                P.dma("pool", OUT[i * 128:(i + 1) * 128, :], o32[:].rearrange("p h e -> p (h e)"))
            yield

    def phase_finish(self):
        nc, P = self.nc, self.P
        CT = self.CTXL
        with self.scope() as es:
            sb = lambda n, s, dt=F32: self.sb(es, n, s, dt)
            onw = self.bcast_load(es, "onw", self.onorm, HD)
            wo = self.load_cast(es, "wouta", self.w_out_a, D)
            Gm = self.load_mod(es, "Gm", 2)
            of_ = [sb("of_", [128, NH, HD]) for _ in range(2)]
            ob_ = [sb("ob_", [128, NH, HD]) for _ in range(2)]
            zt_ = [sb("zt", [128, D]) for _ in range(2)]
            xt_ = [sb("xt", [128, D]) for _ in range(2)]
            osq_ = [sb("osq", [128, NH, HD]) for _ in range(2)]
            orn_ = [sb("orn", [128, 4 * NH]) for _ in range(2)]
            yg_ = [sb("yg", [128, D], BF16) for _ in range(2)]
            ygT = [sb("ygT", [128, 8, 128], BF16) for _ in range(2)]
            x1t = [sb("x1t", [128, D]) for _ in range(2)]
            bc = lambda ap: ap.unsqueeze(2).to_broadcast([128, ap.shape[1], HD])

            def loads(i):
                if i >= self.NT:
                    return
                sl = i % 2
                P.dma("sp", of_[sl][:].rearrange("p h e -> p (h e)"), self.OF[i * 128:(i + 1) * 128, :])
                P.dma("sp", ob_[sl][:].rearrange("p h e -> p (h e)"), self.OB[i * 128:(i + 1) * 128, :])
                P.dma("sp", zt_[sl][:], self.P0[CT + i * 128:CT + (i + 1) * 128, 3 * D:4 * D])
                P.dma("sp", xt_[sl][:], self.x[i * 128:(i + 1) * 128, :])

            loads(0)
            loads(1)

            def fbody(i):
                sl = i % 2
                o32, ob, zt, xt = of_[sl], ob_[sl], zt_[sl], xt_[sl]
                osq, orn, yg = osq_[sl], orn_[sl], yg_[sl]
                P.tt(o32[:], o32[:], ob[:], ALU.add)
                yield
                P.act(osq[:], o32[:], AF.Square)
                P.red(orn[:, 0:8], osq[:], ALU.add)
                P.ts(orn[:, 8:16], orn[:, 0:8], 1.0 / HD, ALU.mult, EPS, ALU.add)
                P.act(orn[:, 16:24], orn[:, 8:16], AF.Sqrt)
                P.recip(orn[:, 24:32], orn[:, 16:24])
                yield
                P.tt(osq[:], o32[:], bc(orn[:, 24:32]), ALU.mult)
                P.tt(osq[:], osq[:], onw[:].unsqueeze(1).to_broadcast([128, NH, HD]), ALU.mult)
                P.act(zt[:], zt[:], AF.Silu)
                P.tt(yg[:], osq[:].rearrange("p h e -> p (h e)"), zt[:], ALU.mult)
                yield
                self.transpose8(ygT[sl], yg, self.pt[sl], i)
                yield
                x1 = x1t[sl]
                for half in range(2):
                    ps = self.pf[(2 * i + half) % 6]
                    for kc in range(8):
                        P.mm(ps[:], ygT[sl][:, kc, :], wo[:, kc, half * 512:(half + 1) * 512], start=(kc == 0), stop=(kc == 7))
                    cs = slice(half * 512, (half + 1) * 512)
                    P.tt(x1[:, cs], ps[:], Gm[:, cs], ALU.mult)
                    P.tt(x1[:, cs], x1[:, cs], xt[:, cs], ALU.add)
                loads(i + 2)
                yield
                P.dma("pool", self.X1[i * 128:(i + 1) * 128, :], x1[:], w=[("x1", i)])
                yield
            self.run2(self.NT, fbody)

    def phase_moe(self, layer, XIN, XOUT, final):
        nc, P = self.nc, self.P
        NT, NB = self.NT, self.NBLK
        xin_key = "x1" if layer == 0 else "x3"
        with self.scope() as es:
            sb = lambda n, s, dt=F32: self.sb(es, n, s, dt)
            A = self.load_mod(es, "Af", 6 * layer + 3)
            B = self.load_mod(es, "Bf", 6 * layer + 4)
            G = self.load_mod(es, "Gf", 6 * layer + 5)
            rt = sb("rt", [128, 8, 36])
            P.dma("sp", rt[:], self.router[layer].rearrange("(kc p) n -> p kc n", p=128))
            E1 = sb("E1", [128, NT]); E2 = sb("E2", [128, NT])
            R1 = sb("R1", [128, NT]); R2 = sb("R2", [128, NT])
            W1 = sb("W1", [128, NT]); W2 = sb("W2", [128, NT])
            carry = sb("carry", [128, NE])
            P.memset(carry[:], 0.0)
            iotaE = sb("iotaE", [128, NE])
            P.copy(iotaE[:], self.coli[:, 0:NE])
            with self.scope() as e1:
                s1 = lambda n, s, dt=F32: self.sb(e1, n, s, dt)
                xt = [s1("xt", [128, D]) for _ in range(2)]
                bufs = []
                for _ in range(2):
                    bufs.append(dict(
                        sq=s1("sq", [128, D]), ss=s1("ss", [128, 4]), hf=s1("hf", [128, D]), hb=s1("hb", [128, D], BF16),
                        hT=s1("hT", [128, 8, 128]), lg=s1("lg", [128, 36]), sm=s1("sm", [128, 64]),
                        lem=s1("lem", [128, NE]), top8=s1("top8", [128, 8]), m1=s1("m1", [128, NE]),
                        m12=s1("m12", [128, NE]), m2=s1("m2", [128, NE]), m12b=s1("m12b", [128, NE], BF16),
                        pos=s1("pos", [128, NE]), tmp=s1("tmp", [128, NE])))

                def rt_body(i):
                    bb = bufs[i % 2]
                    sq, ss, hf, hb, hT, lg, sm = bb["sq"], bb["ss"], bb["hf"], bb["hb"], bb["hT"], bb["lg"], bb["sm"]
                    lem, top8, m1, m12, m2, m12b, pos, tmp = bb["lem"], bb["top8"], bb["m1"], bb["m12"], bb["m2"], bb["m12b"], bb["pos"], bb["tmp"]
                    x_ = xt[i % 2]
                    P.dma("sp", x_[:], XIN[i * 128:(i + 1) * 128, :], r=[(xin_key, i)])
                    self.modulate(x_, A, B, sq, ss, hf, None)
                    P.copy(hb[:], hf[:], eng="act")
                    P.dma("pool", self.HF[i * 128:(i + 1) * 128, :], hb[:], w=[("hf", i)])
                    yield
                    for half in range(2):
                        ps = self.pf[half]
                        for kk in range(4):
                            kc = half * 4 + kk
                            P.tr(ps[:, kk * 128:(kk + 1) * 128], hf[:, kc * 128:(kc + 1) * 128], self.ident_f[:])
                        self.evac(hT[:, half * 4:(half + 1) * 4, :], ps[:].rearrange("p (k e) -> p k e", e=128), half)
                    yield
                    ps = self.pf[2 + 2 * (i % 2)]
                    for kc in range(8):
                        P.mm(ps[:, 0:36], hT[:, kc, :], rt[:, kc, :], start=(kc == 0), stop=(kc == 7))
                    P.copy(lg[:], ps[:, 0:36])
                    yield
                    P.red(sm[:, 0:1], lg[:, 0:4], ALU.max)
                    P.ts(sm[:, 1:2], sm[:, 0:1], -1.0, ALU.mult)
                    P.act(sm[:, 4:8], lg[:, 0:4], AF.Exp, bias=sm[:, 1:2])
                    P.red(sm[:, 2:3], sm[:, 4:8], ALU.add)
                    P.recip(sm[:, 3:4], sm[:, 2:3])
                    P.ts(sm[:, 8:12], lg[:, 0:4], sm[:, 0:1], ALU.is_ge)
                    P.ts(sm[:, 12:16], sm[:, 8:12], BIG, ALU.mult, -BIG, ALU.add)
                    P.tt(lem[:].rearrange("p (g e) -> p g e", e=8), lg[:, 4:36].rearrange("p (g e) -> p g e", e=8),
                         sm[:, 12:16].unsqueeze(2).to_broadcast([128, 4, 8]), ALU.add)
                    yield
                    P.op("dve", lambda e, o=top8, i_=lem: e.max(out=o[:], in_=i_[:]), r=[lem], w=[top8])
                    P.ts(m1[:], lem[:], top8[:, 0:1], ALU.is_ge)
                    P.ts(m12[:], lem[:], top8[:, 1:2], ALU.is_ge)
                    P.tt(m2[:], m12[:], m1[:], ALU.subtract)
                    P.copy(m12b[:], m12[:], eng="act")
                    yield
                    P.tt(sm[:, 16:17], top8[:, 0:1], top8[:, 1:2], ALU.subtract)
                    P.act(sm[:, 17:18], sm[:, 16:17], AF.Sigmoid)
                    P.tt(W1[:, i:i + 1], sm[:, 17:18], sm[:, 3:4], ALU.mult)
                    P.tt(W2[:, i:i + 1], sm[:, 3:4], W1[:, i:i + 1], ALU.subtract)
                    yield
                    ps = self.pf[3 + 2 * (i % 2)]
                    P.mm(ps[:, 0:NE], self.Us_b[:], m12b[:])
                    P.mm(ps[:, NE:2 * NE], self.ones_b[:], m12b[:])
                    P.tt(pos[:], ps[:, 0:NE], carry[:], ALU.add)
                    P.tt(carry[:], carry[:], ps[:, NE:2 * NE], ALU.add)
                    for (mk, Ed, Rd) in ((m1, E1, R1), (m2, E2, R2)):
                        P.tt(tmp[:], mk[:], iotaE[:], ALU.mult)
                        P.red(Ed[:, i:i + 1], tmp[:], ALU.add)
                        P.tt(tmp[:], mk[:], pos[:], ALU.mult)
                        P.red(Rd[:, i:i + 1], tmp[:], ALU.add)
                    yield
                self.run2(NT, rt_body)
            D1i = sb("D1i", [128, NT], I32); D2i = sb("D2i", [128, NT], I32)
            WIi = sb("WIi", [128, NB], I32)
            with self.scope() as e2:
                s2 = lambda n, s, dt=F32: self.sb(e2, n, s, dt)
                NTH = 2 * NT + 1
                thr = s2("thr", [128, NTH])
                P.op("pool", lambda e: e.iota(thr[:], pattern=[[128, NTH]], base=0, channel_multiplier=0,
                                               allow_small_or_imprecise_dtypes=True), w=[thr])
                cmp = s2("cmp", [128, NE, NTH])
                P.tt(cmp[:], carry[:].unsqueeze(2).to_broadcast([128, NE, NTH]),
                     thr[:].unsqueeze(1).to_broadcast([128, NE, NTH]), ALU.is_gt)
                padded = s2("padded", [128, NE])
                P.red(padded[:], cmp[:], ALU.add)
                P.ts(padded[:], padded[:], 128.0, ALU.mult)
                le = s2("le", [128, NE, NE])
                P.tt(le[:], self.coli[:, 0:NE].unsqueeze(2).to_broadcast([128, NE, NE]),
                     self.coli[:, 0:NE].unsqueeze(1).to_broadcast([128, NE, NE]), ALU.is_ge)
                P.tt(le[:], le[:], padded[:].unsqueeze(1).to_broadcast([128, NE, NE]), ALU.mult)
                ends = s2("ends", [128, NE]); starts = s2("starts", [128, NE])
                P.red(ends[:], le[:], ALU.add)
                P.tt(starts[:], ends[:], padded[:], ALU.subtract)
                oh = s2("oh", [128, NT, NE])
                dd = s2("dd", [128, NT])
                for (Ed, Rd, Di) in ((E1, R1, D1i), (E2, R2, D2i)):
                    P.tt(oh[:], Ed[:].unsqueeze(2).to_broadcast([128, NT, NE]),
                         iotaE[:].unsqueeze(1).to_broadcast([128, NT, NE]), ALU.is_equal)
                    P.tt(oh[:], oh[:], starts[:].unsqueeze(1).to_broadcast([128, NT, NE]), ALU.mult)
                    P.red(dd[:], oh[:], ALU.add)
                    P.tt(dd[:], dd[:], Rd[:], ALU.add)
                    P.copy(Di[:], dd[:])
                bthr = s2("bthr", [128, NB])
                P.op("pool", lambda e: e.iota(bthr[:], pattern=[[128, NB]], base=0, channel_multiplier=0,
                                               allow_small_or_imprecise_dtypes=True), w=[bthr])
                cb = s2("cb", [128, NB, NE])
                P.tt(cb[:], ends[:].unsqueeze(1).to_broadcast([128, NB, NE]),
                     bthr[:].unsqueeze(2).to_broadcast([128, NB, NE]), ALU.is_le)
                be = s2("be", [128, NB + 2])
                P.memset(be[:, 0:2], -1.0)
                P.red(be[:, 2:NB + 2], cb[:], ALU.add)
                P.ts(be[:, 2:NB + 2], be[:, 2:NB + 2], float(NE - 1), ALU.min)
                need = s2("need", [128, NB])
                P.tt(need[:], be[:, 2:NB + 2], be[:, 1:NB + 1], ALU.not_equal)
                P.memset(need[:, NB // 2:NB // 2 + 1], 1.0)
                wi = s2("wi", [128, NB])
                P.ts(wi[:], be[:, 2:NB + 2], 128.0, ALU.mult, self.rowi[:, 0:1], ALU.add)
                P.ts(wi[:], wi[:], -1.0e6, ALU.add)
                P.tt(wi[:], wi[:], need[:], ALU.mult)
                P.ts(wi[:], wi[:], 1.0e6, ALU.add)
                P.copy(WIi[:], wi[:])
            with self.scope() as e3:
                s3 = lambda n, s, dt=F32: self.sb(e3, n, s, dt)
                hbt = [s3("hbt", [128, D], BF16) for _ in range(2)]
                for i in range(NT):
                    h_ = hbt[i % 2]
                    P.dma("sp", h_[:], self.HF[i * 128:(i + 1) * 128, :], r=[("hf", i)])
                    for Di in (D1i, D2i):
                        def sc(e, Di=Di, h_=h_, i=i):
                            if "bx" not in self.regcache:
                                self.regcache["bx"] = e.to_reg(self.NBLK * 128 - 1)
                            return e.indirect_dma_start(
                                out=self.XB[:, :], out_offset=bass.IndirectOffsetOnAxis(ap=Di[:, i:i + 1], axis=0),
                                in_=h_[:, :], in_offset=None, bounds_check=self.regcache["bx"], oob_is_err=False)
                        P.op("pool", sc, r=[h_, Di], w=[], dma=True)
            with self.scope() as e4:
                s4 = lambda n, s, dt=F32: self.sb(e4, n, s, dt)
                stg = [s4("stg", [128, 3 * 4096]) for _ in range(2)]
                w1b_ = [s4("w1b", [128, 8, DE], BF16) for _ in range(2)]
                w3b_ = [s4("w3b", [128, 8, DE], BF16) for _ in range(2)]
                w2b_ = [s4("w2b", [128, 4, D], BF16) for _ in range(2)]
                xbt = [s4("xbt", [128, D], BF16) for _ in range(2)]
                xT_ = [s4("xT", [128, 8, 128], BF16) for _ in range(2)]
                h1_ = [s4("h1", [128, DE]) for _ in range(2)]
                gb_ = [s4("gb", [128, DE], BF16) for _ in range(2)]
                gT_ = [s4("gT", [128, 4, 128], BF16) for _ in range(2)]
                yo = [s4("yo", [128, D]) for _ in range(2)]
                HB = NB // 2
                lane = lambda b: 0 if b < HB else 1

                def gather(b, ln):
                    if b >= NB or lane(b) != ln:
                        return
                    sl = ln

                    def gw(e, b=b, sl=sl):
                        if "bc" not in self.regcache:
                            self.regcache["bc"] = e.to_reg(NE * 128 - 1)
                        return e.indirect_dma_start(
                            out=stg[sl][:, :], out_offset=None, in_=self.wall[layer][:, :],
                            in_offset=bass.IndirectOffsetOnAxis(ap=WIi[:, b:b + 1], axis=0),
                            bounds_check=self.regcache["bc"], oob_is_err=False)
                    P.op("pool", gw, r=[WIi], w=[stg[sl]], dma=True)

                def xload(b, ln):
                    if b >= NB or lane(b) != ln:
                        return
                    P.dma("sp", xbt[ln][:], self.XB[b * 128:(b + 1) * 128, :], r=[], w=[xbt[ln]])

                gather(0, 0)
                gather(HB, 1)
                xload(0, 0)
                xload(HB, 1)
                def blk(b):
                    sl = lane(b)
                    w1b, w3b, w2b, xT, h1, gb, gT = w1b_[sl], w3b_[sl], w2b_[sl], xT_[sl], h1_[sl], gb_[sl], gT_[sl]
                    P.copy(w1b[:].rearrange("p k n -> p (k n)"), stg[sl][:, 0:4096], eng="dve")
                    P.copy(w3b[:].rearrange("p k n -> p (k n)"), stg[sl][:, 4096:8192], eng="act")
                    P.copy(w2b[:, 0:2, :].rearrange("p k n -> p (k n)"), stg[sl][:, 8192:10240], eng="act")
                    P.copy(w2b[:, 2:4, :].rearrange("p k n -> p (k n)"), stg[sl][:, 10240:12288], eng="dve")
                    gather(b + 1, sl)
                    yield
                    self.transpose8(xT, xbt[sl], self.pt[sl], b)
                    xload(b + 1, sl)
                    yield
                    p1, p3 = self.pf[2 * sl], self.pf[2 * sl + 1]
                    for kc in range(8):
                        P.mm(p1[:], xT[:, kc, :], w1b[:, kc, :], start=(kc == 0), stop=(kc == 7))
                    for kc in range(8):
                        P.mm(p3[:], xT[:, kc, :], w3b[:, kc, :], start=(kc == 0), stop=(kc == 7))
                    yield
                    P.act(h1[:], p1[:], AF.Silu)
                    P.tt(gb[:], h1[:], p3[:], ALU.mult)
                    yield
                    pt_ = self.pt[1 - sl]
                    for fc in range(4):
                        P.tr(pt_[:, fc, :], gb[:, fc * 128:(fc + 1) * 128], self.ident_b[:])
                    self.evac(gT[:], pt_[:, 0:4, :], b + 1)
                    yield
                    y_ = yo[sl]
                    for half in range(2):
                        ps = self.pf[4 + half]
                        for fc in range(4):
                            P.mm(ps[:], gT[:, fc, :], w2b[:, fc, half * 512:(half + 1) * 512], start=(fc == 0), stop=(fc == 3))
                        self.evac(y_[:, half * 512:(half + 1) * 512], ps[:], half)
                    yield
                    P.dma("pool", self.YB[b * 128:(b + 1) * 128, :], y_[:], w=["yb_all"])
                    yield
                def lane_stream(ln):
                    for b in (range(0, HB) if ln == 0 else range(HB, NB)):
                        yield from blk(b)
                gens = [lane_stream(0), lane_stream(1)]
                while gens:
                    for g_ in list(gens):
                        try:
                            next(g_)
                        except StopIteration:
                            gens.remove(g_)
            with self.scope() as e5:
                s5 = lambda n, s, dt=F32: self.sb(e5, n, s, dt)
                y1 = [s5("y1", [128, D]) for _ in range(2)]
                y2 = [s5("y2", [128, D]) for _ in range(2)]
                xt = [s5("xt", [128, D]) for _ in range(2)]
                acc_ = [s5("acc", [128, D]) for _ in range(2)]
                xo_ = [s5("xo", [128, D]) for _ in range(2)]
                if final:
                    fn = self.bcast_load(e5, "fn", self.final_norm, D)
                    sq_ = [s5("sq", [128, D]) for _ in range(2)]; ss_ = [s5("ss", [128, 4]) for _ in range(2)]
                def cload(i):
                    if i >= NT:
                        return
                    sl = i % 2
                    for (yy, Di) in ((y1, D1i), (y2, D2i)):
                        def gy(e, yy=yy, Di=Di, i=i, sl=sl):
                            if "bx" not in self.regcache:
                                self.regcache["bx"] = e.to_reg(self.NBLK * 128 - 1)
                            return e.indirect_dma_start(
                                out=yy[sl][:, :], out_offset=None, in_=self.YB[:, :],
                                in_offset=bass.IndirectOffsetOnAxis(ap=Di[:, i:i + 1], axis=0),
                                bounds_check=self.regcache["bx"], oob_is_err=False)
                        P.op("pool", gy, r=[Di], w=[yy[sl]], dma=True)
                    P.dma("sp", xt[sl][:], XIN[i * 128:(i + 1) * 128, :], r=[(xin_key, i)])

                cload(0)
                cload(1)

                def cmb(i):
                    sl = i % 2
                    acc = acc_[sl]
                    if final:
                        sq, ss = sq_[sl], ss_[sl]
                    P.ts(acc[:], y1[sl][:], W1[:, i:i + 1], ALU.mult)
                    P.stt(acc[:], y2[sl][:], W2[:, i:i + 1], acc[:], ALU.mult, ALU.add)
                    P.tt(acc[:], acc[:], G[:], ALU.mult)
                    yield
                    xo = xo_[sl]
                    P.tt(xo[:], acc[:], xt[sl][:], ALU.add)
                    cload(i + 2)
                    yield
                    if not final:
                        P.dma("act", XOUT[i * 128:(i + 1) * 128, :], xo[:], w=[("x2", i)])
                    else:
                        P.act(sq[:], xo[:], AF.Square)
                        P.red(ss[:, 0:1], sq[:], ALU.add)
                        P.ts(ss[:, 1:2], ss[:, 0:1], 1.0 / D, ALU.mult, EPS, ALU.add)
                        P.act(ss[:, 2:3], ss[:, 1:2], AF.Sqrt)
                        P.recip(ss[:, 3:4], ss[:, 2:3])
                        P.stt(sq[:], xo[:], ss[:, 3:4], fn[:], ALU.mult, ALU.mult)
                        P.dma("act", XOUT[i * 128:(i + 1) * 128, :], sq[:], w=[("out", i)])
                    yield
                self.run2(NT, cmb)

    def phase_convD(self):
        nc, P = self.nc, self.P
        with self.scope() as es:
            sb = lambda n, s, dt=F32: self.sb(es, n, s, dt)
            wb = self.load_cast(es, "winb", self.w_in_b, 3 * D)
            A = self.load_mod(es, "A1", 6)
            B = self.load_mod(es, "B1", 7)
            xt = [sb("xt", [128, D]) for _ in range(2)]
            sq_ = [sb("sq", [128, D]) for _ in range(2)]; ss_ = [sb("ss", [128, 4]) for _ in range(2)]
            hf_ = [sb("hf", [128, D]) for _ in range(2)]; hb_ = [sb("hb", [128, D], BF16) for _ in range(2)]
            hT = [sb("hT", [128, 8, 128], BF16) for _ in range(2)]
            gbt = [sb("gbt", [128, D]) for _ in range(2)]
            gcc_ = [sb("gc_", [128, D]) for _ in range(2)]
            ut = [sb("ut", [128, D]) for _ in range(2)]
            def dbody(i):
                x_ = xt[i % 2]
                sq, ss, hf, hb, gc_ = sq_[i % 2], ss_[i % 2], hf_[i % 2], hb_[i % 2], gcc_[i % 2]
                P.dma("sp", x_[:], self.X2[i * 128:(i + 1) * 128, :], r=[("x2", i)])
                self.modulate(x_, A, B, sq, ss, hf, hb)
                yield
                hT_ = hT[i % 2]
                self.transpose8(hT_, hb, self.pt[i % 2], i)
                yield
                g_, u_ = gbt[i % 2], ut[i % 2]
                for g in range(6):
                    ps = self.pf[(g + 3 * (i % 2)) % 6]
                    for kc in range(8):
                        P.mm(ps[:], hT_[:, kc, :], wb[:, kc, g * 512:(g + 1) * 512], start=(kc == 0), stop=(kc == 7))
                    cs = slice((g % 2) * 512, (g % 2 + 1) * 512)
                    if g < 2:
                        self.evac(g_[:, cs], ps[:], g)
                    elif g < 4:
                        self.evac(gc_[:, cs], ps[:], g)
                    else:
                        P.tt(u_[:, cs], ps[:], gc_[:, cs], ALU.mult)
                    if g % 2 == 1:
                        yield
                P.dma("pool", self.GB[i * 128:(i + 1) * 128, :], g_[:], w=[("gb", i)])
                P.dma("pool", self.U1[i * 128:(i + 1) * 128, :], u_[:], w=[("u1", i)])
                yield
            self.run2(self.NT, dbody)

    def phase_convE(self):
        nc, P = self.nc, self.P
        NT = self.NT
        H = D // 2
        with self.scope() as es:
            sb = lambda n, s, dt=F32: self.sb(es, n, s, dt)
            wo = self.load_cast(es, "woutb", self.w_out_b, D)
            cb = [self.bcast_load(es, "cb%d" % s, self.conv_b[s], D) for s in range(3)]
            Gm = self.load_mod(es, "Gm1", 8)
            mm_ = sb("mm_", [128, 4])
            P.ts(mm_[:, 0:1], self.rowi[:, 0:1], 64.0, ALU.is_equal)
            P.ts(mm_[:, 1:2], self.rowi[:, 0:1], 0.0, ALU.is_equal)
            P.tt(mm_[:, 0:1], mm_[:, 0:1], mm_[:, 1:2], ALU.add)
            P.ts(mm_[:, 0:1], mm_[:, 0:1], -1.0, ALU.mult, 1.0, ALU.add)
            P.ts(mm_[:, 2:3], self.rowi[:, 0:1], 63.0, ALU.is_equal)
            P.ts(mm_[:, 3:4], self.rowi[:, 0:1], 127.0, ALU.is_equal)
            P.tt(mm_[:, 2:3], mm_[:, 2:3], mm_[:, 3:4], ALU.add)
            P.ts(mm_[:, 2:3], mm_[:, 2:3], -1.0, ALU.mult, 1.0, ALU.add)
            um = [sb("um", [128, D]) for _ in range(2)]
            up = [sb("up", [128, D]) for _ in range(2)]
            uc = [sb("uc", [128, D]) for _ in range(2)]
            gbt = [sb("gbt", [128, D]) for _ in range(2)]
            xt = [sb("xt", [128, D]) for _ in range(2)]
            yc_ = [sb("yc", [128, D]) for _ in range(2)]
            vbb_ = [sb("vb_", [128, D], BF16) for _ in range(2)]
            vT_ = [sb("vT", [128, 8, 128], BF16) for _ in range(2)]
            x3t = [sb("x3t", [128, D]) for _ in range(2)]
            for t in um + up:
                P.memset(t[:], 0.0, eng="pool")
            def ebody(i):
                sl = i % 2
                r0 = i * 128
                yc, vb_, vT = yc_[sl], vbb_[sl], vT_[sl]
                um_, up_, uc_ = um[sl], up[sl], uc[sl]
                P.dma("sp", uc_[:], self.U1[r0:r0 + 128, :], r=[("u1", i)])
                P.dma("sp", gbt[sl][:], self.GB[r0:r0 + 128, :], r=[("gb", i)])
                P.dma("sp", xt[sl][:], self.X2[r0:r0 + 128, :], r=[("x2", i)])
                if i == 0:
                    P.dma("sp", um_[1:128, 0:H], self.U1[0:127, 0:H], r=[("u1", 0)], w=[um_])
                else:
                    P.dma("sp", um_[:, 0:H], self.U1[r0 - 1:r0 + 127, 0:H], r=[("u1", i), ("u1", i - 1)], w=[um_])
                if i == NT - 1:
                    P.dma("sp", up_[0:127, 0:H], self.U1[r0 + 1:r0 + 128, 0:H], r=[("u1", i)], w=[up_])
                else:
                    P.dma("sp", up_[:, 0:H], self.U1[r0 + 1:r0 + 129, 0:H], r=[("u1", i), ("u1", i + 1)], w=[up_])
                if i == 0:
                    P.memset(um_[0:64, H:D], 0.0, eng="pool")
                    P.dma("sp", um_[64:128, H:D], self.U1[0:64, H:D], r=[("u1", 0)], w=[um_])
                else:
                    P.dma("sp", um_[:, H:D], self.U1[r0 - 64:r0 + 64, H:D], r=[("u1", i), ("u1", i - 1)], w=[um_])
                if i == NT - 1:
                    P.memset(up_[64:128, H:D], 0.0, eng="pool")
                    P.dma("sp", up_[0:64, H:D], self.U1[r0 + 64:r0 + 128, H:D], r=[("u1", i)], w=[up_])
                else:
                    P.dma("sp", up_[:, H:D], self.U1[r0 + 64:r0 + 192, H:D], r=[("u1", i), ("u1", i + 1)], w=[up_])
                yield
                P.ts(um_[:, 0:H], um_[:, 0:H], mm_[:, 0:1], ALU.mult)
                P.ts(up_[:, 0:H], up_[:, 0:H], mm_[:, 2:3], ALU.mult, eng="pool")
                P.tt(um_[:], um_[:], cb[0][:], ALU.mult)
                P.tt(up_[:], up_[:], cb[2][:], ALU.mult)
                yield
                P.tt(yc[:], uc_[:], cb[1][:], ALU.mult)
                P.tt(yc[:], yc[:], um_[:], ALU.add)
                P.tt(yc[:], yc[:], up_[:], ALU.add)
                P.tt(vb_[:], yc[:], gbt[sl][:], ALU.mult)
                yield
                self.transpose8(vT, vb_, self.pt[sl], i)
                yield
                x3 = x3t[sl]
                for half in range(2):
                    ps = self.pf[half + 2 * sl]
                    for kc in range(8):
                        P.mm(ps[:], vT[:, kc, :], wo[:, kc, half * 512:(half + 1) * 512], start=(kc == 0), stop=(kc == 7))
                    cs = slice(half * 512, (half + 1) * 512)
                    P.tt(x3[:, cs], ps[:], Gm[:, cs], ALU.mult)
                    P.tt(x3[:, cs], x3[:, cs], xt[sl][:, cs], ALU.add)
                P.dma("pool", self.X3[r0:r0 + 128, :], x3[:], w=[("x3", i)])
                yield
            self.run2(NT, ebody)


def make_in_maps(inputs, S, CTXL, n_cores, phases=99):
    f = lambda a: np.ascontiguousarray(np.asarray(a, dtype=np.float32))
    moe = {}
    if phases >= 4:
        w1 = f(inputs["w1"]); w3 = f(inputs["w3"]); w2 = f(inputs["w2"])
        for l in range(2):
            wall = np.empty((NE, 128, 3, 4096), np.float32)
            wall[:, :, 0, :] = w1[l].reshape(NE, 8, 128, DE).transpose(0, 2, 1, 3).reshape(NE, 128, 4096)
            wall[:, :, 1, :] = w3[l].reshape(NE, 8, 128, DE).transpose(0, 2, 1, 3).reshape(NE, 128, 4096)
            wall[:, :, 2, :] = w2[l].reshape(NE, 4, 128, D).transpose(0, 2, 1, 3).reshape(NE, 128, 4096)
            moe["wall%d" % l] = wall.reshape(NE * 128, 3 * 4096)
    router = np.ascontiguousarray(np.concatenate([f(inputs["router_g"]), f(inputs["router_e"])], axis=-1))
    shared = {
        "c_ctx": f(inputs["c_ctx"]), "ada_w": f(inputs["ada_w"]), "ada_b": f(inputs["ada_b"]),
        "norm_mix": f(inputs["norm_mix"]), "norm_ffn": f(inputs["norm_ffn"]),
        "w_in_a": f(inputs["w_in_a"])[0], "conv_a": f(inputs["conv_a"])[0], "a_log": f(inputs["a_log_a"])[0],
        "dt_bias": f(inputs["dt_bias_a"])[0], "onorm": f(inputs["onorm_a"])[0], "w_out_a": f(inputs["w_out_a"])[0],
        "w_in_b": f(inputs["w_in_b"])[0], "conv_b": f(inputs["conv_b"])[0], "w_out_b": f(inputs["w_out_b"])[0],
        "router": router, "final_norm": f(inputs["final_norm"]),
    }
    shared.update(moe)
    x = f(inputs["x"]); c = f(inputs["c"]); ctx = f(inputs["ctx"])
    maps = []
    for b in range(n_cores):
        m = dict(shared)
        m["x"] = np.ascontiguousarray(x[b, :S])
        m["c"] = np.ascontiguousarray(c[b])
        m["ctx"] = np.ascontiguousarray(ctx[b, :CTXL])
        maps.append(m)
    return maps


def run(inputs, S, CTXL, n_cores, phases=99, debug=(), trace=False):
    bld = Builder(S, CTXL, phases=phases, debug=debug)
    nc = bld.build()
    import time as _t
    t0 = _t.time()
    maps = make_in_maps(inputs, S, CTXL, n_cores, phases)
    print("in_maps", _t.time() - t0, flush=True)
    res = run_bass_kernel_spmd(nc, maps, core_ids=list(range(n_cores)))
    print("spmd", _t.time() - t0, flush=True)
    return res, bld


def kernel(**inputs):
    res, _ = run(inputs, 8192, 256, 8)
    return np.stack([np.asarray(r["out"], dtype=np.float32) for r in res.results], axis=0)
```

```python
import numpy as np
from contextlib import ExitStack, contextmanager
import concourse.bass as bass
import concourse.mybir as mybir
from concourse.bass_utils import run_bass_kernel_spmd

F32 = mybir.dt.float32
BF16 = mybir.dt.bfloat16
I32 = mybir.dt.int32
AF = mybir.ActivationFunctionType
ALU = mybir.AluOpType
AX = mybir.AxisListType

D = 1024
NH = 8
HD = 128
NE = 32
DE = 512
EPS = 1e-6
BIG = 30000.0
GRID_W = 64


class Op:
    __slots__ = ("eng", "fn", "deps", "need", "sem", "val", "isdma", "idx")


class Prog:
    def __init__(self, nc, ndma=32):
        self.nc = nc
        self.es = ExitStack()
        self.engs = ["pe", "dve", "act", "pool", "sp"]
        self.q = {k: [] for k in self.engs}
        self.esem = {k: self.es.enter_context(nc.semaphore("s_" + k)) for k in self.engs}
        self.dsem = [self.es.enter_context(nc.semaphore("d%d" % i)) for i in range(ndma)]
        self.dlast = [None] * ndma
        self.dcount = [0] * ndma
        self.dnext = 0
        self.lastw = {}
        self.readers = {}
        self.nops = 0

    @staticmethod
    def key(x):
        if isinstance(x, (str, tuple)):
            return x
        return x.name

    def op(self, eng, fn, r=(), w=(), dma=False):
        o = Op()
        o.eng = eng
        o.fn = fn
        o.need = False
        o.isdma = dma
        o.sem = None
        o.val = None
        o.idx = self.nops
        self.nops += 1
        deps = {}
        rk = [self.key(x) for x in r]
        wk = [self.key(x) for x in w]

        def add(d):
            if d is None:
                return
            if d.isdma:
                deps[("d", id(d))] = d
            else:
                if d.eng == eng and eng == "pe" and not dma:
                    return
                cur = deps.get(("e", d.eng))
                if cur is None or cur.idx < d.idx:
                    deps[("e", d.eng)] = d

        for k in rk:
            add(self.lastw.get(k))
        for k in wk:
            add(self.lastw.get(k))
            for rd in self.readers.get(k, ()):
                add(rd)
        if dma:
            i = self.dnext
            self.dnext = (self.dnext + 1) % len(self.dsem)
            add(self.dlast[i])
            self.dcount[i] += 16
            o.sem = self.dsem[i]
            o.val = self.dcount[i]
            self.dlast[i] = o
        o.deps = list(deps.values())
        for d in o.deps:
            d.need = True
        for k in rk:
            self.readers.setdefault(k, []).append(o)
        for k in wk:
            self.lastw[k] = o
            self.readers[k] = []
        self.q[eng].append(o)
        return o

    def dma(self, eng, out, in_, r=None, w=None, **kw):
        r = [in_] if r is None else r
        w = [out] if w is None else w
        return self.op(eng, lambda e: e.dma_start(out=out, in_=in_, **kw), r=r, w=w, dma=True)

    def mm(self, out, lhsT, rhs, start=True, stop=True, r=None, w=None):
        r = [lhsT, rhs] if r is None else r
        w = [out] if w is None else w
        return self.op("pe", lambda e: e.matmul(out, lhsT, rhs, start=start, stop=stop), r=r, w=w)

    def tr(self, out, in_, ident, r=None, w=None):
        r = [in_, ident] if r is None else r
        w = [out] if w is None else w
        return self.op("pe", lambda e: e.transpose(out, in_, ident), r=r, w=w)

    def act(self, out, in_, func, bias=None, scale=None, r=None, w=None):
        rr = [in_] + ([bias] if bias is not None and not isinstance(bias, (int, float)) else [])
        r = rr if r is None else r
        w = [out] if w is None else w
        kw = {}
        if bias is not None:
            kw["bias"] = bias
        if scale is not None:
            kw["scale"] = scale
        return self.op("act", lambda e: e.activation(out=out, in_=in_, func=func, **kw), r=r, w=w)

    def tt(self, out, in0, in1, op, eng="dve", r=None, w=None):
        r = [in0, in1] if r is None else r
        w = [out] if w is None else w
        return self.op(eng, lambda e: e.tensor_tensor(out=out, in0=in0, in1=in1, op=op), r=r, w=w)

    def ts(self, out, in0, s1, op0, s2=None, op1=None, eng="dve", r=None, w=None):
        rr = [in0] + [s for s in (s1, s2) if s is not None and not isinstance(s, (int, float))]
        r = rr if r is None else r
        w = [out] if w is None else w
        if op1 is None:
            return self.op(eng, lambda e: e.tensor_scalar(out=out, in0=in0, scalar1=s1, scalar2=None, op0=op0), r=r, w=w)
        return self.op(eng, lambda e: e.tensor_scalar(out=out, in0=in0, scalar1=s1, scalar2=s2, op0=op0, op1=op1), r=r, w=w)

    def stt(self, out, in0, scalar, in1, op0, op1, r=None, w=None):
        rr = [in0, in1] + ([scalar] if not isinstance(scalar, (int, float)) else [])
        r = rr if r is None else r
        w = [out] if w is None else w
        return self.op("dve", lambda e: e.scalar_tensor_tensor(out=out, in0=in0, scalar=scalar, in1=in1, op0=op0, op1=op1), r=r, w=w)

    def copy(self, out, in_, eng="dve", r=None, w=None):
        r = [in_] if r is None else r
        w = [out] if w is None else w
        if eng == "act":
            return self.op("act", lambda e: e.copy(out=out, in_=in_), r=r, w=w)
        return self.op(eng, lambda e: e.tensor_copy(out=out, in_=in_), r=r, w=w)

    def red(self, out, in_, op, axis=AX.X, r=None, w=None):
        r = [in_] if r is None else r
        w = [out] if w is None else w
        return self.op("dve", lambda e: e.tensor_reduce(out=out, in_=in_, axis=axis, op=op), r=r, w=w)

    def memset(self, ap, val, eng="dve"):
        return self.op(eng, lambda e: e.memset(ap, val), r=[], w=[ap])

    def recip(self, out, in_, r=None, w=None):
        r = [in_] if r is None else r
        w = [out] if w is None else w
        return self.op("dve", lambda e: e.reciprocal(out=out, in_=in_), r=r, w=w)

    def barrier(self):
        lasts = [d for d in self.dlast if d is not None]
        for e in self.engs:
            for o in reversed(self.q[e]):
                if not o.isdma and o.fn is not None:
                    lasts.append(o)
                    break
        for d in lasts:
            d.need = True
        for e in self.engs:
            b = Op()
            b.eng = e
            b.fn = None
            b.need = False
            b.isdma = False
            b.sem = None
            b.val = None
            b.idx = self.nops
            self.nops += 1
            b.deps = list(lasts)
            self.q[e].append(b)
        self.lastw = {}
        self.readers = {}

    def finish(self):
        fin = Op()
        fin.eng = "sp"
        fin.fn = None
        fin.need = False
        fin.isdma = False
        fin.idx = self.nops
        deps = [d for d in self.dlast if d is not None]
        for e in self.engs:
            for o in reversed(self.q[e]):
                if not o.isdma and o.fn is not None:
                    o.need = True
                    deps.append(o)
                    break
        fin.deps = deps
        self.q["sp"].append(fin)
        for e in self.engs:
            c = 0
            for o in self.q[e]:
                if o.isdma or o.fn is None:
                    continue
                if o.need:
                    c += 1
                    o.val = c
                    o.sem = self.esem[e]
        self.counts = {e: len(self.q[e]) for e in self.engs}

        def replay(name, e):
            waited = {}
            for o in self.q[name]:
                for d in o.deps:
                    sk = id(d.sem)
                    if waited.get(sk, 0) < d.val:
                        e.wait_ge(d.sem, d.val)
                        waited[sk] = d.val
                if o.fn is None:
                    continue
                ins = o.fn(e)
                if o.isdma:
                    ins.then_inc(o.sem, 16)
                elif o.need:
                    ins.then_inc(o.sem, 1)

        nc = self.nc
        with nc.Block() as block:
            @block.tensor
            def _(e):
                replay("pe", e)

            @block.vector
            def _(e):
                replay("dve", e)

            @block.scalar
            def _(e):
                replay("act", e)

            @block.gpsimd
            def _(e):
                replay("pool", e)

            @block.sync
            def _(e):
                replay("sp", e)
        self.es.close()


class Builder:
    def __init__(self, S, CTXL, phases=99, debug=()):
        self.S = S
        self.CTXL = CTXL
        self.NT = S // 128
        self.NC = CTXL // 128
        self.T = S
        self.NBLK = (2 * S) // 128 + NE
        self.phases = phases
        self.debug = set(debug)
        nc = self.nc = bass.Bass("TRN2", target_bir_lowering=False)
        self.P = Prog(nc)
        self.top = ExitStack()
        self._n = 0
        self.regcache = {}
        self.pfi = [0]

        def din(name, shape, dt=F32):
            return nc.dram_tensor(name, list(shape), dt, kind="ExternalInput").ap()

        self.x = din("x", [S, D])
        self.c = din("c", [D])
        self.ctx = din("ctx", [CTXL, D])
        self.c_ctx = din("c_ctx", [D])
        self.ada_w = din("ada_w", [2, D, 6 * D])
        self.ada_b = din("ada_b", [2, 6 * D])
        self.norm_mix = din("norm_mix", [2, D])
        self.norm_ffn = din("norm_ffn", [2, D])
        self.w_in_a = din("w_in_a", [D, 4 * D + 32])
        self.conv_a = din("conv_a", [3, 3 * D])
        self.a_log = din("a_log", [2, NH])
        self.dt_bias = din("dt_bias", [2, NH])
        self.onorm = din("onorm", [HD])
        self.w_out_a = din("w_out_a", [D, D])
        self.w_in_b = din("w_in_b", [D, 3 * D])
        self.conv_b = din("conv_b", [3, D])
        self.w_out_b = din("w_out_b", [D, D])
        self.router = din("router", [2, D, 36])
        if phases >= 4:
            self.wall = [din("wall%d" % l, [NE * 128, 3 * 4096]) for l in range(2)]
        self.final_norm = din("final_norm", [D])
        self.out = nc.dram_tensor("out", [S, D], F32, kind="ExternalOutput").ap()

        TT = S + CTXL
        self.MODP = self.scr("modp", [16, 128, D])
        self.P0 = self.scr("p0", [TT, 4 * D + 32])
        self.OF = self.scr("of", [S, D])
        self.OB = self.scr("ob", [S, D])
        self.QKV = self.scr("qkv", [TT, 3 * D], BF16)
        self.X1 = self.scr("x1", [S, D])
        self.X2 = self.scr("x2", [S, D])
        self.X3 = self.scr("x3", [S, D])
        self.HF = self.scr("hfb", [S, D], BF16)
        self.XB = self.scr("xb", [self.NBLK * 128, D], BF16)
        self.YB = self.scr("yb", [self.NBLK * 128, D])
        self.U1 = self.scr("u1", [S, D])
        self.GB = self.scr("gb", [S, D])

    @contextmanager
    def scope(self):
        with ExitStack() as es:
            yield es
            self.P.barrier()

    def run2(self, n, body, lag=0):
        def stream(par):
            for i in range(par, n, 2):
                yield from body(i)
        gens = [stream(0), stream(1)]
        for _ in range(lag):
            next(gens[0])
        while gens:
            for g in list(gens):
                try:
                    next(g)
                except StopIteration:
                    gens.remove(g)

    def scr(self, name, shape, dt=F32):
        kind = "ExternalOutput" if name in self.debug else "Internal"
        return self.nc.dram_tensor(name, list(shape), dt, kind=kind).ap()

    def sb(self, es, name, shape, dt=F32):
        self._n += 1
        return es.enter_context(self.nc.sbuf_tensor("%s_%d" % (name, self._n), list(shape), dt))

    def build(self):
        nc, P = self.nc, self.P
        top = self.top
        self.pf = [top.enter_context(nc.psum_tensor("pf%d" % i, [128, 512], F32)) for i in range(6)]
        self.pt = [top.enter_context(nc.psum_tensor("pt%d" % i, [128, 8, 128], BF16)) for i in range(2)]
        self.consts()
        self.phase_ada()
        if self.phases >= 1:
            self.phase_projA()
        if self.phases >= 2:
            self.phase_qkv()
        if self.phases >= 3:
            self.phase_delta_both()
            self.phase_finish()
        if self.phases >= 4:
            self.phase_moe(0, self.X1, self.X2, final=False)
        if self.phases >= 5:
            self.phase_convD()
        if self.phases >= 6:
            self.phase_convE()
        if self.phases >= 7:
            self.phase_moe(1, self.X3, self.out, final=True)
        P.finish()
        top.close()
        return nc

    def consts(self):
        nc, P, top = self.nc, self.P, self.top
        sb = lambda n, s, d=F32: self.sb(top, n, s, d)
        rowi = sb("rowi", [128, 128])
        coli = sb("coli", [128, 128])
        P.op("pool", lambda e: e.iota(rowi[:], pattern=[[0, 128]], base=0, channel_multiplier=1,
                                       allow_small_or_imprecise_dtypes=True), w=[rowi])
        P.op("pool", lambda e: e.iota(coli[:], pattern=[[1, 128]], base=0, channel_multiplier=0,
                                       allow_small_or_imprecise_dtypes=True), w=[coli])
        self.rowi, self.coli = rowi, coli
        self.ident_f = sb("ident_f", [128, 128])
        self.ident_b = sb("ident_b", [128, 128], BF16)
        self.ones_f = sb("ones_f", [128, 128])
        self.ones_b = sb("ones_b", [128, 128], BF16)
        P.tt(self.ident_f[:], rowi[:], coli[:], ALU.is_equal)
        P.copy(self.ident_b[:], self.ident_f[:])
        P.memset(self.ones_f[:], 1.0)
        P.memset(self.ones_b[:], 1.0)
        self.U = [sb("Uf", [128, 128]), sb("Ub", [128, 128])]
        P.tt(self.U[0][:], rowi[:], coli[:], ALU.is_le)
        P.tt(self.U[1][:], rowi[:], coli[:], ALU.is_ge)
        self.SM = [sb("SMf", [128, 128]), sb("SMb", [128, 128])]
        P.tt(self.SM[0][:], rowi[:], coli[:], ALU.is_gt)
        P.tt(self.SM[1][:], rowi[:], coli[:], ALU.is_lt)
        self.BM = [sb("BMf", [128, 4, 128]), sb("BMb", [128, 4, 128])]
        for d in range(2):
            for h in range(4):
                P.ts(self.BM[d][:, h, :], self.SM[1 - d][:], BIG, ALU.mult)
        self.Us_b = sb("Us_b", [128, 128], BF16)
        P.copy(self.Us_b[:], self.SM[1][:])
        bd = {}
        colb = sb("colb", [128, 128]); rowb = sb("rowb", [128, 128])
        for sz in (8, 16, 32, 64):
            P.op("pool", lambda e, sz=sz: e.iota(colb[:], pattern=[[1, 128 // sz], [0, sz]], base=0, channel_multiplier=0,
                                                  allow_small_or_imprecise_dtypes=True), w=[colb])
            P.tr(self.pf[0][:, 0:128], colb[:], self.ident_f[:])
            P.copy(rowb[:], self.pf[0][:, 0:128])
            bd[sz] = sb("bd%d" % sz, [128, 128])
            P.tt(bd[sz][:], rowb[:], colb[:], ALU.is_equal)
        self.BD8 = sb("BD8b", [128, 128], BF16)
        P.copy(self.BD8[:], bd[8][:])
        self.MS = {}
        for sz in (8, 16, 32, 64):
            self.MS[sz] = sb("MS%d" % sz, [128, 128], BF16)
            if sz < 64:
                P.tt(self.MS[sz][:], bd[2 * sz][:], bd[sz][:], ALU.subtract)
            else:
                P.ts(self.MS[sz][:], bd[sz][:], -1.0, ALU.mult, 1.0, ALU.add)
        self.epsc = sb("epsc", [128, 1])
        P.memset(self.epsc[:], EPS)

    def evac(self, out, in_, i):
        if i % 2 == 0:
            return self.P.copy(out, in_, eng="dve")
        return self.P.copy(out, in_, eng="act")

    def load_cast(self, es, name, src_ap, ncols, chunk=512, kparts=8, qeng="sp"):
        P = self.P
        wb = self.sb(es, name, [128, kparts, ncols], BF16)
        with self.scope() as tmp:
            st = [self.sb(tmp, name + "_st", [128, kparts, chunk]) for _ in range(2)]
            src = src_ap.rearrange("(kc p) n -> p kc n", p=128)
            i = 0
            for c0 in range(0, ncols, chunk):
                cw = min(chunk, ncols - c0)
                s = st[i % 2]
                P.dma(qeng, s[:, :, 0:cw], src[:, :, c0:c0 + cw])
                if i % 2 == 0:
                    P.copy(wb[:, :, c0:c0 + cw], s[:, :, 0:cw], eng="dve")
                else:
                    P.copy(wb[:, :, c0:c0 + cw], s[:, :, 0:cw], eng="pool")
                i += 1
        return wb

    def bcast_load(self, es, name, vec_ap, n):
        t = self.sb(es, name, [128, n])
        self.P.dma("sp", t[:], vec_ap.partition_broadcast(128))
        return t

    def modulate(self, xt, A, B, sq, ss, hf, hb):
        P = self.P
        P.act(sq[:], xt[:], AF.Square)
        P.red(ss[:, 0:1], sq[:], ALU.add)
        P.ts(ss[:, 1:2], ss[:, 0:1], 1.0 / D, ALU.mult, EPS, ALU.add)
        P.act(ss[:, 2:3], ss[:, 1:2], AF.Sqrt)
        P.recip(ss[:, 3:4], ss[:, 2:3])
        P.stt(hf[:], xt[:], ss[:, 3:4], A[:], ALU.mult, ALU.mult)
        if hb is not None:
            P.tt(hb[:], hf[:], B[:], ALU.add)
        else:
            P.tt(hf[:], hf[:], B[:], ALU.add)

    def transpose8(self, dstT, src_bf, ptile, i=0, n=8):
        P = self.P
        for kc in range(n):
            P.tr(ptile[:, kc, :], src_bf[:, kc * 128:(kc + 1) * 128], self.ident_b[:])
        self.evac(dstT[:, 0:n, :], ptile[:, 0:n, :], i)

    def phase_ada(self):
        nc, P = self.nc, self.P
        with self.scope() as es:
            sb = lambda n, s, d=F32: self.sb(es, n, s, d)
            craw = sb("craw", [128, 2, 8])
            P.dma("sp", craw[:, 0, :], self.c.rearrange("(kc p) -> p kc", p=128), allow_slow_non_contiguous=True)
            P.dma("sp", craw[:, 1, :], self.c_ctx.rearrange("(kc p) -> p kc", p=128), allow_slow_non_contiguous=True)
            csil = sb("csil", [128, 2, 8])
            P.act(csil[:], craw[:], AF.Silu)
            cb = sb("cb", [128, 2, 8, 128])
            for j in range(2):
                P.copy(cb[:, j, :, :], csil[:, j, :].unsqueeze(2).to_broadcast([128, 8, 128]))
            wst = [sb("adaw", [128, 8, 512]) for _ in range(2)]
            mb = sb("mb", [128, 6 * D])
            bb = sb("bb", [128, 6 * D])
            nm = sb("nm", [128, D])
            res = [sb("res", [128, D]) for _ in range(2)]
            cnt = 0
            for (layer, j) in ((0, 0), (0, 1), (1, 0)):
                P.dma("sp", bb[:], self.ada_b[layer].partition_broadcast(128))
                for n in range(12):
                    w = wst[cnt % 2]
                    P.dma("sp", w[:], self.ada_w[layer].rearrange("(kc p) n -> p kc n", p=128)[:, :, n * 512:(n + 1) * 512])
                    ps = self.pf[cnt % 2]
                    for kc in range(8):
                        P.mm(ps[:], cb[:, j, kc, :], w[:, kc, :], start=(kc == 0), stop=(kc == 7))
                    P.tt(mb[:, n * 512:(n + 1) * 512], ps[:], bb[:, n * 512:(n + 1) * 512], ALU.add)
                    cnt += 1
                outs = []
                for sub, (nrm, base) in enumerate(((self.norm_mix, 0), (self.norm_ffn, 3))):
                    if j == 1 and sub == 1:
                        continue
                    P.dma("sp", nm[:], nrm[layer].partition_broadcast(128))
                    r0 = res[0]
                    P.stt(r0[:], mb[:, (base + 1) * D:(base + 2) * D], 1.0, nm[:], ALU.add, ALU.mult)
                    ka = (12 if j == 1 else 6 * layer + base)
                    P.dma("sp", self.MODP[ka], r0[:], w=[("modp", ka)])
                    P.dma("sp", self.MODP[ka + 1], mb[:, base * D:(base + 1) * D], w=[("modp", ka + 1)])
                    if j == 0:
                        P.dma("sp", self.MODP[ka + 2], mb[:, (base + 2) * D:(base + 3) * D], w=[("modp", ka + 2)])

    def load_mod(self, es, name, k):
        t = self.sb(es, name, [128, D])
        self.P.dma("sp", t[:], self.MODP[k], r=[("modp", k)])
        return t

    def phase_projA(self):
        nc, P = self.nc, self.P
        NCOL = 4 * D + 32
        with self.scope() as es:
            sb = lambda n, s, d=F32: self.sb(es, n, s, d)
            wb = self.load_cast(es, "wina", self.w_in_a, NCOL)
            A = [self.load_mod(es, "A", 0), self.load_mod(es, "Ac", 12)]
            B = [self.load_mod(es, "B", 1), self.load_mod(es, "Bc", 13)]
            xt = [sb("xt", [128, D]) for _ in range(2)]
            sq = sb("sq", [128, D])
            ss = sb("ss", [128, 4])
            hf = sb("hf", [128, D])
            hb = sb("hb", [128, D], BF16)
            hT = [sb("hT", [128, 8, 128], BF16) for _ in range(2)]
            po = [sb("po", [128, NCOL]) for _ in range(2)]
            tiles = [("c", i) for i in range(self.NC)] + [("l", i) for i in range(self.NT)]
            for ti, (kind, i) in enumerate(tiles):
                src = self.ctx if kind == "c" else self.x
                j = 1 if kind == "c" else 0
                row0 = i * 128 + (0 if kind == "c" else self.CTXL)
                x_ = xt[ti % 2]
                P.dma("sp", x_[:], src[i * 128:(i + 1) * 128, :])
                self.modulate(x_, A[j], B[j], sq, ss, hf, hb)
                hT_ = hT[ti % 2]
                self.transpose8(hT_, hb, self.pt[ti % 2], ti)
                po_ = po[ti % 2]
                g = 0
                for c0 in range(0, NCOL, 512):
                    cw = min(512, NCOL - c0)
                    ps = self.pf[g % 4]
                    for kc in range(8):
                        P.mm(ps[:, 0:cw], hT_[:, kc, :], wb[:, kc, c0:c0 + cw], start=(kc == 0), stop=(kc == 7))
                    self.evac(po_[:, c0:c0 + cw], ps[:, 0:cw], g)
                    g += 1
                P.dma("pool", self.P0[row0:row0 + 128, :], po_[:], w=[("p0", row0 // 128)])

    def phase_qkv(self):
        nc, P = self.nc, self.P
        CT = self.CTXL
        with self.scope() as es:
            sb = lambda n, s, dt=F32: self.sb(es, n, s, dt)
            cw = [self.bcast_load(es, "cw%d" % s, self.conv_a[s], 3 * D) for s in range(3)]
            pm_ = [sb("pm", [128, 3 * D]) for _ in range(2)]
            pc_ = [sb("pc", [128, 3 * D]) for _ in range(2)]
            pp_ = [sb("pp", [128, 3 * D]) for _ in range(2)]
            qo_ = [sb("qo", [128, 3 * D], BF16) for _ in range(2)]
            so_ = [sb("so", [128, 3 * D]) for _ in range(2)]
            ssq_ = [sb("ssq", [128, 64]) for _ in range(2)]
            bc = lambda ap: ap.unsqueeze(2).to_broadcast([128, ap.shape[1], HD])
            tiles = [("c", i) for i in range(self.NC)] + [("l", i) for i in range(self.NT)]

            def loads(ti):
                if ti >= len(tiles):
                    return
                kind, i = tiles[ti]
                pm, pc, pp = pm_[ti % 2], pc_[ti % 2], pp_[ti % 2]
                ntile = self.NC if kind == "c" else self.NT
                row0 = (0 if kind == "c" else CT) + i * 128
                P.dma("sp", pc[:], self.P0[row0:row0 + 128, 0:3 * D])
                if i == 0:
                    P.memset(pm[:], 0.0, eng="pool")
                    P.dma("sp", pm[1:128, :], self.P0[row0:row0 + 127, 0:3 * D], w=[pm])
                else:
                    P.dma("sp", pm[:], self.P0[row0 - 1:row0 + 127, 0:3 * D])
                if i == ntile - 1:
                    P.memset(pp[:], 0.0, eng="pool")
                    P.dma("sp", pp[0:127, :], self.P0[row0 + 1:row0 + 128, 0:3 * D], w=[pp])
                else:
                    P.dma("sp", pp[:], self.P0[row0 + 1:row0 + 129, 0:3 * D])

            loads(0)
            loads(1)

            def qbody(ti):
                kind, i = tiles[ti]
                pm, pc, pp, qo, ssq = pm_[ti % 2], pc_[ti % 2], pp_[ti % 2], qo_[ti % 2], ssq_[ti % 2]
                so = so_[ti % 2]
                row0 = (0 if kind == "c" else CT) + i * 128
                P.tt(pm[:], pm[:], cw[0][:], ALU.mult)
                P.tt(pc[:], pc[:], cw[1][:], ALU.mult)
                P.tt(pp[:], pp[:], cw[2][:], ALU.mult)
                yield
                for c in range(6):
                    ps = self.pf[c]
                    cs = slice(c * 512, (c + 1) * 512)
                    P.mm(ps[:], self.ident_f[:], pm[:, cs], start=True, stop=False)
                    P.mm(ps[:], self.ident_f[:], pc[:, cs], start=False, stop=False)
                    P.mm(ps[:], self.ident_f[:], pp[:, cs], start=False, stop=True)
                    P.act(so[:, cs], ps[:], AF.Silu)
                    if c % 2 == 1:
                        yield
                P.act(pm[:, 0:2 * D], so[:, 0:2 * D], AF.Square)
                yield
                P.red(ssq[:, 0:16], pm[:, 0:2 * D].rearrange("p (h e) -> p h e", e=HD), ALU.add)
                P.ts(ssq[:, 16:32], ssq[:, 0:16], EPS, ALU.add)
                P.act(ssq[:, 32:48], ssq[:, 16:32], AF.Sqrt)
                P.recip(ssq[:, 48:64], ssq[:, 32:48])
                P.ts(ssq[:, 48:56], ssq[:, 48:56], HD ** -0.5, ALU.mult)
                P.tt(qo[:, 0:D].rearrange("p (h e) -> p h e", e=HD), so[:, 0:D].rearrange("p (h e) -> p h e", e=HD),
                     bc(ssq[:, 48:56]), ALU.mult)
                P.tt(qo[:, D:2 * D].rearrange("p (h e) -> p h e", e=HD), so[:, D:2 * D].rearrange("p (h e) -> p h e", e=HD),
                     bc(ssq[:, 56:64]), ALU.mult, eng="pool")
                P.copy(qo[:, 2 * D:3 * D], so[:, 2 * D:3 * D], eng="act")
                yield
                loads(ti + 2)
                P.dma("pool", self.QKV[row0:row0 + 128, :], qo[:])
                yield
            self.run2(len(tiles), qbody)

    def phase_delta_both(self):
        with self.scope() as es:
            gens = [self.delta_gen(es, 0), self.delta_gen(es, 1)]
            while gens:
                for g in list(gens):
                    try:
                        next(g)
                    except StopIteration:
                        gens.remove(g)

    def delta_gen(self, es, d):
        nc, P = self.nc, self.P
        CT = self.CTXL
        sb = lambda n, s, dt=F32: self.sb(es, n, s, dt)
        nA = self.bcast_load(es, "nA", self.a_log[d], NH)
        P.act(nA[:], nA[:], AF.Exp)
        P.ts(nA[:], nA[:], -1.0, ALU.mult)
        dtb = self.bcast_load(es, "dtb", self.dt_bias[d], NH)
        qv = sb("qv", [128, 3 * D], BF16)
        gts = sb("gts", [128, 32])
        sml = sb("sml", [128, 16 * NH])
        kT = sb("kT", [128, NH, HD], BF16)
        qT = sb("qT", [128, NH, HD], BF16)
        qd_b = sb("qd_b", [128, NH, HD], BF16)
        qdT = sb("qdT", [128, NH, HD], BF16)
        vb = sb("vb", [128, NH, HD], BF16)
        kbe = sb("kbe", [128, NH, HD], BF16)
        kdec = sb("kdec", [128, NH, HD], BF16)
        rhsR = sb("rhsR", [128, NH, HD])
        Dm = sb("Dm", [128, NH, HD])
        As = sb("As", [128, NH, HD], BF16)
        qk_b = sb("qk_b", [128, NH, HD], BF16)
        qkT = sb("qkT", [128, NH, HD], BF16)
        L_b = sb("L_b", [128, NH, HD], BF16); N_b = sb("N_b", [128, NH, HD], BF16)
        Ld = sb("Ld", [128, NH, HD], BF16); Nd = sb("Nd", [128, NH, HD], BF16)
        P1 = sb("P1", [128, NH, HD], BF16); Q1 = sb("Q1", [128, NH, HD], BF16)
        P2 = sb("P2", [128, NH, HD], BF16); Q2 = sb("Q2", [128, NH, HD], BF16)
        Tw = [sb("Tw", [128, NH, HD], BF16) for _ in range(2)]
        Xw = [sb("Xw", [128, NH, HD], BF16) for _ in range(2)]
        u32 = sb("u32", [128, NH, HD])
        wT = sb("wT", [128, NH, HD], BF16)
        vnew = sb("vnew", [128, NH, HD], BF16)
        S32 = sb("S32", [128, NH, HD])
        Sb = sb("Sb", [128, NH, HD], BF16)
        Stmp = sb("Stmp", [128, NH, HD])
        o32 = sb("o32", [128, NH, HD])
        P.memset(S32[:], 0.0)
        P.memset(Sb[:], 0.0, eng="pool")
        OUT = self.OF if d == 0 else self.OB
        ptd = self.pt[d]
        if d == 0:
            order = [("c", i) for i in range(self.NC)] + [("l", i) for i in range(self.NT)]
        else:
            order = [("c", i) for i in reversed(range(self.NC))] + [("l", i) for i in reversed(range(self.NT))]
        bc = lambda ap: ap.unsqueeze(2).to_broadcast([128, ap.shape[1], HD])
        mb = lambda m: m[:].unsqueeze(1).to_broadcast([128, NH, HD])
        v4 = lambda ps: ps[:].rearrange("p (h e) -> p h e", e=HD)
        pfi = self.pfi

        def nextpf():
            pfi[0] += 1
            return self.pf[pfi[0] % 6]

        def mm4(lhs, rhs, hg):
            ps = nextpf()
            for hh in range(4):
                h = hg * 4 + hh
                P.mm(ps[:, hh * 128:(hh + 1) * 128], lhs[:, h, :], rhs[:, h, :])
            return ps

        for ti, (kind, i) in enumerate(order):
            row0 = (0 if kind == "c" else CT) + i * 128
            P.dma("sp", qv[:], self.QKV[row0:row0 + 128, :])
            P.dma("sp", gts[:], self.P0[row0:row0 + 128, 4 * D:4 * D + 32])
            qn_b = qv[:, 0:D].rearrange("p (h e) -> p h e", e=HD)
            kn_b = qv[:, D:2 * D].rearrange("p (h e) -> p h e", e=HD)
            v_b = qv[:, 2 * D:3 * D].rearrange("p (h e) -> p h e", e=HD)
            yield
            beta = sml[:, 0:8]
            P.act(beta, gts[:, d * 8:d * 8 + 8], AF.Sigmoid)
            xg = sml[:, 8:16]
            P.tt(xg, gts[:, 16 + d * 8:16 + d * 8 + 8], dtb[:], ALU.add)
            P.stt(sml[:, 16:24], xg, -1.0, xg, ALU.mult, ALU.max)
            P.act(sml[:, 24:32], sml[:, 16:24], AF.Exp, scale=-1.0)
            P.act(sml[:, 32:40], sml[:, 24:32], AF.Ln, bias=1.0)
            P.stt(sml[:, 40:48], xg, 0.0, sml[:, 32:40], ALU.max, ALU.add)
            g = sml[:, 48:56]
            P.tt(g, sml[:, 40:48], nA[:], ALU.mult)
            yield
            ps = nextpf()
            P.mm(ps[:, 0:8], self.U[d][:], g)
            P.mm(ps[:, 8:16], self.ones_f[:], g)
            gc = sml[:, 56:64]
            gl = sml[:, 64:72]
            P.copy(sml[:, 56:72], ps[:, 0:16])
            for (src, dst) in ((kn_b, kT), (qn_b, qT)):
                for h in range(NH):
                    P.tr(ptd[:, h, :], src[:, h, :], self.ident_b[:])
                self.evac(dst[:], ptd[:], d)
                yield
            egc = sml[:, 72:80]
            P.act(egc, gc, AF.Exp)
            egl = sml[:, 80:88]
            P.act(egl, gl, AF.Exp)
            kdc = sml[:, 88:96]
            P.tt(kdc, gl, gc, ALU.subtract)
            P.act(kdc, kdc, AF.Exp)
            bq = sml[:, 96:104]
            P.tt(bq, beta, egc, ALU.mult)
            yield
            P.tt(vb[:], v_b, bc(beta), ALU.mult)
            P.tt(kbe[:], kn_b, bc(bq), ALU.mult, eng="pool")
            P.tt(kdec[:], kn_b, bc(kdc), ALU.mult, eng="pool")
            P.tt(qd_b[:], qn_b, bc(egc), ALU.mult)
            P.tt(rhsR[:], self.U[d][:].unsqueeze(1).to_broadcast([128, NH, HD]), bc(g), ALU.mult, eng="pool")
            yield
            for h in range(NH):
                P.tr(ptd[:, h, :], qd_b[:, h, :], self.ident_b[:])
            self.evac(qdT[:], ptd[:], d + 1)
            yield
            for hg in range(2):
                ps = nextpf()
                P.mm(ps[:], self.ones_f[:], rhsR[:, hg * 4:(hg + 1) * 4, :].rearrange("p h e -> p (h e)"), start=True, stop=False)
                P.mm(ps[:], self.ident_f[:], self.BM[d][:].rearrange("p h e -> p (h e)"), start=False, stop=True)
                for hh in range(4):
                    h = hg * 4 + hh
                    P.act(Dm[:, h, :], ps[:, hh * 128:(hh + 1) * 128], AF.Exp, bias=gc[:, h:h + 1], scale=-1.0)
                yield
            for hg in range(2):
                hs = slice(hg * 4, (hg + 1) * 4)
                P.tt(As[:, hs, :], v4(mm4(kT, kT, hg)), self.SM[d][:].unsqueeze(1).to_broadcast([128, 4, HD]), ALU.mult)
                P.tt(qk_b[:, hs, :], v4(mm4(qT, kT, hg)), Dm[:, hs, :], ALU.mult)
                yield
            for h in range(NH):
                P.stt(L_b[:, h, :], As[:, h, :], beta[:, h:h + 1], Dm[:, h, :], ALU.mult, ALU.mult)
            yield
            for (src, dst) in ((L_b, N_b), (qk_b, qkT)):
                for h in range(NH):
                    P.tr(ptd[:, h, :], src[:, h, :], self.ident_b[:])
                self.evac(dst[:], ptd[:], d)
                yield
            P.tt(Ld[:], L_b[:], mb(self.BD8), ALU.mult)
            P.tt(Nd[:], N_b[:], mb(self.BD8), ALU.mult, eng="pool")
            P.tt(Tw[0][:], mb(self.ident_b), Ld[:], ALU.subtract)
            P.tt(Xw[0][:], mb(self.ident_b), Nd[:], ALU.subtract, eng="pool")
            yield
            cur = 0
            for (Pa, Qa, Pprev, Qprev) in ((P1, Q1, Ld, Nd), (P2, Q2, P1, Q1)):
                for hg in range(2):
                    hs = slice(hg * 4, (hg + 1) * 4)
                    self.evac(Pa[:, hs, :], v4(mm4(Qprev, Pprev, hg)), 1)
                    self.evac(Qa[:, hs, :], v4(mm4(Pprev, Qprev, hg)), 1)
                yield
                nxt = 1 - cur
                for hg in range(2):
                    hs = slice(hg * 4, (hg + 1) * 4)
                    P.tt(Tw[nxt][:, hs, :], Tw[cur][:, hs, :], v4(mm4(Qa, Tw[cur], hg)), ALU.add)
                    P.tt(Xw[nxt][:, hs, :], Xw[cur][:, hs, :], v4(mm4(Pa, Xw[cur], hg)), ALU.add)
                yield
                cur = nxt
            Cs, CTs, Zb, Zpb = Ld, Nd, P1, Q1
            for sz in (8, 16, 32, 64):
                P.tt(Cs[:], L_b[:], mb(self.MS[sz]), ALU.mult)
                P.tt(CTs[:], N_b[:], mb(self.MS[sz]), ALU.mult, eng="pool")
                for hg in range(2):
                    hs = slice(hg * 4, (hg + 1) * 4)
                    self.evac(Zb[:, hs, :], v4(mm4(Cs, Xw[cur], hg)), 1)
                    if sz < 64:
                        self.evac(Zpb[:, hs, :], v4(mm4(CTs, Tw[cur], hg)), 1)
                yield
                nxt = 1 - cur
                for hg in range(2):
                    hs = slice(hg * 4, (hg + 1) * 4)
                    P.tt(Xw[nxt][:, hs, :], Xw[cur][:, hs, :], v4(mm4(Tw[cur], Zb, hg)), ALU.subtract)
                    if sz < 64:
                        P.tt(Tw[nxt][:, hs, :], Tw[cur][:, hs, :], v4(mm4(Xw[cur], Zpb, hg)), ALU.subtract)
                yield
                cur = nxt
            TTm = Xw[cur]
            for hg in range(2):
                hs = slice(hg * 4, (hg + 1) * 4)
                self.evac(u32[:, hs, :], v4(mm4(TTm, vb, hg)), hg)
                self.evac(wT[:, hs, :], v4(mm4(kbe, TTm, hg)), hg + 1)
            yield
            for hg in range(2):
                hs = slice(hg * 4, (hg + 1) * 4)
                P.tt(vnew[:, hs, :], u32[:, hs, :], v4(mm4(wT, Sb, hg)), ALU.subtract)
                yield
                if kind == "l":
                    ps2 = nextpf()
                    for hh in range(4):
                        h = hg * 4 + hh
                        o_ = ps2[:, hh * 128:(hh + 1) * 128]
                        P.mm(o_, qdT[:, h, :], Sb[:, h, :], start=True, stop=False)
                        P.mm(o_, qkT[:, h, :], vnew[:, h, :], start=False, stop=True)
                    self.evac(o32[:, hs, :], v4(ps2), hg)
                ps3 = mm4(kdec, vnew, hg)
                P.tt(Stmp[:, hs, :], S32[:, hs, :], bc(egl[:, hs]), ALU.mult, eng="pool")
                P.tt(S32[:, hs, :], Stmp[:, hs, :], v4(ps3), ALU.add)
                P.copy(Sb[:, hs, :], S32[:, hs, :], eng="act")
                yield
            if kind == "l":
                P.dma("pool", OUT[i * 128:(i + 1) * 128, :], o32[:].rearrange("p h e -> p (h e)"))
            yield

    def phase_finish(self):
        nc, P = self.nc, self.P
        CT = self.CTXL
        with self.scope() as es:
            sb = lambda n, s, dt=F32: self.sb(es, n, s, dt)
            onw = self.bcast_load(es, "onw", self.onorm, HD)
            wo = self.load_cast(es, "wouta", self.w_out_a, D)
            Gm = self.load_mod(es, "Gm", 2)
            of_ = [sb("of_", [128, NH, HD]) for _ in range(2)]
            ob_ = [sb("ob_", [128, NH, HD]) for _ in range(2)]
            zt_ = [sb("zt", [128, D]) for _ in range(2)]
            xt_ = [sb("xt", [128, D]) for _ in range(2)]
            osq_ = [sb("osq", [128, NH, HD]) for _ in range(2)]
            orn_ = [sb("orn", [128, 4 * NH]) for _ in range(2)]
            yg_ = [sb("yg", [128, D], BF16) for _ in range(2)]
            ygT = [sb("ygT", [128, 8, 128], BF16) for _ in range(2)]
            x1t = [sb("x1t", [128, D]) for _ in range(2)]
            bc = lambda ap: ap.unsqueeze(2).to_broadcast([128, ap.shape[1], HD])

            def loads(i):
                if i >= self.NT:
                    return
                sl = i % 2
                P.dma("sp", of_[sl][:].rearrange("p h e -> p (h e)"), self.OF[i * 128:(i + 1) * 128, :])
                P.dma("sp", ob_[sl][:].rearrange("p h e -> p (h e)"), self.OB[i * 128:(i + 1) * 128, :])
                P.dma("sp", zt_[sl][:], self.P0[CT + i * 128:CT + (i + 1) * 128, 3 * D:4 * D])
                P.dma("sp", xt_[sl][:], self.x[i * 128:(i + 1) * 128, :])

            loads(0)
            loads(1)

            def fbody(i):
                sl = i % 2
                o32, ob, zt, xt = of_[sl], ob_[sl], zt_[sl], xt_[sl]
                osq, orn, yg = osq_[sl], orn_[sl], yg_[sl]
                P.tt(o32[:], o32[:], ob[:], ALU.add)
                yield
                P.act(osq[:], o32[:], AF.Square)
                P.red(orn[:, 0:8], osq[:], ALU.add)
                P.ts(orn[:, 8:16], orn[:, 0:8], 1.0 / HD, ALU.mult, EPS, ALU.add)
                P.act(orn[:, 16:24], orn[:, 8:16], AF.Sqrt)
                P.recip(orn[:, 24:32], orn[:, 16:24])
                yield
                P.tt(osq[:], o32[:], bc(orn[:, 24:32]), ALU.mult)
                P.tt(osq[:], osq[:], onw[:].unsqueeze(1).to_broadcast([128, NH, HD]), ALU.mult)
                P.act(zt[:], zt[:], AF.Silu)
                P.tt(yg[:], osq[:].rearrange("p h e -> p (h e)"), zt[:], ALU.mult)
                yield
                self.transpose8(ygT[sl], yg, self.pt[sl], i)
                yield
                x1 = x1t[sl]
                for half in range(2):
                    ps = self.pf[(2 * i + half) % 6]
                    for kc in range(8):
                        P.mm(ps[:], ygT[sl][:, kc, :], wo[:, kc, half * 512:(half + 1) * 512], start=(kc == 0), stop=(kc == 7))
                    cs = slice(half * 512, (half + 1) * 512)
                    P.tt(x1[:, cs], ps[:], Gm[:, cs], ALU.mult)
                    P.tt(x1[:, cs], x1[:, cs], xt[:, cs], ALU.add)
                loads(i + 2)
                yield
                P.dma("pool", self.X1[i * 128:(i + 1) * 128, :], x1[:], w=[("x1", i)])
                yield
            self.run2(self.NT, fbody)

    def phase_moe(self, layer, XIN, XOUT, final):
        nc, P = self.nc, self.P
        NT, NB = self.NT, self.NBLK
        xin_key = "x1" if layer == 0 else "x3"
        with self.scope() as es:
            sb = lambda n, s, dt=F32: self.sb(es, n, s, dt)
            A = self.load_mod(es, "Af", 6 * layer + 3)
            B = self.load_mod(es, "Bf", 6 * layer + 4)
            G = self.load_mod(es, "Gf", 6 * layer + 5)
            rt = sb("rt", [128, 8, 36])
            P.dma("sp", rt[:], self.router[layer].rearrange("(kc p) n -> p kc n", p=128))
            E1 = sb("E1", [128, NT]); E2 = sb("E2", [128, NT])
            R1 = sb("R1", [128, NT]); R2 = sb("R2", [128, NT])
            W1 = sb("W1", [128, NT]); W2 = sb("W2", [128, NT])
            carry = sb("carry", [128, NE])
            P.memset(carry[:], 0.0)
            iotaE = sb("iotaE", [128, NE])
            P.copy(iotaE[:], self.coli[:, 0:NE])
            with self.scope() as e1:
                s1 = lambda n, s, dt=F32: self.sb(e1, n, s, dt)
                xt = [s1("xt", [128, D]) for _ in range(2)]
                bufs = []
                for _ in range(2):
                    bufs.append(dict(
                        sq=s1("sq", [128, D]), ss=s1("ss", [128, 4]), hf=s1("hf", [128, D]), hb=s1("hb", [128, D], BF16),
                        hT=s1("hT", [128, 8, 128]), lg=s1("lg", [128, 36]), sm=s1("sm", [128, 64]),
                        lem=s1("lem", [128, NE]), top8=s1("top8", [128, 8]), m1=s1("m1", [128, NE]),
                        m12=s1("m12", [128, NE]), m2=s1("m2", [128, NE]), m12b=s1("m12b", [128, NE], BF16),
                        pos=s1("pos", [128, NE]), tmp=s1("tmp", [128, NE])))

                def rt_body(i):
                    bb = bufs[i % 2]
                    sq, ss, hf, hb, hT, lg, sm = bb["sq"], bb["ss"], bb["hf"], bb["hb"], bb["hT"], bb["lg"], bb["sm"]
                    lem, top8, m1, m12, m2, m12b, pos, tmp = bb["lem"], bb["top8"], bb["m1"], bb["m12"], bb["m2"], bb["m12b"], bb["pos"], bb["tmp"]
                    x_ = xt[i % 2]
                    P.dma("sp", x_[:], XIN[i * 128:(i + 1) * 128, :], r=[(xin_key, i)])
                    self.modulate(x_, A, B, sq, ss, hf, None)
                    P.copy(hb[:], hf[:], eng="act")
                    P.dma("pool", self.HF[i * 128:(i + 1) * 128, :], hb[:], w=[("hf", i)])
                    yield
                    for half in range(2):
                        ps = self.pf[half]
                        for kk in range(4):
                            kc = half * 4 + kk
                            P.tr(ps[:, kk * 128:(kk + 1) * 128], hf[:, kc * 128:(kc + 1) * 128], self.ident_f[:])
                        self.evac(hT[:, half * 4:(half + 1) * 4, :], ps[:].rearrange("p (k e) -> p k e", e=128), half)
                    yield
                    ps = self.pf[2 + 2 * (i % 2)]
                    for kc in range(8):
                        P.mm(ps[:, 0:36], hT[:, kc, :], rt[:, kc, :], start=(kc == 0), stop=(kc == 7))
                    P.copy(lg[:], ps[:, 0:36])
                    yield
                    P.red(sm[:, 0:1], lg[:, 0:4], ALU.max)
                    P.ts(sm[:, 1:2], sm[:, 0:1], -1.0, ALU.mult)
                    P.act(sm[:, 4:8], lg[:, 0:4], AF.Exp, bias=sm[:, 1:2])
                    P.red(sm[:, 2:3], sm[:, 4:8], ALU.add)
                    P.recip(sm[:, 3:4], sm[:, 2:3])
                    P.ts(sm[:, 8:12], lg[:, 0:4], sm[:, 0:1], ALU.is_ge)
                    P.ts(sm[:, 12:16], sm[:, 8:12], BIG, ALU.mult, -BIG, ALU.add)
                    P.tt(lem[:].rearrange("p (g e) -> p g e", e=8), lg[:, 4:36].rearrange("p (g e) -> p g e", e=8),
                         sm[:, 12:16].unsqueeze(2).to_broadcast([128, 4, 8]), ALU.add)
                    yield
                    P.op("dve", lambda e, o=top8, i_=lem: e.max(out=o[:], in_=i_[:]), r=[lem], w=[top8])
                    P.ts(m1[:], lem[:], top8[:, 0:1], ALU.is_ge)
                    P.ts(m12[:], lem[:], top8[:, 1:2], ALU.is_ge)
                    P.tt(m2[:], m12[:], m1[:], ALU.subtract)
                    P.copy(m12b[:], m12[:], eng="act")
                    yield
                    P.tt(sm[:, 16:17], top8[:, 0:1], top8[:, 1:2], ALU.subtract)
                    P.act(sm[:, 17:18], sm[:, 16:17], AF.Sigmoid)
                    P.tt(W1[:, i:i + 1], sm[:, 17:18], sm[:, 3:4], ALU.mult)
                    P.tt(W2[:, i:i + 1], sm[:, 3:4], W1[:, i:i + 1], ALU.subtract)
                    yield
                    ps = self.pf[3 + 2 * (i % 2)]
                    P.mm(ps[:, 0:NE], self.Us_b[:], m12b[:])
                    P.mm(ps[:, NE:2 * NE], self.ones_b[:], m12b[:])
                    P.tt(pos[:], ps[:, 0:NE], carry[:], ALU.add)
                    P.tt(carry[:], carry[:], ps[:, NE:2 * NE], ALU.add)
                    for (mk, Ed, Rd) in ((m1, E1, R1), (m2, E2, R2)):
                        P.tt(tmp[:], mk[:], iotaE[:], ALU.mult)
                        P.red(Ed[:, i:i + 1], tmp[:], ALU.add)
                        P.tt(tmp[:], mk[:], pos[:], ALU.mult)
                        P.red(Rd[:, i:i + 1], tmp[:], ALU.add)
                    yield
                self.run2(NT, rt_body)
            D1i = sb("D1i", [128, NT], I32); D2i = sb("D2i", [128, NT], I32)
            WIi = sb("WIi", [128, NB], I32)
            with self.scope() as e2:
                s2 = lambda n, s, dt=F32: self.sb(e2, n, s, dt)
                NTH = 2 * NT + 1
                thr = s2("thr", [128, NTH])
                P.op("pool", lambda e: e.iota(thr[:], pattern=[[128, NTH]], base=0, channel_multiplier=0,
                                               allow_small_or_imprecise_dtypes=True), w=[thr])
                cmp = s2("cmp", [128, NE, NTH])
                P.tt(cmp[:], carry[:].unsqueeze(2).to_broadcast([128, NE, NTH]),
                     thr[:].unsqueeze(1).to_broadcast([128, NE, NTH]), ALU.is_gt)
                padded = s2("padded", [128, NE])
                P.red(padded[:], cmp[:], ALU.add)
                P.ts(padded[:], padded[:], 128.0, ALU.mult)
                le = s2("le", [128, NE, NE])
                P.tt(le[:], self.coli[:, 0:NE].unsqueeze(2).to_broadcast([128, NE, NE]),
                     self.coli[:, 0:NE].unsqueeze(1).to_broadcast([128, NE, NE]), ALU.is_ge)
                P.tt(le[:], le[:], padded[:].unsqueeze(1).to_broadcast([128, NE, NE]), ALU.mult)
                ends = s2("ends", [128, NE]); starts = s2("starts", [128, NE])
                P.red(ends[:], le[:], ALU.add)
                P.tt(starts[:], ends[:], padded[:], ALU.subtract)
                oh = s2("oh", [128, NT, NE])
                dd = s2("dd", [128, NT])
                for (Ed, Rd, Di) in ((E1, R1, D1i), (E2, R2, D2i)):
                    P.tt(oh[:], Ed[:].unsqueeze(2).to_broadcast([128, NT, NE]),
                         iotaE[:].unsqueeze(1).to_broadcast([128, NT, NE]), ALU.is_equal)
                    P.tt(oh[:], oh[:], starts[:].unsqueeze(1).to_broadcast([128, NT, NE]), ALU.mult)
                    P.red(dd[:], oh[:], ALU.add)
                    P.tt(dd[:], dd[:], Rd[:], ALU.add)
                    P.copy(Di[:], dd[:])
                bthr = s2("bthr", [128, NB])
                P.op("pool", lambda e: e.iota(bthr[:], pattern=[[128, NB]], base=0, channel_multiplier=0,
                                               allow_small_or_imprecise_dtypes=True), w=[bthr])
                cb = s2("cb", [128, NB, NE])
                P.tt(cb[:], ends[:].unsqueeze(1).to_broadcast([128, NB, NE]),
                     bthr[:].unsqueeze(2).to_broadcast([128, NB, NE]), ALU.is_le)
                be = s2("be", [128, NB + 2])
                P.memset(be[:, 0:2], -1.0)
                P.red(be[:, 2:NB + 2], cb[:], ALU.add)
                P.ts(be[:, 2:NB + 2], be[:, 2:NB + 2], float(NE - 1), ALU.min)
                need = s2("need", [128, NB])
                P.tt(need[:], be[:, 2:NB + 2], be[:, 1:NB + 1], ALU.not_equal)
                P.memset(need[:, NB // 2:NB // 2 + 1], 1.0)
                wi = s2("wi", [128, NB])
                P.ts(wi[:], be[:, 2:NB + 2], 128.0, ALU.mult, self.rowi[:, 0:1], ALU.add)
                P.ts(wi[:], wi[:], -1.0e6, ALU.add)
                P.tt(wi[:], wi[:], need[:], ALU.mult)
                P.ts(wi[:], wi[:], 1.0e6, ALU.add)
                P.copy(WIi[:], wi[:])
            with self.scope() as e3:
                s3 = lambda n, s, dt=F32: self.sb(e3, n, s, dt)
                hbt = [s3("hbt", [128, D], BF16) for _ in range(2)]
                for i in range(NT):
                    h_ = hbt[i % 2]
                    P.dma("sp", h_[:], self.HF[i * 128:(i + 1) * 128, :], r=[("hf", i)])
                    for Di in (D1i, D2i):
                        def sc(e, Di=Di, h_=h_, i=i):
                            if "bx" not in self.regcache:
                                self.regcache["bx"] = e.to_reg(self.NBLK * 128 - 1)
                            return e.indirect_dma_start(
                                out=self.XB[:, :], out_offset=bass.IndirectOffsetOnAxis(ap=Di[:, i:i + 1], axis=0),
                                in_=h_[:, :], in_offset=None, bounds_check=self.regcache["bx"], oob_is_err=False)
                        P.op("pool", sc, r=[h_, Di], w=[], dma=True)
            with self.scope() as e4:
                s4 = lambda n, s, dt=F32: self.sb(e4, n, s, dt)
                stg = [s4("stg", [128, 3 * 4096]) for _ in range(2)]
                w1b_ = [s4("w1b", [128, 8, DE], BF16) for _ in range(2)]
                w3b_ = [s4("w3b", [128, 8, DE], BF16) for _ in range(2)]
                w2b_ = [s4("w2b", [128, 4, D], BF16) for _ in range(2)]
                xbt = [s4("xbt", [128, D], BF16) for _ in range(2)]
                xT_ = [s4("xT", [128, 8, 128], BF16) for _ in range(2)]
                h1_ = [s4("h1", [128, DE]) for _ in range(2)]
                gb_ = [s4("gb", [128, DE], BF16) for _ in range(2)]
                gT_ = [s4("gT", [128, 4, 128], BF16) for _ in range(2)]
                yo = [s4("yo", [128, D]) for _ in range(2)]
                HB = NB // 2
                lane = lambda b: 0 if b < HB else 1

                def gather(b, ln):
                    if b >= NB or lane(b) != ln:
                        return
                    sl = ln

                    def gw(e, b=b, sl=sl):
                        if "bc" not in self.regcache:
                            self.regcache["bc"] = e.to_reg(NE * 128 - 1)
                        return e.indirect_dma_start(
                            out=stg[sl][:, :], out_offset=None, in_=self.wall[layer][:, :],
                            in_offset=bass.IndirectOffsetOnAxis(ap=WIi[:, b:b + 1], axis=0),
                            bounds_check=self.regcache["bc"], oob_is_err=False)
                    P.op("pool", gw, r=[WIi], w=[stg[sl]], dma=True)

                def xload(b, ln):
                    if b >= NB or lane(b) != ln:
                        return
                    P.dma("sp", xbt[ln][:], self.XB[b * 128:(b + 1) * 128, :], r=[], w=[xbt[ln]])

                gather(0, 0)
                gather(HB, 1)
                xload(0, 0)
                xload(HB, 1)
                def blk(b):
                    sl = lane(b)
                    w1b, w3b, w2b, xT, h1, gb, gT = w1b_[sl], w3b_[sl], w2b_[sl], xT_[sl], h1_[sl], gb_[sl], gT_[sl]
                    w1f = w1b[:].rearrange("p k n -> p (k n)")
                    w3f = w3b[:].rearrange("p k n -> p (k n)")
                    w2f = w2b[:].rearrange("p k n -> p (k n)")
                    for q in range(4):
                        cq = slice(q * 1024, (q + 1) * 1024)
                        P.copy(w1f[:, cq], stg[sl][:, q * 1024:(q + 1) * 1024], eng="dve")
                        P.copy(w3f[:, cq], stg[sl][:, 4096 + q * 1024:4096 + (q + 1) * 1024], eng="act")
                        P.copy(w2f[:, cq], stg[sl][:, 8192 + q * 1024:8192 + (q + 1) * 1024], eng="act" if q < 2 else "dve")
                        yield
                    gather(b + 1, sl)
                    yield
                    self.transpose8(xT, xbt[sl], self.pt[sl], b)
                    xload(b + 1, sl)
                    yield
                    p1, p3 = self.pf[2 * sl], self.pf[2 * sl + 1]
                    for kc in range(8):
                        P.mm(p1[:], xT[:, kc, :], w1b[:, kc, :], start=(kc == 0), stop=(kc == 7))
                    for kc in range(8):
                        P.mm(p3[:], xT[:, kc, :], w3b[:, kc, :], start=(kc == 0), stop=(kc == 7))
                    yield
                    P.act(h1[:], p1[:], AF.Silu)
                    P.tt(gb[:], h1[:], p3[:], ALU.mult)
                    yield
                    pt_ = self.pt[1 - sl]
                    for fc in range(4):
                        P.tr(pt_[:, fc, :], gb[:, fc * 128:(fc + 1) * 128], self.ident_b[:])
                    self.evac(gT[:], pt_[:, 0:4, :], b + 1)
                    yield
                    y_ = yo[sl]
                    for half in range(2):
                        ps = self.pf[4 + half]
                        for fc in range(4):
                            P.mm(ps[:], gT[:, fc, :], w2b[:, fc, half * 512:(half + 1) * 512], start=(fc == 0), stop=(fc == 3))
                        self.evac(y_[:, half * 512:(half + 1) * 512], ps[:], half)
                    yield
                    P.dma("pool", self.YB[b * 128:(b + 1) * 128, :], y_[:], w=["yb_all"])
                    yield
                def lane_stream(ln):
                    for b in (range(0, HB) if ln == 0 else range(HB, NB)):
                        yield from blk(b)
                gens = [lane_stream(0), lane_stream(1)]
                for _ in range(6):
                    next(gens[0])
                while gens:
                    for g_ in list(gens):
                        try:
                            next(g_)
                        except StopIteration:
                            gens.remove(g_)
            with self.scope() as e5:
                s5 = lambda n, s, dt=F32: self.sb(e5, n, s, dt)
                y1 = [s5("y1", [128, D]) for _ in range(2)]
                y2 = [s5("y2", [128, D]) for _ in range(2)]
                xt = [s5("xt", [128, D]) for _ in range(2)]
                acc_ = [s5("acc", [128, D]) for _ in range(2)]
                xo_ = [s5("xo", [128, D]) for _ in range(2)]
                if final:
                    fn = self.bcast_load(e5, "fn", self.final_norm, D)
                    sq_ = [s5("sq", [128, D]) for _ in range(2)]; ss_ = [s5("ss", [128, 4]) for _ in range(2)]
                def cload(i):
                    if i >= NT:
                        return
                    sl = i % 2
                    for (yy, Di) in ((y1, D1i), (y2, D2i)):
                        def gy(e, yy=yy, Di=Di, i=i, sl=sl):
                            if "bx" not in self.regcache:
                                self.regcache["bx"] = e.to_reg(self.NBLK * 128 - 1)
                            return e.indirect_dma_start(
                                out=yy[sl][:, :], out_offset=None, in_=self.YB[:, :],
                                in_offset=bass.IndirectOffsetOnAxis(ap=Di[:, i:i + 1], axis=0),
                                bounds_check=self.regcache["bx"], oob_is_err=False)
                        P.op("pool", gy, r=[Di], w=[yy[sl]], dma=True)
                    P.dma("sp", xt[sl][:], XIN[i * 128:(i + 1) * 128, :], r=[(xin_key, i)])

                cload(0)
                cload(1)

                def cmb(i):
                    sl = i % 2
                    acc = acc_[sl]
                    if final:
                        sq, ss = sq_[sl], ss_[sl]
                    P.ts(acc[:], y1[sl][:], W1[:, i:i + 1], ALU.mult)
                    P.stt(acc[:], y2[sl][:], W2[:, i:i + 1], acc[:], ALU.mult, ALU.add)
                    P.tt(acc[:], acc[:], G[:], ALU.mult)
                    yield
                    xo = xo_[sl]
                    P.tt(xo[:], acc[:], xt[sl][:], ALU.add)
                    cload(i + 2)
                    yield
                    if not final:
                        P.dma("act", XOUT[i * 128:(i + 1) * 128, :], xo[:], w=[("x2", i)])
                    else:
                        P.act(sq[:], xo[:], AF.Square)
                        P.red(ss[:, 0:1], sq[:], ALU.add)
                        P.ts(ss[:, 1:2], ss[:, 0:1], 1.0 / D, ALU.mult, EPS, ALU.add)
                        P.act(ss[:, 2:3], ss[:, 1:2], AF.Sqrt)
                        P.recip(ss[:, 3:4], ss[:, 2:3])
                        P.stt(sq[:], xo[:], ss[:, 3:4], fn[:], ALU.mult, ALU.mult)
                        P.dma("act", XOUT[i * 128:(i + 1) * 128, :], sq[:], w=[("out", i)])
                    yield
                self.run2(NT, cmb)

    def phase_convD(self):
        nc, P = self.nc, self.P
        with self.scope() as es:
            sb = lambda n, s, dt=F32: self.sb(es, n, s, dt)
            wb = self.load_cast(es, "winb", self.w_in_b, 3 * D)
            A = self.load_mod(es, "A1", 6)
            B = self.load_mod(es, "B1", 7)
            xt = [sb("xt", [128, D]) for _ in range(2)]
            sq_ = [sb("sq", [128, D]) for _ in range(2)]; ss_ = [sb("ss", [128, 4]) for _ in range(2)]
            hf_ = [sb("hf", [128, D]) for _ in range(2)]; hb_ = [sb("hb", [128, D], BF16) for _ in range(2)]
            hT = [sb("hT", [128, 8, 128], BF16) for _ in range(2)]
            gbt = [sb("gbt", [128, D]) for _ in range(2)]
            gcc_ = [sb("gc_", [128, D]) for _ in range(2)]
            ut = [sb("ut", [128, D]) for _ in range(2)]
            def dbody(i):
                x_ = xt[i % 2]
                sq, ss, hf, hb, gc_ = sq_[i % 2], ss_[i % 2], hf_[i % 2], hb_[i % 2], gcc_[i % 2]
                P.dma("sp", x_[:], self.X2[i * 128:(i + 1) * 128, :], r=[("x2", i)])
                self.modulate(x_, A, B, sq, ss, hf, hb)
                yield
                hT_ = hT[i % 2]
                self.transpose8(hT_, hb, self.pt[i % 2], i)
                yield
                g_, u_ = gbt[i % 2], ut[i % 2]
                for g in range(6):
                    ps = self.pf[(g + 3 * (i % 2)) % 6]
                    for kc in range(8):
                        P.mm(ps[:], hT_[:, kc, :], wb[:, kc, g * 512:(g + 1) * 512], start=(kc == 0), stop=(kc == 7))
                    cs = slice((g % 2) * 512, (g % 2 + 1) * 512)
                    if g < 2:
                        self.evac(g_[:, cs], ps[:], g)
                    elif g < 4:
                        self.evac(gc_[:, cs], ps[:], g)
                    else:
                        P.tt(u_[:, cs], ps[:], gc_[:, cs], ALU.mult)
                    if g % 2 == 1:
                        yield
                P.dma("pool", self.GB[i * 128:(i + 1) * 128, :], g_[:], w=[("gb", i)])
                P.dma("pool", self.U1[i * 128:(i + 1) * 128, :], u_[:], w=[("u1", i)])
                yield
            self.run2(self.NT, dbody)

    def phase_convE(self):
        nc, P = self.nc, self.P
        NT = self.NT
        H = D // 2
        with self.scope() as es:
            sb = lambda n, s, dt=F32: self.sb(es, n, s, dt)
            wo = self.load_cast(es, "woutb", self.w_out_b, D)
            cb = [self.bcast_load(es, "cb%d" % s, self.conv_b[s], D) for s in range(3)]
            Gm = self.load_mod(es, "Gm1", 8)
            mm_ = sb("mm_", [128, 4])
            P.ts(mm_[:, 0:1], self.rowi[:, 0:1], 64.0, ALU.is_equal)
            P.ts(mm_[:, 1:2], self.rowi[:, 0:1], 0.0, ALU.is_equal)
            P.tt(mm_[:, 0:1], mm_[:, 0:1], mm_[:, 1:2], ALU.add)
            P.ts(mm_[:, 0:1], mm_[:, 0:1], -1.0, ALU.mult, 1.0, ALU.add)
            P.ts(mm_[:, 2:3], self.rowi[:, 0:1], 63.0, ALU.is_equal)
            P.ts(mm_[:, 3:4], self.rowi[:, 0:1], 127.0, ALU.is_equal)
            P.tt(mm_[:, 2:3], mm_[:, 2:3], mm_[:, 3:4], ALU.add)
            P.ts(mm_[:, 2:3], mm_[:, 2:3], -1.0, ALU.mult, 1.0, ALU.add)
            um = [sb("um", [128, D]) for _ in range(2)]
            up = [sb("up", [128, D]) for _ in range(2)]
            uc = [sb("uc", [128, D]) for _ in range(2)]
            gbt = [sb("gbt", [128, D]) for _ in range(2)]
            xt = [sb("xt", [128, D]) for _ in range(2)]
            yc_ = [sb("yc", [128, D]) for _ in range(2)]
            vbb_ = [sb("vb_", [128, D], BF16) for _ in range(2)]
            vT_ = [sb("vT", [128, 8, 128], BF16) for _ in range(2)]
            x3t = [sb("x3t", [128, D]) for _ in range(2)]
            for t in um + up:
                P.memset(t[:], 0.0, eng="pool")
            def ebody(i):
                sl = i % 2
                r0 = i * 128
                yc, vb_, vT = yc_[sl], vbb_[sl], vT_[sl]
                um_, up_, uc_ = um[sl], up[sl], uc[sl]
                P.dma("sp", uc_[:], self.U1[r0:r0 + 128, :], r=[("u1", i)])
                P.dma("sp", gbt[sl][:], self.GB[r0:r0 + 128, :], r=[("gb", i)])
                P.dma("sp", xt[sl][:], self.X2[r0:r0 + 128, :], r=[("x2", i)])
                if i == 0:
                    P.dma("sp", um_[1:128, 0:H], self.U1[0:127, 0:H], r=[("u1", 0)], w=[um_])
                else:
                    P.dma("sp", um_[:, 0:H], self.U1[r0 - 1:r0 + 127, 0:H], r=[("u1", i), ("u1", i - 1)], w=[um_])
                if i == NT - 1:
                    P.dma("sp", up_[0:127, 0:H], self.U1[r0 + 1:r0 + 128, 0:H], r=[("u1", i)], w=[up_])
                else:
                    P.dma("sp", up_[:, 0:H], self.U1[r0 + 1:r0 + 129, 0:H], r=[("u1", i), ("u1", i + 1)], w=[up_])
                if i == 0:
                    P.memset(um_[0:64, H:D], 0.0, eng="pool")
                    P.dma("sp", um_[64:128, H:D], self.U1[0:64, H:D], r=[("u1", 0)], w=[um_])
                else:
                    P.dma("sp", um_[:, H:D], self.U1[r0 - 64:r0 + 64, H:D], r=[("u1", i), ("u1", i - 1)], w=[um_])
                if i == NT - 1:
                    P.memset(up_[64:128, H:D], 0.0, eng="pool")
                    P.dma("sp", up_[0:64, H:D], self.U1[r0 + 64:r0 + 128, H:D], r=[("u1", i)], w=[up_])
                else:
                    P.dma("sp", up_[:, H:D], self.U1[r0 + 64:r0 + 192, H:D], r=[("u1", i), ("u1", i + 1)], w=[up_])
                yield
                P.ts(um_[:, 0:H], um_[:, 0:H], mm_[:, 0:1], ALU.mult)
                P.ts(up_[:, 0:H], up_[:, 0:H], mm_[:, 2:3], ALU.mult, eng="pool")
                P.tt(um_[:], um_[:], cb[0][:], ALU.mult)
                P.tt(up_[:], up_[:], cb[2][:], ALU.mult)
                yield
                P.tt(yc[:], uc_[:], cb[1][:], ALU.mult)
                P.tt(yc[:], yc[:], um_[:], ALU.add)
                P.tt(yc[:], yc[:], up_[:], ALU.add)
                P.tt(vb_[:], yc[:], gbt[sl][:], ALU.mult)
                yield
                self.transpose8(vT, vb_, self.pt[sl], i)
                yield
                x3 = x3t[sl]
                for half in range(2):
                    ps = self.pf[half + 2 * sl]
                    for kc in range(8):
                        P.mm(ps[:], vT[:, kc, :], wo[:, kc, half * 512:(half + 1) * 512], start=(kc == 0), stop=(kc == 7))
                    cs = slice(half * 512, (half + 1) * 512)
                    P.tt(x3[:, cs], ps[:], Gm[:, cs], ALU.mult)
                    P.tt(x3[:, cs], x3[:, cs], xt[sl][:, cs], ALU.add)
                P.dma("pool", self.X3[r0:r0 + 128, :], x3[:], w=[("x3", i)])
                yield
            self.run2(NT, ebody)


def make_in_maps(inputs, S, CTXL, n_cores, phases=99):
    f = lambda a: np.ascontiguousarray(np.asarray(a, dtype=np.float32))
    moe = {}
    if phases >= 4:
        w1 = f(inputs["w1"]); w3 = f(inputs["w3"]); w2 = f(inputs["w2"])
        for l in range(2):
            wall = np.empty((NE, 128, 3, 4096), np.float32)
            wall[:, :, 0, :] = w1[l].reshape(NE, 8, 128, DE).transpose(0, 2, 1, 3).reshape(NE, 128, 4096)
            wall[:, :, 1, :] = w3[l].reshape(NE, 8, 128, DE).transpose(0, 2, 1, 3).reshape(NE, 128, 4096)
            wall[:, :, 2, :] = w2[l].reshape(NE, 4, 128, D).transpose(0, 2, 1, 3).reshape(NE, 128, 4096)
            moe["wall%d" % l] = wall.reshape(NE * 128, 3 * 4096)
    router = np.ascontiguousarray(np.concatenate([f(inputs["router_g"]), f(inputs["router_e"])], axis=-1))
    shared = {
        "c_ctx": f(inputs["c_ctx"]), "ada_w": f(inputs["ada_w"]), "ada_b": f(inputs["ada_b"]),
        "norm_mix": f(inputs["norm_mix"]), "norm_ffn": f(inputs["norm_ffn"]),
        "w_in_a": f(inputs["w_in_a"])[0], "conv_a": f(inputs["conv_a"])[0], "a_log": f(inputs["a_log_a"])[0],
        "dt_bias": f(inputs["dt_bias_a"])[0], "onorm": f(inputs["onorm_a"])[0], "w_out_a": f(inputs["w_out_a"])[0],
        "w_in_b": f(inputs["w_in_b"])[0], "conv_b": f(inputs["conv_b"])[0], "w_out_b": f(inputs["w_out_b"])[0],
        "router": router, "final_norm": f(inputs["final_norm"]),
    }
    shared.update(moe)
    x = f(inputs["x"]); c = f(inputs["c"]); ctx = f(inputs["ctx"])
    maps = []
    for b in range(n_cores):
        m = dict(shared)
        m["x"] = np.ascontiguousarray(x[b, :S])
        m["c"] = np.ascontiguousarray(c[b])
        m["ctx"] = np.ascontiguousarray(ctx[b, :CTXL])
        maps.append(m)
    return maps


def run(inputs, S, CTXL, n_cores, phases=99, debug=(), trace=False):
    bld = Builder(S, CTXL, phases=phases, debug=debug)
    nc = bld.build()
    import time as _t
    t0 = _t.time()
    maps = make_in_maps(inputs, S, CTXL, n_cores, phases)
    print("in_maps", _t.time() - t0, flush=True)
    res = run_bass_kernel_spmd(nc, maps, core_ids=list(range(n_cores)))
    print("spmd", _t.time() - t0, flush=True)
    return res, bld


def kernel(**inputs):
    res, _ = run(inputs, 8192, 256, 8)
    return np.stack([np.asarray(r["out"], dtype=np.float32) for r in res.results], axis=0)
```

```python
import numpy as np
from contextlib import ExitStack, contextmanager
import concourse.bass as bass
import concourse.mybir as mybir
from concourse.bass_utils import run_bass_kernel_spmd

F32 = mybir.dt.float32
BF16 = mybir.dt.bfloat16
I32 = mybir.dt.int32
AF = mybir.ActivationFunctionType
ALU = mybir.AluOpType
AX = mybir.AxisListType

D = 1024
NH = 8
HD = 128
NE = 32
DE = 512
EPS = 1e-6
BIG = 30000.0
GRID_W = 64


class Op:
    __slots__ = ("eng", "fn", "deps", "need", "sem", "val", "isdma", "idx")


class Prog:
    def __init__(self, nc, ndma=32):
        self.nc = nc
        self.es = ExitStack()
        self.engs = ["pe", "dve", "act", "pool", "sp"]
        self.q = {k: [] for k in self.engs}
        self.esem = {k: self.es.enter_context(nc.semaphore("s_" + k)) for k in self.engs}
        self.dsem = [self.es.enter_context(nc.semaphore("d%d" % i)) for i in range(ndma)]
        self.dlast = [None] * ndma
        self.dcount = [0] * ndma
        self.dnext = 0
        self.lastw = {}
        self.readers = {}
        self.nops = 0

    @staticmethod
    def key(x):
        if isinstance(x, (str, tuple)):
            return x
        return x.name

    def op(self, eng, fn, r=(), w=(), dma=False):
        o = Op()
        o.eng = eng
        o.fn = fn
        o.need = False
        o.isdma = dma
        o.sem = None
        o.val = None
        o.idx = self.nops
        self.nops += 1
        deps = {}
        rk = [self.key(x) for x in r]
        wk = [self.key(x) for x in w]

        def add(d):
            if d is None:
                return
            if d.isdma:
                deps[("d", id(d))] = d
            else:
                if d.eng == eng and eng == "pe" and not dma:
                    return
                cur = deps.get(("e", d.eng))
                if cur is None or cur.idx < d.idx:
                    deps[("e", d.eng)] = d

        for k in rk:
            add(self.lastw.get(k))
        for k in wk:
            add(self.lastw.get(k))
            for rd in self.readers.get(k, ()):
                add(rd)
        if dma:
            i = self.dnext
            self.dnext = (self.dnext + 1) % len(self.dsem)
            add(self.dlast[i])
            self.dcount[i] += 16
            o.sem = self.dsem[i]
            o.val = self.dcount[i]
            self.dlast[i] = o
        o.deps = list(deps.values())
        for d in o.deps:
            d.need = True
        for k in rk:
            self.readers.setdefault(k, []).append(o)
        for k in wk:
            self.lastw[k] = o
            self.readers[k] = []
        self.q[eng].append(o)
        return o

    def dma(self, eng, out, in_, r=None, w=None, **kw):
        r = [in_] if r is None else r
        w = [out] if w is None else w
        return self.op(eng, lambda e: e.dma_start(out=out, in_=in_, **kw), r=r, w=w, dma=True)

    def mm(self, out, lhsT, rhs, start=True, stop=True, r=None, w=None):
        r = [lhsT, rhs] if r is None else r
        w = [out] if w is None else w
        return self.op("pe", lambda e: e.matmul(out, lhsT, rhs, start=start, stop=stop), r=r, w=w)

    def tr(self, out, in_, ident, r=None, w=None):
        r = [in_, ident] if r is None else r
        w = [out] if w is None else w
        return self.op("pe", lambda e: e.transpose(out, in_, ident), r=r, w=w)

    def act(self, out, in_, func, bias=None, scale=None, r=None, w=None):
        rr = [in_] + ([bias] if bias is not None and not isinstance(bias, (int, float)) else [])
        r = rr if r is None else r
        w = [out] if w is None else w
        kw = {}
        if bias is not None:
            kw["bias"] = bias
        if scale is not None:
            kw["scale"] = scale
        return self.op("act", lambda e: e.activation(out=out, in_=in_, func=func, **kw), r=r, w=w)

    def tt(self, out, in0, in1, op, eng="dve", r=None, w=None):
        r = [in0, in1] if r is None else r
        w = [out] if w is None else w
        return self.op(eng, lambda e: e.tensor_tensor(out=out, in0=in0, in1=in1, op=op), r=r, w=w)

    def ts(self, out, in0, s1, op0, s2=None, op1=None, eng="dve", r=None, w=None):
        rr = [in0] + [s for s in (s1, s2) if s is not None and not isinstance(s, (int, float))]
        r = rr if r is None else r
        w = [out] if w is None else w
        if op1 is None:
            return self.op(eng, lambda e: e.tensor_scalar(out=out, in0=in0, scalar1=s1, scalar2=None, op0=op0), r=r, w=w)
        return self.op(eng, lambda e: e.tensor_scalar(out=out, in0=in0, scalar1=s1, scalar2=s2, op0=op0, op1=op1), r=r, w=w)

    def stt(self, out, in0, scalar, in1, op0, op1, r=None, w=None):
        rr = [in0, in1] + ([scalar] if not isinstance(scalar, (int, float)) else [])
        r = rr if r is None else r
        w = [out] if w is None else w
        return self.op("dve", lambda e: e.scalar_tensor_tensor(out=out, in0=in0, scalar=scalar, in1=in1, op0=op0, op1=op1), r=r, w=w)

    def copy(self, out, in_, eng="dve", r=None, w=None):
        r = [in_] if r is None else r
        w = [out] if w is None else w
        if eng == "act":
            return self.op("act", lambda e: e.copy(out=out, in_=in_), r=r, w=w)
        return self.op(eng, lambda e: e.tensor_copy(out=out, in_=in_), r=r, w=w)

    def red(self, out, in_, op, axis=AX.X, r=None, w=None):
        r = [in_] if r is None else r
        w = [out] if w is None else w
        return self.op("dve", lambda e: e.tensor_reduce(out=out, in_=in_, axis=axis, op=op), r=r, w=w)

    def memset(self, ap, val, eng="dve"):
        return self.op(eng, lambda e: e.memset(ap, val), r=[], w=[ap])

    def recip(self, out, in_, r=None, w=None):
        r = [in_] if r is None else r
        w = [out] if w is None else w
        return self.op("dve", lambda e: e.reciprocal(out=out, in_=in_), r=r, w=w)

    def barrier(self):
        lasts = [d for d in self.dlast if d is not None]
        for e in self.engs:
            for o in reversed(self.q[e]):
                if not o.isdma and o.fn is not None:
                    lasts.append(o)
                    break
        for d in lasts:
            d.need = True
        for e in self.engs:
            b = Op()
            b.eng = e
            b.fn = None
            b.need = False
            b.isdma = False
            b.sem = None
            b.val = None
            b.idx = self.nops
            self.nops += 1
            b.deps = list(lasts)
            self.q[e].append(b)
        self.lastw = {}
        self.readers = {}

    def finish(self):
        fin = Op()
        fin.eng = "sp"
        fin.fn = None
        fin.need = False
        fin.isdma = False
        fin.idx = self.nops
        deps = [d for d in self.dlast if d is not None]
        for e in self.engs:
            for o in reversed(self.q[e]):
                if not o.isdma and o.fn is not None:
                    o.need = True
                    deps.append(o)
                    break
        fin.deps = deps
        self.q["sp"].append(fin)
        for e in self.engs:
            c = 0
            for o in self.q[e]:
                if o.isdma or o.fn is None:
                    continue
                if o.need:
                    c += 1
                    o.val = c
                    o.sem = self.esem[e]
        self.counts = {e: len(self.q[e]) for e in self.engs}

        def replay(name, e):
            waited = {}
            for o in self.q[name]:
                for d in o.deps:
                    sk = id(d.sem)
                    if waited.get(sk, 0) < d.val:
                        e.wait_ge(d.sem, d.val)
                        waited[sk] = d.val
                if o.fn is None:
                    continue
                ins = o.fn(e)
                if o.isdma:
                    ins.then_inc(o.sem, 16)
                elif o.need:
                    ins.then_inc(o.sem, 1)

        nc = self.nc
        with nc.Block() as block:
            @block.tensor
            def _(e):
                replay("pe", e)

            @block.vector
            def _(e):
                replay("dve", e)

            @block.scalar
            def _(e):
                replay("act", e)

            @block.gpsimd
            def _(e):
                replay("pool", e)

            @block.sync
            def _(e):
                replay("sp", e)
        self.es.close()


class Builder:
    def __init__(self, S, CTXL, phases=99, debug=()):
        self.S = S
        self.CTXL = CTXL
        self.NT = S // 128
        self.NC = CTXL // 128
        self.T = S
        self.NBLK = (2 * S) // 128 + NE
        self.phases = phases
        self.debug = set(debug)
        nc = self.nc = bass.Bass("TRN2", target_bir_lowering=False)
        self.P = Prog(nc)
        self.top = ExitStack()
        self._n = 0
        self.regcache = {}
        self.pfi = [0]

        def din(name, shape, dt=F32):
            return nc.dram_tensor(name, list(shape), dt, kind="ExternalInput").ap()

        self.x = din("x", [S, D])
        self.c = din("c", [D])
        self.ctx = din("ctx", [CTXL, D])
        self.c_ctx = din("c_ctx", [D])
        self.ada_w = din("ada_w", [2, D, 6 * D])
        self.ada_b = din("ada_b", [2, 6 * D])
        self.norm_mix = din("norm_mix", [2, D])
        self.norm_ffn = din("norm_ffn", [2, D])
        self.w_in_a = din("w_in_a", [D, 4 * D + 32])
        self.conv_a = din("conv_a", [3, 3 * D])
        self.a_log = din("a_log", [2, NH])
        self.dt_bias = din("dt_bias", [2, NH])
        self.onorm = din("onorm", [HD])
        self.w_out_a = din("w_out_a", [D, D])
        self.w_in_b = din("w_in_b", [D, 3 * D])
        self.conv_b = din("conv_b", [3, D])
        self.w_out_b = din("w_out_b", [D, D])
        self.router = din("router", [2, D, 36])
        if phases >= 4:
            self.wall = [din("wall%d" % l, [NE * 128, 3 * 4096]) for l in range(2)]
        self.final_norm = din("final_norm", [D])
        self.out = nc.dram_tensor("out", [S, D], F32, kind="ExternalOutput").ap()

        TT = S + CTXL
        self.MODP = self.scr("modp", [16, 128, D])
        self.P0 = self.scr("p0", [TT, 4 * D + 32])
        self.OF = self.scr("of", [S, D])
        self.OB = self.scr("ob", [S, D])
        self.QKV = self.scr("qkv", [TT, 3 * D], BF16)
        self.X1 = self.scr("x1", [S, D])
        self.X2 = self.scr("x2", [S, D])
        self.X3 = self.scr("x3", [S, D])
        self.HF = self.scr("hfb", [S, D], BF16)
        self.XB = self.scr("xb", [self.NBLK * 128, D], BF16)
        self.YB = self.scr("yb", [self.NBLK * 128, D])
        self.U1 = self.scr("u1", [S, D])
        self.GB = self.scr("gb", [S, D])

    @contextmanager
    def scope(self):
        with ExitStack() as es:
            yield es
            self.P.barrier()

    def run2(self, n, body, lag=0):
        def stream(par):
            for i in range(par, n, 2):
                yield from body(i)
        gens = [stream(0), stream(1)]
        for _ in range(lag):
            next(gens[0])
        while gens:
            for g in list(gens):
                try:
                    next(g)
                except StopIteration:
                    gens.remove(g)

    def scr(self, name, shape, dt=F32):
        kind = "ExternalOutput" if name in self.debug else "Internal"
        return self.nc.dram_tensor(name, list(shape), dt, kind=kind).ap()

    def sb(self, es, name, shape, dt=F32):
        self._n += 1
        return es.enter_context(self.nc.sbuf_tensor("%s_%d" % (name, self._n), list(shape), dt))

    def build(self):
        nc, P = self.nc, self.P
        top = self.top
        self.pf = [top.enter_context(nc.psum_tensor("pf%d" % i, [128, 512], F32)) for i in range(6)]
        self.pt = [top.enter_context(nc.psum_tensor("pt%d" % i, [128, 8, 128], BF16)) for i in range(2)]
        self.consts()
        self.phase_ada()
        if self.phases >= 1:
            self.phase_projA()
        if self.phases >= 2:
            self.phase_qkv()
        if self.phases >= 3:
            self.phase_delta_both()
            self.phase_finish()
        if self.phases >= 4:
            self.phase_moe(0, self.X1, self.X2, final=False)
        if self.phases >= 5:
            self.phase_convD()
        if self.phases >= 6:
            self.phase_convE()
        if self.phases >= 7:
            self.phase_moe(1, self.X3, self.out, final=True)
        P.finish()
        top.close()
        return nc

    def consts(self):
        nc, P, top = self.nc, self.P, self.top
        sb = lambda n, s, d=F32: self.sb(top, n, s, d)
        rowi = sb("rowi", [128, 128])
        coli = sb("coli", [128, 128])
        P.op("pool", lambda e: e.iota(rowi[:], pattern=[[0, 128]], base=0, channel_multiplier=1,
                                       allow_small_or_imprecise_dtypes=True), w=[rowi])
        P.op("pool", lambda e: e.iota(coli[:], pattern=[[1, 128]], base=0, channel_multiplier=0,
                                       allow_small_or_imprecise_dtypes=True), w=[coli])
        self.rowi, self.coli = rowi, coli
        self.ident_f = sb("ident_f", [128, 128])
        self.ident_b = sb("ident_b", [128, 128], BF16)
        self.ones_f = sb("ones_f", [128, 128])
        self.ones_b = sb("ones_b", [128, 128], BF16)
        P.tt(self.ident_f[:], rowi[:], coli[:], ALU.is_equal)
        P.copy(self.ident_b[:], self.ident_f[:])
        P.memset(self.ones_f[:], 1.0)
        P.memset(self.ones_b[:], 1.0)
        self.U = [sb("Uf", [128, 128]), sb("Ub", [128, 128])]
        P.tt(self.U[0][:], rowi[:], coli[:], ALU.is_le)
        P.tt(self.U[1][:], rowi[:], coli[:], ALU.is_ge)
        self.SM = [sb("SMf", [128, 128]), sb("SMb", [128, 128])]
        P.tt(self.SM[0][:], rowi[:], coli[:], ALU.is_gt)
        P.tt(self.SM[1][:], rowi[:], coli[:], ALU.is_lt)
        self.BM = [sb("BMf", [128, 4, 128]), sb("BMb", [128, 4, 128])]
        for d in range(2):
            for h in range(4):
                P.ts(self.BM[d][:, h, :], self.SM[1 - d][:], BIG, ALU.mult)
        self.Us_b = sb("Us_b", [128, 128], BF16)
        P.copy(self.Us_b[:], self.SM[1][:])
        bd = {}
        colb = sb("colb", [128, 128]); rowb = sb("rowb", [128, 128])
        for sz in (8, 16, 32, 64):
            P.op("pool", lambda e, sz=sz: e.iota(colb[:], pattern=[[1, 128 // sz], [0, sz]], base=0, channel_multiplier=0,
                                                  allow_small_or_imprecise_dtypes=True), w=[colb])
            P.tr(self.pf[0][:, 0:128], colb[:], self.ident_f[:])
            P.copy(rowb[:], self.pf[0][:, 0:128])
            bd[sz] = sb("bd%d" % sz, [128, 128])
            P.tt(bd[sz][:], rowb[:], colb[:], ALU.is_equal)
        self.BD8 = sb("BD8b", [128, 128], BF16)
        P.copy(self.BD8[:], bd[8][:])
        self.MS = {}
        for sz in (8, 16, 32, 64):
            self.MS[sz] = sb("MS%d" % sz, [128, 128], BF16)
            if sz < 64:
                P.tt(self.MS[sz][:], bd[2 * sz][:], bd[sz][:], ALU.subtract)
            else:
                P.ts(self.MS[sz][:], bd[sz][:], -1.0, ALU.mult, 1.0, ALU.add)
        self.epsc = sb("epsc", [128, 1])
        P.memset(self.epsc[:], EPS)

    def evac(self, out, in_, i):
        if i % 2 == 0:
            return self.P.copy(out, in_, eng="dve")
        return self.P.copy(out, in_, eng="act")

    def load_cast(self, es, name, src_ap, ncols, chunk=512, kparts=8, qeng="sp"):
        P = self.P
        wb = self.sb(es, name, [128, kparts, ncols], BF16)
        with self.scope() as tmp:
            st = [self.sb(tmp, name + "_st", [128, kparts, chunk]) for _ in range(2)]
            src = src_ap.rearrange("(kc p) n -> p kc n", p=128)
            i = 0
            for c0 in range(0, ncols, chunk):
                cw = min(chunk, ncols - c0)
                s = st[i % 2]
                P.dma(qeng, s[:, :, 0:cw], src[:, :, c0:c0 + cw])
                if i % 2 == 0:
                    P.copy(wb[:, :, c0:c0 + cw], s[:, :, 0:cw], eng="dve")
                else:
                    P.copy(wb[:, :, c0:c0 + cw], s[:, :, 0:cw], eng="pool")
                i += 1
        return wb

    def bcast_load(self, es, name, vec_ap, n):
        t = self.sb(es, name, [128, n])
        self.P.dma("sp", t[:], vec_ap.partition_broadcast(128))
        return t

    def modulate(self, xt, A, B, sq, ss, hf, hb):
        P = self.P
        P.act(sq[:], xt[:], AF.Square)
        P.red(ss[:, 0:1], sq[:], ALU.add)
        P.ts(ss[:, 1:2], ss[:, 0:1], 1.0 / D, ALU.mult, EPS, ALU.add)
        P.act(ss[:, 2:3], ss[:, 1:2], AF.Sqrt)
        P.recip(ss[:, 3:4], ss[:, 2:3])
        P.stt(hf[:], xt[:], ss[:, 3:4], A[:], ALU.mult, ALU.mult)
        if hb is not None:
            P.tt(hb[:], hf[:], B[:], ALU.add)
        else:
            P.tt(hf[:], hf[:], B[:], ALU.add)

    def transpose8(self, dstT, src_bf, ptile, i=0, n=8):
        P = self.P
        for kc in range(n):
            P.tr(ptile[:, kc, :], src_bf[:, kc * 128:(kc + 1) * 128], self.ident_b[:])
        self.evac(dstT[:, 0:n, :], ptile[:, 0:n, :], i)

    def phase_ada(self):
        nc, P = self.nc, self.P
        with self.scope() as es:
            sb = lambda n, s, d=F32: self.sb(es, n, s, d)
            craw = sb("craw", [128, 2, 8])
            P.dma("sp", craw[:, 0, :], self.c.rearrange("(kc p) -> p kc", p=128), allow_slow_non_contiguous=True)
            P.dma("sp", craw[:, 1, :], self.c_ctx.rearrange("(kc p) -> p kc", p=128), allow_slow_non_contiguous=True)
            csil = sb("csil", [128, 2, 8])
            P.act(csil[:], craw[:], AF.Silu)
            cb = sb("cb", [128, 2, 8, 128])
            for j in range(2):
                P.copy(cb[:, j, :, :], csil[:, j, :].unsqueeze(2).to_broadcast([128, 8, 128]))
            wst = [sb("adaw", [128, 8, 512]) for _ in range(2)]
            mb = sb("mb", [128, 6 * D])
            bb = sb("bb", [128, 6 * D])
            nm = sb("nm", [128, D])
            res = [sb("res", [128, D]) for _ in range(2)]
            cnt = 0
            for (layer, j) in ((0, 0), (0, 1), (1, 0)):
                P.dma("sp", bb[:], self.ada_b[layer].partition_broadcast(128))
                for n in range(12):
                    w = wst[cnt % 2]
                    P.dma("sp", w[:], self.ada_w[layer].rearrange("(kc p) n -> p kc n", p=128)[:, :, n * 512:(n + 1) * 512])
                    ps = self.pf[cnt % 2]
                    for kc in range(8):
                        P.mm(ps[:], cb[:, j, kc, :], w[:, kc, :], start=(kc == 0), stop=(kc == 7))
                    P.tt(mb[:, n * 512:(n + 1) * 512], ps[:], bb[:, n * 512:(n + 1) * 512], ALU.add)
                    cnt += 1
                outs = []
                for sub, (nrm, base) in enumerate(((self.norm_mix, 0), (self.norm_ffn, 3))):
                    if j == 1 and sub == 1:
                        continue
                    P.dma("sp", nm[:], nrm[layer].partition_broadcast(128))
                    r0 = res[0]
                    P.stt(r0[:], mb[:, (base + 1) * D:(base + 2) * D], 1.0, nm[:], ALU.add, ALU.mult)
                    ka = (12 if j == 1 else 6 * layer + base)
                    P.dma("sp", self.MODP[ka], r0[:], w=[("modp", ka)])
                    P.dma("sp", self.MODP[ka + 1], mb[:, base * D:(base + 1) * D], w=[("modp", ka + 1)])
                    if j == 0:
                        P.dma("sp", self.MODP[ka + 2], mb[:, (base + 2) * D:(base + 3) * D], w=[("modp", ka + 2)])

    def load_mod(self, es, name, k):
        t = self.sb(es, name, [128, D])
        self.P.dma("sp", t[:], self.MODP[k], r=[("modp", k)])
        return t

    def phase_projA(self):
        nc, P = self.nc, self.P
        NCOL = 4 * D + 32
        with self.scope() as es:
            sb = lambda n, s, d=F32: self.sb(es, n, s, d)
            wb = self.load_cast(es, "wina", self.w_in_a, NCOL)
            A = [self.load_mod(es, "A", 0), self.load_mod(es, "Ac", 12)]
            B = [self.load_mod(es, "B", 1), self.load_mod(es, "Bc", 13)]
            xt = [sb("xt", [128, D]) for _ in range(2)]
            sq = sb("sq", [128, D])
            ss = sb("ss", [128, 4])
            hf = sb("hf", [128, D])
            hb = sb("hb", [128, D], BF16)
            hT = [sb("hT", [128, 8, 128], BF16) for _ in range(2)]
            po = [sb("po", [128, NCOL]) for _ in range(2)]
            tiles = [("c", i) for i in range(self.NC)] + [("l", i) for i in range(self.NT)]
            for ti, (kind, i) in enumerate(tiles):
                src = self.ctx if kind == "c" else self.x
                j = 1 if kind == "c" else 0
                row0 = i * 128 + (0 if kind == "c" else self.CTXL)
                x_ = xt[ti % 2]
                P.dma("sp", x_[:], src[i * 128:(i + 1) * 128, :])
                self.modulate(x_, A[j], B[j], sq, ss, hf, hb)
                hT_ = hT[ti % 2]
                self.transpose8(hT_, hb, self.pt[ti % 2], ti)
                po_ = po[ti % 2]
                g = 0
                for c0 in range(0, NCOL, 512):
                    cw = min(512, NCOL - c0)
                    ps = self.pf[g % 4]
                    for kc in range(8):
                        P.mm(ps[:, 0:cw], hT_[:, kc, :], wb[:, kc, c0:c0 + cw], start=(kc == 0), stop=(kc == 7))
                    self.evac(po_[:, c0:c0 + cw], ps[:, 0:cw], g)
                    g += 1
                P.dma("pool", self.P0[row0:row0 + 128, :], po_[:], w=[("p0", row0 // 128)])

    def phase_qkv(self):
        nc, P = self.nc, self.P
        CT = self.CTXL
        with self.scope() as es:
            sb = lambda n, s, dt=F32: self.sb(es, n, s, dt)
            cw = [self.bcast_load(es, "cw%d" % s, self.conv_a[s], 3 * D) for s in range(3)]
            pm_ = [sb("pm", [128, 3 * D]) for _ in range(2)]
            pc_ = [sb("pc", [128, 3 * D]) for _ in range(2)]
            pp_ = [sb("pp", [128, 3 * D]) for _ in range(2)]
            qo_ = [sb("qo", [128, 3 * D], BF16) for _ in range(2)]
            so_ = [sb("so", [128, 3 * D]) for _ in range(2)]
            ssq_ = [sb("ssq", [128, 64]) for _ in range(2)]
            bc = lambda ap: ap.unsqueeze(2).to_broadcast([128, ap.shape[1], HD])
            tiles = [("c", i) for i in range(self.NC)] + [("l", i) for i in range(self.NT)]

            def loads(ti):
                if ti >= len(tiles):
                    return
                kind, i = tiles[ti]
                pm, pc, pp = pm_[ti % 2], pc_[ti % 2], pp_[ti % 2]
                ntile = self.NC if kind == "c" else self.NT
                row0 = (0 if kind == "c" else CT) + i * 128
                P.dma("sp", pc[:], self.P0[row0:row0 + 128, 0:3 * D])
                if i == 0:
                    P.memset(pm[:], 0.0, eng="pool")
                    P.dma("sp", pm[1:128, :], self.P0[row0:row0 + 127, 0:3 * D], w=[pm])
                else:
                    P.dma("sp", pm[:], self.P0[row0 - 1:row0 + 127, 0:3 * D])
                if i == ntile - 1:
                    P.memset(pp[:], 0.0, eng="pool")
                    P.dma("sp", pp[0:127, :], self.P0[row0 + 1:row0 + 128, 0:3 * D], w=[pp])
                else:
                    P.dma("sp", pp[:], self.P0[row0 + 1:row0 + 129, 0:3 * D])

            loads(0)
            loads(1)

            def qbody(ti):
                kind, i = tiles[ti]
                pm, pc, pp, qo, ssq = pm_[ti % 2], pc_[ti % 2], pp_[ti % 2], qo_[ti % 2], ssq_[ti % 2]
                so = so_[ti % 2]
                row0 = (0 if kind == "c" else CT) + i * 128
                P.tt(pm[:], pm[:], cw[0][:], ALU.mult)
                P.tt(pc[:], pc[:], cw[1][:], ALU.mult)
                P.tt(pp[:], pp[:], cw[2][:], ALU.mult)
                yield
                for c in range(6):
                    ps = self.pf[c]
                    cs = slice(c * 512, (c + 1) * 512)
                    P.mm(ps[:], self.ident_f[:], pm[:, cs], start=True, stop=False)
                    P.mm(ps[:], self.ident_f[:], pc[:, cs], start=False, stop=False)
                    P.mm(ps[:], self.ident_f[:], pp[:, cs], start=False, stop=True)
                    P.act(so[:, cs], ps[:], AF.Silu)
                    if c % 2 == 1:
                        yield
                P.act(pm[:, 0:2 * D], so[:, 0:2 * D], AF.Square)
                yield
                P.red(ssq[:, 0:16], pm[:, 0:2 * D].rearrange("p (h e) -> p h e", e=HD), ALU.add)
                P.ts(ssq[:, 16:32], ssq[:, 0:16], EPS, ALU.add)
                P.act(ssq[:, 32:48], ssq[:, 16:32], AF.Sqrt)
                P.recip(ssq[:, 48:64], ssq[:, 32:48])
                P.ts(ssq[:, 48:56], ssq[:, 48:56], HD ** -0.5, ALU.mult)
                P.tt(qo[:, 0:D].rearrange("p (h e) -> p h e", e=HD), so[:, 0:D].rearrange("p (h e) -> p h e", e=HD),
                     bc(ssq[:, 48:56]), ALU.mult)
                P.tt(qo[:, D:2 * D].rearrange("p (h e) -> p h e", e=HD), so[:, D:2 * D].rearrange("p (h e) -> p h e", e=HD),
                     bc(ssq[:, 56:64]), ALU.mult, eng="pool")
                P.copy(qo[:, 2 * D:3 * D], so[:, 2 * D:3 * D], eng="act")
                yield
                loads(ti + 2)
                P.dma("pool", self.QKV[row0:row0 + 128, :], qo[:])
                yield
            self.run2(len(tiles), qbody)

    def phase_delta_both(self):
        with self.scope() as es:
            gens = [self.delta_gen(es, 0), self.delta_gen(es, 1)]
            while gens:
                for g in list(gens):
                    try:
                        next(g)
                    except StopIteration:
                        gens.remove(g)

    def delta_gen(self, es, d):
        nc, P = self.nc, self.P
        CT = self.CTXL
        sb = lambda n, s, dt=F32: self.sb(es, n, s, dt)
        nA = self.bcast_load(es, "nA", self.a_log[d], NH)
        P.act(nA[:], nA[:], AF.Exp)
        P.ts(nA[:], nA[:], -1.0, ALU.mult)
        dtb = self.bcast_load(es, "dtb", self.dt_bias[d], NH)
        qv = sb("qv", [128, 3 * D], BF16)
        gts = sb("gts", [128, 32])
        sml = sb("sml", [128, 16 * NH])
        kT = sb("kT", [128, NH, HD], BF16)
        qT = sb("qT", [128, NH, HD], BF16)
        qd_b = sb("qd_b", [128, NH, HD], BF16)
        qdT = sb("qdT", [128, NH, HD], BF16)
        vb = sb("vb", [128, NH, HD], BF16)
        kbe = sb("kbe", [128, NH, HD], BF16)
        kdec = sb("kdec", [128, NH, HD], BF16)
        rhsR = sb("rhsR", [128, NH, HD])
        Dm = sb("Dm", [128, NH, HD])
        As = sb("As", [128, NH, HD], BF16)
        qk_b = sb("qk_b", [128, NH, HD], BF16)
        qkT = sb("qkT", [128, NH, HD], BF16)
        L_b = sb("L_b", [128, NH, HD], BF16); N_b = sb("N_b", [128, NH, HD], BF16)
        Ld = sb("Ld", [128, NH, HD], BF16); Nd = sb("Nd", [128, NH, HD], BF16)
        P1 = sb("P1", [128, NH, HD], BF16); Q1 = sb("Q1", [128, NH, HD], BF16)
        P2 = sb("P2", [128, NH, HD], BF16); Q2 = sb("Q2", [128, NH, HD], BF16)
        Tw = [sb("Tw", [128, NH, HD], BF16) for _ in range(2)]
        Xw = [sb("Xw", [128, NH, HD], BF16) for _ in range(2)]
        u32 = sb("u32", [128, NH, HD])
        wT = sb("wT", [128, NH, HD], BF16)
        vnew = sb("vnew", [128, NH, HD], BF16)
        S32 = sb("S32", [128, NH, HD])
        Sb = sb("Sb", [128, NH, HD], BF16)
        Stmp = sb("Stmp", [128, NH, HD])
        o32 = sb("o32", [128, NH, HD])
        P.memset(S32[:], 0.0)
        P.memset(Sb[:], 0.0, eng="pool")
        OUT = self.OF if d == 0 else self.OB
        ptd = self.pt[d]
        if d == 0:
            order = [("c", i) for i in range(self.NC)] + [("l", i) for i in range(self.NT)]
        else:
            order = [("c", i) for i in reversed(range(self.NC))] + [("l", i) for i in reversed(range(self.NT))]
        bc = lambda ap: ap.unsqueeze(2).to_broadcast([128, ap.shape[1], HD])
        mb = lambda m: m[:].unsqueeze(1).to_broadcast([128, NH, HD])
        v4 = lambda ps: ps[:].rearrange("p (h e) -> p h e", e=HD)
        pfi = self.pfi

        def nextpf():
            pfi[0] += 1
            return self.pf[pfi[0] % 6]

        def mm4(lhs, rhs, hg):
            ps = nextpf()
            for hh in range(4):
                h = hg * 4 + hh
                P.mm(ps[:, hh * 128:(hh + 1) * 128], lhs[:, h, :], rhs[:, h, :])
            return ps

        for ti, (kind, i) in enumerate(order):
            row0 = (0 if kind == "c" else CT) + i * 128
            P.dma("sp", qv[:], self.QKV[row0:row0 + 128, :])
            P.dma("sp", gts[:], self.P0[row0:row0 + 128, 4 * D:4 * D + 32])
            qn_b = qv[:, 0:D].rearrange("p (h e) -> p h e", e=HD)
            kn_b = qv[:, D:2 * D].rearrange("p (h e) -> p h e", e=HD)
            v_b = qv[:, 2 * D:3 * D].rearrange("p (h e) -> p h e", e=HD)
            yield
            beta = sml[:, 0:8]
            P.act(beta, gts[:, d * 8:d * 8 + 8], AF.Sigmoid)
            xg = sml[:, 8:16]
            P.tt(xg, gts[:, 16 + d * 8:16 + d * 8 + 8], dtb[:], ALU.add)
            P.stt(sml[:, 16:24], xg, -1.0, xg, ALU.mult, ALU.max)
            P.act(sml[:, 24:32], sml[:, 16:24], AF.Exp, scale=-1.0)
            P.act(sml[:, 32:40], sml[:, 24:32], AF.Ln, bias=1.0)
            P.stt(sml[:, 40:48], xg, 0.0, sml[:, 32:40], ALU.max, ALU.add)
            g = sml[:, 48:56]
            P.tt(g, sml[:, 40:48], nA[:], ALU.mult)
            yield
            ps = nextpf()
            P.mm(ps[:, 0:8], self.U[d][:], g)
            P.mm(ps[:, 8:16], self.ones_f[:], g)
            gc = sml[:, 56:64]
            gl = sml[:, 64:72]
            P.copy(sml[:, 56:72], ps[:, 0:16])
            for (src, dst) in ((kn_b, kT), (qn_b, qT)):
                for h in range(NH):
                    P.tr(ptd[:, h, :], src[:, h, :], self.ident_b[:])
                self.evac(dst[:], ptd[:], 1)
                yield
            egc = sml[:, 72:80]
            P.act(egc, gc, AF.Exp)
            egl = sml[:, 80:88]
            P.act(egl, gl, AF.Exp)
            kdc = sml[:, 88:96]
            P.tt(kdc, gl, gc, ALU.subtract)
            P.act(kdc, kdc, AF.Exp)
            bq = sml[:, 96:104]
            P.tt(bq, beta, egc, ALU.mult)
            yield
            P.tt(vb[:], v_b, bc(beta), ALU.mult)
            P.tt(kbe[:], kn_b, bc(bq), ALU.mult, eng="pool")
            P.tt(kdec[:], kn_b, bc(kdc), ALU.mult, eng="pool")
            P.tt(qd_b[:], qn_b, bc(egc), ALU.mult)
            P.tt(rhsR[:], self.U[d][:].unsqueeze(1).to_broadcast([128, NH, HD]), bc(g), ALU.mult, eng="pool")
            yield
            for h in range(NH):
                P.tr(ptd[:, h, :], qd_b[:, h, :], self.ident_b[:])
            self.evac(qdT[:], ptd[:], 1)
            yield
            for hg in range(2):
                ps = nextpf()
                P.mm(ps[:], self.ones_f[:], rhsR[:, hg * 4:(hg + 1) * 4, :].rearrange("p h e -> p (h e)"), start=True, stop=False)
                P.mm(ps[:], self.ident_f[:], self.BM[d][:].rearrange("p h e -> p (h e)"), start=False, stop=True)
                for hh in range(4):
                    h = hg * 4 + hh
                    P.act(Dm[:, h, :], ps[:, hh * 128:(hh + 1) * 128], AF.Exp, bias=gc[:, h:h + 1], scale=-1.0)
                yield
            for hg in range(2):
                hs = slice(hg * 4, (hg + 1) * 4)
                P.tt(As[:, hs, :], v4(mm4(kT, kT, hg)), self.SM[d][:].unsqueeze(1).to_broadcast([128, 4, HD]), ALU.mult)
                P.tt(qk_b[:, hs, :], v4(mm4(qT, kT, hg)), Dm[:, hs, :], ALU.mult)
                yield
            for h in range(NH):
                P.stt(L_b[:, h, :], As[:, h, :], beta[:, h:h + 1], Dm[:, h, :], ALU.mult, ALU.mult)
            yield
            for (src, dst) in ((L_b, N_b), (qk_b, qkT)):
                for h in range(NH):
                    P.tr(ptd[:, h, :], src[:, h, :], self.ident_b[:])
                self.evac(dst[:], ptd[:], 1)
                yield
            P.tt(Ld[:], L_b[:], mb(self.BD8), ALU.mult)
            P.tt(Nd[:], N_b[:], mb(self.BD8), ALU.mult, eng="pool")
            P.tt(Tw[0][:], mb(self.ident_b), Ld[:], ALU.subtract)
            P.tt(Xw[0][:], mb(self.ident_b), Nd[:], ALU.subtract, eng="pool")
            yield
            cur = 0
            for (Pa, Qa, Pprev, Qprev) in ((P1, Q1, Ld, Nd), (P2, Q2, P1, Q1)):
                for hg in range(2):
                    hs = slice(hg * 4, (hg + 1) * 4)
                    self.evac(Pa[:, hs, :], v4(mm4(Qprev, Pprev, hg)), 1)
                    self.evac(Qa[:, hs, :], v4(mm4(Pprev, Qprev, hg)), 1)
                yield
                nxt = 1 - cur
                for hg in range(2):
                    hs = slice(hg * 4, (hg + 1) * 4)
                    P.tt(Tw[nxt][:, hs, :], Tw[cur][:, hs, :], v4(mm4(Qa, Tw[cur], hg)), ALU.add)
                    P.tt(Xw[nxt][:, hs, :], Xw[cur][:, hs, :], v4(mm4(Pa, Xw[cur], hg)), ALU.add)
                yield
                cur = nxt
            Cs, CTs, Zb, Zpb = Ld, Nd, P1, Q1
            for sz in (8, 16, 32, 64):
                P.tt(Cs[:], L_b[:], mb(self.MS[sz]), ALU.mult)
                P.tt(CTs[:], N_b[:], mb(self.MS[sz]), ALU.mult, eng="pool")
                for hg in range(2):
                    hs = slice(hg * 4, (hg + 1) * 4)
                    self.evac(Zb[:, hs, :], v4(mm4(Cs, Xw[cur], hg)), 1)
                    if sz < 64:
                        self.evac(Zpb[:, hs, :], v4(mm4(CTs, Tw[cur], hg)), 1)
                yield
                nxt = 1 - cur
                for hg in range(2):
                    hs = slice(hg * 4, (hg + 1) * 4)
                    P.tt(Xw[nxt][:, hs, :], Xw[cur][:, hs, :], v4(mm4(Tw[cur], Zb, hg)), ALU.subtract)
                    if sz < 64:
                        P.tt(Tw[nxt][:, hs, :], Tw[cur][:, hs, :], v4(mm4(Xw[cur], Zpb, hg)), ALU.subtract)
                yield
                cur = nxt
            TTm = Xw[cur]
            for hg in range(2):
                hs = slice(hg * 4, (hg + 1) * 4)
                self.evac(u32[:, hs, :], v4(mm4(TTm, vb, hg)), 1)
                self.evac(wT[:, hs, :], v4(mm4(kbe, TTm, hg)), 1)
            yield
            for hg in range(2):
                hs = slice(hg * 4, (hg + 1) * 4)
                P.tt(vnew[:, hs, :], u32[:, hs, :], v4(mm4(wT, Sb, hg)), ALU.subtract)
                yield
                if kind == "l":
                    ps2 = nextpf()
                    for hh in range(4):
                        h = hg * 4 + hh
                        o_ = ps2[:, hh * 128:(hh + 1) * 128]
                        P.mm(o_, qdT[:, h, :], Sb[:, h, :], start=True, stop=False)
                        P.mm(o_, qkT[:, h, :], vnew[:, h, :], start=False, stop=True)
                    self.evac(o32[:, hs, :], v4(ps2), 1)
                ps3 = mm4(kdec, vnew, hg)
                P.tt(Stmp[:, hs, :], S32[:, hs, :], bc(egl[:, hs]), ALU.mult, eng="pool")
                P.tt(S32[:, hs, :], Stmp[:, hs, :], v4(ps3), ALU.add)
                P.copy(Sb[:, hs, :], S32[:, hs, :], eng="act")
                yield
            if kind == "l":
                P.dma("pool", OUT[i * 128:(i + 1) * 128, :], o32[:].rearrange("p h e -> p (h e)"))
            yield

    def phase_finish(self):
        nc, P = self.nc, self.P
        CT = self.CTXL
        with self.scope() as es:
            sb = lambda n, s, dt=F32: self.sb(es, n, s, dt)
            onw = self.bcast_load(es, "onw", self.onorm, HD)
            wo = self.load_cast(es, "wouta", self.w_out_a, D)
            Gm = self.load_mod(es, "Gm", 2)
            of_ = [sb("of_", [128, NH, HD]) for _ in range(2)]
            ob_ = [sb("ob_", [128, NH, HD]) for _ in range(2)]
            zt_ = [sb("zt", [128, D]) for _ in range(2)]
            xt_ = [sb("xt", [128, D]) for _ in range(2)]
            osq_ = [sb("osq", [128, NH, HD]) for _ in range(2)]
            orn_ = [sb("orn", [128, 4 * NH]) for _ in range(2)]
            yg_ = [sb("yg", [128, D], BF16) for _ in range(2)]
            ygT = [sb("ygT", [128, 8, 128], BF16) for _ in range(2)]
            x1t = [sb("x1t", [128, D]) for _ in range(2)]
            bc = lambda ap: ap.unsqueeze(2).to_broadcast([128, ap.shape[1], HD])

            def loads(i):
                if i >= self.NT:
                    return
                sl = i % 2
                P.dma("sp", of_[sl][:].rearrange("p h e -> p (h e)"), self.OF[i * 128:(i + 1) * 128, :])
                P.dma("sp", ob_[sl][:].rearrange("p h e -> p (h e)"), self.OB[i * 128:(i + 1) * 128, :])
                P.dma("sp", zt_[sl][:], self.P0[CT + i * 128:CT + (i + 1) * 128, 3 * D:4 * D])
                P.dma("sp", xt_[sl][:], self.x[i * 128:(i + 1) * 128, :])

            loads(0)
            loads(1)

            def fbody(i):
                sl = i % 2
                o32, ob, zt, xt = of_[sl], ob_[sl], zt_[sl], xt_[sl]
                osq, orn, yg = osq_[sl], orn_[sl], yg_[sl]
                P.tt(o32[:], o32[:], ob[:], ALU.add)
                yield
                P.act(osq[:], o32[:], AF.Square)
                P.red(orn[:, 0:8], osq[:], ALU.add)
                P.ts(orn[:, 8:16], orn[:, 0:8], 1.0 / HD, ALU.mult, EPS, ALU.add)
                P.act(orn[:, 16:24], orn[:, 8:16], AF.Sqrt)
                P.recip(orn[:, 24:32], orn[:, 16:24])
                yield
                P.tt(osq[:], o32[:], bc(orn[:, 24:32]), ALU.mult)
                P.tt(osq[:], osq[:], onw[:].unsqueeze(1).to_broadcast([128, NH, HD]), ALU.mult)
                P.act(zt[:], zt[:], AF.Silu)
                P.tt(yg[:], osq[:].rearrange("p h e -> p (h e)"), zt[:], ALU.mult)
                yield
                self.transpose8(ygT[sl], yg, self.pt[sl], i)
                yield
                x1 = x1t[sl]
                for half in range(2):
                    ps = self.pf[(2 * i + half) % 6]
                    for kc in range(8):
                        P.mm(ps[:], ygT[sl][:, kc, :], wo[:, kc, half * 512:(half + 1) * 512], start=(kc == 0), stop=(kc == 7))
                    cs = slice(half * 512, (half + 1) * 512)
                    P.tt(x1[:, cs], ps[:], Gm[:, cs], ALU.mult)
                    P.tt(x1[:, cs], x1[:, cs], xt[:, cs], ALU.add)
                loads(i + 2)
                yield
                P.dma("pool", self.X1[i * 128:(i + 1) * 128, :], x1[:], w=[("x1", i)])
                yield
            self.run2(self.NT, fbody)

    def phase_moe(self, layer, XIN, XOUT, final):
        nc, P = self.nc, self.P
        NT, NB = self.NT, self.NBLK
        xin_key = "x1" if layer == 0 else "x3"
        with self.scope() as es:
            sb = lambda n, s, dt=F32: self.sb(es, n, s, dt)
            A = self.load_mod(es, "Af", 6 * layer + 3)
            B = self.load_mod(es, "Bf", 6 * layer + 4)
            G = self.load_mod(es, "Gf", 6 * layer + 5)
            rt = sb("rt", [128, 8, 36])
            P.dma("sp", rt[:], self.router[layer].rearrange("(kc p) n -> p kc n", p=128))
            E1 = sb("E1", [128, NT]); E2 = sb("E2", [128, NT])
            R1 = sb("R1", [128, NT]); R2 = sb("R2", [128, NT])
            W1 = sb("W1", [128, NT]); W2 = sb("W2", [128, NT])
            carry = sb("carry", [128, NE])
            P.memset(carry[:], 0.0)
            iotaE = sb("iotaE", [128, NE])
            P.copy(iotaE[:], self.coli[:, 0:NE])
            with self.scope() as e1:
                s1 = lambda n, s, dt=F32: self.sb(e1, n, s, dt)
                xt = [s1("xt", [128, D]) for _ in range(2)]
                bufs = []
                for _ in range(2):
                    bufs.append(dict(
                        sq=s1("sq", [128, D]), ss=s1("ss", [128, 4]), hf=s1("hf", [128, D]), hb=s1("hb", [128, D], BF16),
                        hT=s1("hT", [128, 8, 128]), lg=s1("lg", [128, 36]), sm=s1("sm", [128, 64]),
                        lem=s1("lem", [128, NE]), top8=s1("top8", [128, 8]), m1=s1("m1", [128, NE]),
                        m12=s1("m12", [128, NE]), m2=s1("m2", [128, NE]), m12b=s1("m12b", [128, NE], BF16),
                        pos=s1("pos", [128, NE]), tmp=s1("tmp", [128, NE])))

                def rt_body(i):
                    bb = bufs[i % 2]
                    sq, ss, hf, hb, hT, lg, sm = bb["sq"], bb["ss"], bb["hf"], bb["hb"], bb["hT"], bb["lg"], bb["sm"]
                    lem, top8, m1, m12, m2, m12b, pos, tmp = bb["lem"], bb["top8"], bb["m1"], bb["m12"], bb["m2"], bb["m12b"], bb["pos"], bb["tmp"]
                    x_ = xt[i % 2]
                    P.dma("sp", x_[:], XIN[i * 128:(i + 1) * 128, :], r=[(xin_key, i)])
                    self.modulate(x_, A, B, sq, ss, hf, None)
                    P.copy(hb[:], hf[:], eng="act")
                    P.dma("pool", self.HF[i * 128:(i + 1) * 128, :], hb[:], w=[("hf", i)])
                    yield
                    for half in range(2):
                        ps = self.pf[half]
                        for kk in range(4):
                            kc = half * 4 + kk
                            P.tr(ps[:, kk * 128:(kk + 1) * 128], hf[:, kc * 128:(kc + 1) * 128], self.ident_f[:])
                        self.evac(hT[:, half * 4:(half + 1) * 4, :], ps[:].rearrange("p (k e) -> p k e", e=128), half)
                    yield
                    ps = self.pf[2 + 2 * (i % 2)]
                    for kc in range(8):
                        P.mm(ps[:, 0:36], hT[:, kc, :], rt[:, kc, :], start=(kc == 0), stop=(kc == 7))
                    P.copy(lg[:], ps[:, 0:36])
                    yield
                    P.red(sm[:, 0:1], lg[:, 0:4], ALU.max)
                    P.ts(sm[:, 1:2], sm[:, 0:1], -1.0, ALU.mult)
                    P.act(sm[:, 4:8], lg[:, 0:4], AF.Exp, bias=sm[:, 1:2])
                    P.red(sm[:, 2:3], sm[:, 4:8], ALU.add)
                    P.recip(sm[:, 3:4], sm[:, 2:3])
                    P.ts(sm[:, 8:12], lg[:, 0:4], sm[:, 0:1], ALU.is_ge)
                    P.ts(sm[:, 12:16], sm[:, 8:12], BIG, ALU.mult, -BIG, ALU.add)
                    P.tt(lem[:].rearrange("p (g e) -> p g e", e=8), lg[:, 4:36].rearrange("p (g e) -> p g e", e=8),
                         sm[:, 12:16].unsqueeze(2).to_broadcast([128, 4, 8]), ALU.add)
                    yield
                    P.op("dve", lambda e, o=top8, i_=lem: e.max(out=o[:], in_=i_[:]), r=[lem], w=[top8])
                    P.ts(m1[:], lem[:], top8[:, 0:1], ALU.is_ge)
                    P.ts(m12[:], lem[:], top8[:, 1:2], ALU.is_ge)
                    P.tt(m2[:], m12[:], m1[:], ALU.subtract)
                    P.copy(m12b[:], m12[:], eng="act")
                    yield
                    P.tt(sm[:, 16:17], top8[:, 0:1], top8[:, 1:2], ALU.subtract)
                    P.act(sm[:, 17:18], sm[:, 16:17], AF.Sigmoid)
                    P.tt(W1[:, i:i + 1], sm[:, 17:18], sm[:, 3:4], ALU.mult)
                    P.tt(W2[:, i:i + 1], sm[:, 3:4], W1[:, i:i + 1], ALU.subtract)
                    yield
                    ps = self.pf[3 + 2 * (i % 2)]
                    P.mm(ps[:, 0:NE], self.Us_b[:], m12b[:])
                    P.mm(ps[:, NE:2 * NE], self.ones_b[:], m12b[:])
                    P.tt(pos[:], ps[:, 0:NE], carry[:], ALU.add)
                    P.tt(carry[:], carry[:], ps[:, NE:2 * NE], ALU.add)
                    for (mk, Ed, Rd) in ((m1, E1, R1), (m2, E2, R2)):
                        P.tt(tmp[:], mk[:], iotaE[:], ALU.mult)
                        P.red(Ed[:, i:i + 1], tmp[:], ALU.add)
                        P.tt(tmp[:], mk[:], pos[:], ALU.mult)
                        P.red(Rd[:, i:i + 1], tmp[:], ALU.add)
                    yield
                self.run2(NT, rt_body)
            D1i = sb("D1i", [128, NT], I32); D2i = sb("D2i", [128, NT], I32)
            WIi = sb("WIi", [128, NB], I32)
            with self.scope() as e2:
                s2 = lambda n, s, dt=F32: self.sb(e2, n, s, dt)
                NTH = 2 * NT + 1
                thr = s2("thr", [128, NTH])
                P.op("pool", lambda e: e.iota(thr[:], pattern=[[128, NTH]], base=0, channel_multiplier=0,
                                               allow_small_or_imprecise_dtypes=True), w=[thr])
                cmp = s2("cmp", [128, NE, NTH])
                P.tt(cmp[:], carry[:].unsqueeze(2).to_broadcast([128, NE, NTH]),
                     thr[:].unsqueeze(1).to_broadcast([128, NE, NTH]), ALU.is_gt)
                padded = s2("padded", [128, NE])
                P.red(padded[:], cmp[:], ALU.add)
                P.ts(padded[:], padded[:], 128.0, ALU.mult)
                le = s2("le", [128, NE, NE])
                P.tt(le[:], self.coli[:, 0:NE].unsqueeze(2).to_broadcast([128, NE, NE]),
                     self.coli[:, 0:NE].unsqueeze(1).to_broadcast([128, NE, NE]), ALU.is_ge)
                P.tt(le[:], le[:], padded[:].unsqueeze(1).to_broadcast([128, NE, NE]), ALU.mult)
                ends = s2("ends", [128, NE]); starts = s2("starts", [128, NE])
                P.red(ends[:], le[:], ALU.add)
                P.tt(starts[:], ends[:], padded[:], ALU.subtract)
                oh = s2("oh", [128, NT, NE])
                dd = s2("dd", [128, NT])
                for (Ed, Rd, Di) in ((E1, R1, D1i), (E2, R2, D2i)):
                    P.tt(oh[:], Ed[:].unsqueeze(2).to_broadcast([128, NT, NE]),
                         iotaE[:].unsqueeze(1).to_broadcast([128, NT, NE]), ALU.is_equal)
                    P.tt(oh[:], oh[:], starts[:].unsqueeze(1).to_broadcast([128, NT, NE]), ALU.mult)
                    P.red(dd[:], oh[:], ALU.add)
                    P.tt(dd[:], dd[:], Rd[:], ALU.add)
                    P.copy(Di[:], dd[:])
                bthr = s2("bthr", [128, NB])
                P.op("pool", lambda e: e.iota(bthr[:], pattern=[[128, NB]], base=0, channel_multiplier=0,
                                               allow_small_or_imprecise_dtypes=True), w=[bthr])
                cb = s2("cb", [128, NB, NE])
                P.tt(cb[:], ends[:].unsqueeze(1).to_broadcast([128, NB, NE]),
                     bthr[:].unsqueeze(2).to_broadcast([128, NB, NE]), ALU.is_le)
                be = s2("be", [128, NB + 2])
                P.memset(be[:, 0:2], -1.0)
                P.red(be[:, 2:NB + 2], cb[:], ALU.add)
                P.ts(be[:, 2:NB + 2], be[:, 2:NB + 2], float(NE - 1), ALU.min)
                need = s2("need", [128, NB])
                P.tt(need[:], be[:, 2:NB + 2], be[:, 1:NB + 1], ALU.not_equal)
                P.memset(need[:, NB // 2:NB // 2 + 1], 1.0)
                wi = s2("wi", [128, NB])
                P.ts(wi[:], be[:, 2:NB + 2], 128.0, ALU.mult, self.rowi[:, 0:1], ALU.add)
                P.ts(wi[:], wi[:], -1.0e6, ALU.add)
                P.tt(wi[:], wi[:], need[:], ALU.mult)
                P.ts(wi[:], wi[:], 1.0e6, ALU.add)
                P.copy(WIi[:], wi[:])
            with self.scope() as e3:
                s3 = lambda n, s, dt=F32: self.sb(e3, n, s, dt)
                hbt = [s3("hbt", [128, D], BF16) for _ in range(2)]
                for i in range(NT):
                    h_ = hbt[i % 2]
                    P.dma("sp", h_[:], self.HF[i * 128:(i + 1) * 128, :], r=[("hf", i)])
                    for Di in (D1i, D2i):
                        def sc(e, Di=Di, h_=h_, i=i):
                            if "bx" not in self.regcache:
                                self.regcache["bx"] = e.to_reg(self.NBLK * 128 - 1)
                            return e.indirect_dma_start(
                                out=self.XB[:, :], out_offset=bass.IndirectOffsetOnAxis(ap=Di[:, i:i + 1], axis=0),
                                in_=h_[:, :], in_offset=None, bounds_check=self.regcache["bx"], oob_is_err=False)
                        P.op("pool", sc, r=[h_, Di], w=[], dma=True)
            with self.scope() as e4:
                s4 = lambda n, s, dt=F32: self.sb(e4, n, s, dt)
                stg = [s4("stg", [128, 3 * 4096]) for _ in range(2)]
                w1b_ = [s4("w1b", [128, 8, DE], BF16) for _ in range(2)]
                w3b_ = [s4("w3b", [128, 8, DE], BF16) for _ in range(2)]
                w2b_ = [s4("w2b", [128, 4, D], BF16) for _ in range(2)]
                xbt = [s4("xbt", [128, D], BF16) for _ in range(2)]
                xT_ = [s4("xT", [128, 8, 128], BF16) for _ in range(2)]
                h1_ = [s4("h1", [128, DE]) for _ in range(2)]
                gb_ = [s4("gb", [128, DE], BF16) for _ in range(2)]
                gT_ = [s4("gT", [128, 4, 128], BF16) for _ in range(2)]
                yo = [s4("yo", [128, D]) for _ in range(2)]
                HB = NB // 2
                lane = lambda b: 0 if b < HB else 1

                def gather(b, ln):
                    if b >= NB or lane(b) != ln:
                        return
                    sl = ln

                    def gw(e, b=b, sl=sl):
                        if "bc" not in self.regcache:
                            self.regcache["bc"] = e.to_reg(NE * 128 - 1)
                        return e.indirect_dma_start(
                            out=stg[sl][:, :], out_offset=None, in_=self.wall[layer][:, :],
                            in_offset=bass.IndirectOffsetOnAxis(ap=WIi[:, b:b + 1], axis=0),
                            bounds_check=self.regcache["bc"], oob_is_err=False)
                    P.op("pool", gw, r=[WIi], w=[stg[sl]], dma=True)

                def xload(b, ln):
                    if b >= NB or lane(b) != ln:
                        return
                    P.dma("sp", xbt[ln][:], self.XB[b * 128:(b + 1) * 128, :], r=[], w=[xbt[ln]])

                gather(0, 0)
                gather(HB, 1)
                xload(0, 0)
                xload(HB, 1)
                def blk(b):
                    sl = lane(b)
                    w1b, w3b, w2b, xT, h1, gb, gT = w1b_[sl], w3b_[sl], w2b_[sl], xT_[sl], h1_[sl], gb_[sl], gT_[sl]
                    w1f = w1b[:].rearrange("p k n -> p (k n)")
                    w3f = w3b[:].rearrange("p k n -> p (k n)")
                    w2f = w2b[:].rearrange("p k n -> p (k n)")
                    for q in range(4):
                        cq = slice(q * 1024, (q + 1) * 1024)
                        P.copy(w1f[:, cq], stg[sl][:, q * 1024:(q + 1) * 1024], eng="dve")
                        P.copy(w3f[:, cq], stg[sl][:, 4096 + q * 1024:4096 + (q + 1) * 1024], eng="act")
                        P.copy(w2f[:, cq], stg[sl][:, 8192 + q * 1024:8192 + (q + 1) * 1024], eng="act" if q < 2 else "dve")
                        yield
                    gather(b + 1, sl)
                    yield
                    self.transpose8(xT, xbt[sl], self.pt[sl], b)
                    xload(b + 1, sl)
                    yield
                    p1, p3 = self.pf[2 * sl], self.pf[2 * sl + 1]
                    for kc in range(8):
                        P.mm(p1[:], xT[:, kc, :], w1b[:, kc, :], start=(kc == 0), stop=(kc == 7))
                    for kc in range(8):
                        P.mm(p3[:], xT[:, kc, :], w3b[:, kc, :], start=(kc == 0), stop=(kc == 7))
                    yield
                    P.act(h1[:], p1[:], AF.Silu)
                    P.tt(gb[:], h1[:], p3[:], ALU.mult)
                    yield
                    pt_ = self.pt[1 - sl]
                    for fc in range(4):
                        P.tr(pt_[:, fc, :], gb[:, fc * 128:(fc + 1) * 128], self.ident_b[:])
                    self.evac(gT[:], pt_[:, 0:4, :], b + 1)
                    yield
                    y_ = yo[sl]
                    for half in range(2):
                        ps = self.pf[4 + half]
                        for fc in range(4):
                            P.mm(ps[:], gT[:, fc, :], w2b[:, fc, half * 512:(half + 1) * 512], start=(fc == 0), stop=(fc == 3))
                        self.evac(y_[:, half * 512:(half + 1) * 512], ps[:], half)
                    yield
                    P.dma("pool", self.YB[b * 128:(b + 1) * 128, :], y_[:], w=["yb_all"])
                    yield
                def lane_stream(ln):
                    for b in (range(0, HB) if ln == 0 else range(HB, NB)):
                        yield from blk(b)
                gens = [lane_stream(0), lane_stream(1)]
                for _ in range(6):
                    next(gens[0])
                while gens:
                    for g_ in list(gens):
                        try:
                            next(g_)
                        except StopIteration:
                            gens.remove(g_)
            with self.scope() as e5:
                s5 = lambda n, s, dt=F32: self.sb(e5, n, s, dt)
                y1 = [s5("y1", [128, D]) for _ in range(2)]
                y2 = [s5("y2", [128, D]) for _ in range(2)]
                xt = [s5("xt", [128, D]) for _ in range(2)]
                acc_ = [s5("acc", [128, D]) for _ in range(2)]
                xo_ = [s5("xo", [128, D]) for _ in range(2)]
                if final:
                    fn = self.bcast_load(e5, "fn", self.final_norm, D)
                    sq_ = [s5("sq", [128, D]) for _ in range(2)]; ss_ = [s5("ss", [128, 4]) for _ in range(2)]
                def cload(i):
                    if i >= NT:
                        return
                    sl = i % 2
                    for (yy, Di) in ((y1, D1i), (y2, D2i)):
                        def gy(e, yy=yy, Di=Di, i=i, sl=sl):
                            if "bx" not in self.regcache:
                                self.regcache["bx"] = e.to_reg(self.NBLK * 128 - 1)
                            return e.indirect_dma_start(
                                out=yy[sl][:, :], out_offset=None, in_=self.YB[:, :],
                                in_offset=bass.IndirectOffsetOnAxis(ap=Di[:, i:i + 1], axis=0),
                                bounds_check=self.regcache["bx"], oob_is_err=False)
                        P.op("pool", gy, r=[Di], w=[yy[sl]], dma=True)
                    P.dma("sp", xt[sl][:], XIN[i * 128:(i + 1) * 128, :], r=[(xin_key, i)])

                cload(0)
                cload(1)

                def cmb(i):
                    sl = i % 2
                    acc = acc_[sl]
                    if final:
                        sq, ss = sq_[sl], ss_[sl]
                    P.ts(acc[:], y1[sl][:], W1[:, i:i + 1], ALU.mult)
                    P.stt(acc[:], y2[sl][:], W2[:, i:i + 1], acc[:], ALU.mult, ALU.add)
                    P.tt(acc[:], acc[:], G[:], ALU.mult)
                    yield
                    xo = xo_[sl]
                    P.tt(xo[:], acc[:], xt[sl][:], ALU.add)
                    cload(i + 2)
                    yield
                    if not final:
                        P.dma("act", XOUT[i * 128:(i + 1) * 128, :], xo[:], w=[("x2", i)])
                    else:
                        P.act(sq[:], xo[:], AF.Square)
                        P.red(ss[:, 0:1], sq[:], ALU.add)
                        P.ts(ss[:, 1:2], ss[:, 0:1], 1.0 / D, ALU.mult, EPS, ALU.add)
                        P.act(ss[:, 2:3], ss[:, 1:2], AF.Sqrt)
                        P.recip(ss[:, 3:4], ss[:, 2:3])
                        P.stt(sq[:], xo[:], ss[:, 3:4], fn[:], ALU.mult, ALU.mult)
                        P.dma("act", XOUT[i * 128:(i + 1) * 128, :], sq[:], w=[("out", i)])
                    yield
                self.run2(NT, cmb)

    def phase_convD(self):
        nc, P = self.nc, self.P
        with self.scope() as es:
            sb = lambda n, s, dt=F32: self.sb(es, n, s, dt)
            wb = self.load_cast(es, "winb", self.w_in_b, 3 * D)
            A = self.load_mod(es, "A1", 6)
            B = self.load_mod(es, "B1", 7)
            xt = [sb("xt", [128, D]) for _ in range(2)]
            sq_ = [sb("sq", [128, D]) for _ in range(2)]; ss_ = [sb("ss", [128, 4]) for _ in range(2)]
            hf_ = [sb("hf", [128, D]) for _ in range(2)]; hb_ = [sb("hb", [128, D], BF16) for _ in range(2)]
            hT = [sb("hT", [128, 8, 128], BF16) for _ in range(2)]
            gbt = [sb("gbt", [128, D]) for _ in range(2)]
            gcc_ = [sb("gc_", [128, D]) for _ in range(2)]
            ut = [sb("ut", [128, D]) for _ in range(2)]
            def dbody(i):
                x_ = xt[i % 2]
                sq, ss, hf, hb, gc_ = sq_[i % 2], ss_[i % 2], hf_[i % 2], hb_[i % 2], gcc_[i % 2]
                P.dma("sp", x_[:], self.X2[i * 128:(i + 1) * 128, :], r=[("x2", i)])
                self.modulate(x_, A, B, sq, ss, hf, hb)
                yield
                hT_ = hT[i % 2]
                self.transpose8(hT_, hb, self.pt[i % 2], i)
                yield
                g_, u_ = gbt[i % 2], ut[i % 2]
                for g in range(6):
                    ps = self.pf[(g + 3 * (i % 2)) % 6]
                    for kc in range(8):
                        P.mm(ps[:], hT_[:, kc, :], wb[:, kc, g * 512:(g + 1) * 512], start=(kc == 0), stop=(kc == 7))
                    cs = slice((g % 2) * 512, (g % 2 + 1) * 512)
                    if g < 2:
                        self.evac(g_[:, cs], ps[:], g)
                    elif g < 4:
                        self.evac(gc_[:, cs], ps[:], g)
                    else:
                        P.tt(u_[:, cs], ps[:], gc_[:, cs], ALU.mult)
                    if g % 2 == 1:
                        yield
                P.dma("pool", self.GB[i * 128:(i + 1) * 128, :], g_[:], w=[("gb", i)])
                P.dma("pool", self.U1[i * 128:(i + 1) * 128, :], u_[:], w=[("u1", i)])
                yield
            self.run2(self.NT, dbody)

    def phase_convE(self):
        nc, P = self.nc, self.P
        NT = self.NT
        H = D // 2
        with self.scope() as es:
            sb = lambda n, s, dt=F32: self.sb(es, n, s, dt)
            wo = self.load_cast(es, "woutb", self.w_out_b, D)
            cb = [self.bcast_load(es, "cb%d" % s, self.conv_b[s], D) for s in range(3)]
            Gm = self.load_mod(es, "Gm1", 8)
            mm_ = sb("mm_", [128, 4])
            P.ts(mm_[:, 0:1], self.rowi[:, 0:1], 64.0, ALU.is_equal)
            P.ts(mm_[:, 1:2], self.rowi[:, 0:1], 0.0, ALU.is_equal)
            P.tt(mm_[:, 0:1], mm_[:, 0:1], mm_[:, 1:2], ALU.add)
            P.ts(mm_[:, 0:1], mm_[:, 0:1], -1.0, ALU.mult, 1.0, ALU.add)
            P.ts(mm_[:, 2:3], self.rowi[:, 0:1], 63.0, ALU.is_equal)
            P.ts(mm_[:, 3:4], self.rowi[:, 0:1], 127.0, ALU.is_equal)
            P.tt(mm_[:, 2:3], mm_[:, 2:3], mm_[:, 3:4], ALU.add)
            P.ts(mm_[:, 2:3], mm_[:, 2:3], -1.0, ALU.mult, 1.0, ALU.add)
            um = [sb("um", [128, D]) for _ in range(2)]
            up = [sb("up", [128, D]) for _ in range(2)]
            uc = [sb("uc", [128, D]) for _ in range(2)]
            gbt = [sb("gbt", [128, D]) for _ in range(2)]
            xt = [sb("xt", [128, D]) for _ in range(2)]
            yc_ = [sb("yc", [128, D]) for _ in range(2)]
            vbb_ = [sb("vb_", [128, D], BF16) for _ in range(2)]
            vT_ = [sb("vT", [128, 8, 128], BF16) for _ in range(2)]
            x3t = [sb("x3t", [128, D]) for _ in range(2)]
            for t in um + up:
                P.memset(t[:], 0.0, eng="pool")
            def ebody(i):
                sl = i % 2
                r0 = i * 128
                yc, vb_, vT = yc_[sl], vbb_[sl], vT_[sl]
                um_, up_, uc_ = um[sl], up[sl], uc[sl]
                P.dma("sp", uc_[:], self.U1[r0:r0 + 128, :], r=[("u1", i)])
                P.dma("sp", gbt[sl][:], self.GB[r0:r0 + 128, :], r=[("gb", i)])
                P.dma("sp", xt[sl][:], self.X2[r0:r0 + 128, :], r=[("x2", i)])
                if i == 0:
                    P.dma("sp", um_[1:128, 0:H], self.U1[0:127, 0:H], r=[("u1", 0)], w=[um_])
                else:
                    P.dma("sp", um_[:, 0:H], self.U1[r0 - 1:r0 + 127, 0:H], r=[("u1", i), ("u1", i - 1)], w=[um_])
                if i == NT - 1:
                    P.dma("sp", up_[0:127, 0:H], self.U1[r0 + 1:r0 + 128, 0:H], r=[("u1", i)], w=[up_])
                else:
                    P.dma("sp", up_[:, 0:H], self.U1[r0 + 1:r0 + 129, 0:H], r=[("u1", i), ("u1", i + 1)], w=[up_])
                if i == 0:
                    P.memset(um_[0:64, H:D], 0.0, eng="pool")
                    P.dma("sp", um_[64:128, H:D], self.U1[0:64, H:D], r=[("u1", 0)], w=[um_])
                else:
                    P.dma("sp", um_[:, H:D], self.U1[r0 - 64:r0 + 64, H:D], r=[("u1", i), ("u1", i - 1)], w=[um_])
                if i == NT - 1:
                    P.memset(up_[64:128, H:D], 0.0, eng="pool")
                    P.dma("sp", up_[0:64, H:D], self.U1[r0 + 64:r0 + 128, H:D], r=[("u1", i)], w=[up_])
                else:
                    P.dma("sp", up_[:, H:D], self.U1[r0 + 64:r0 + 192, H:D], r=[("u1", i), ("u1", i + 1)], w=[up_])
                yield
                P.ts(um_[:, 0:H], um_[:, 0:H], mm_[:, 0:1], ALU.mult)
                P.ts(up_[:, 0:H], up_[:, 0:H], mm_[:, 2:3], ALU.mult, eng="pool")
                P.tt(um_[:], um_[:], cb[0][:], ALU.mult)
                P.tt(up_[:], up_[:], cb[2][:], ALU.mult)
                yield
                P.tt(yc[:], uc_[:], cb[1][:], ALU.mult)
                P.tt(yc[:], yc[:], um_[:], ALU.add)
                P.tt(yc[:], yc[:], up_[:], ALU.add)
                P.tt(vb_[:], yc[:], gbt[sl][:], ALU.mult)
                yield
                self.transpose8(vT, vb_, self.pt[sl], i)
                yield
                x3 = x3t[sl]
                for half in range(2):
                    ps = self.pf[half + 2 * sl]
                    for kc in range(8):
                        P.mm(ps[:], vT[:, kc, :], wo[:, kc, half * 512:(half + 1) * 512], start=(kc == 0), stop=(kc == 7))
                    cs = slice(half * 512, (half + 1) * 512)
                    P.tt(x3[:, cs], ps[:], Gm[:, cs], ALU.mult)
                    P.tt(x3[:, cs], x3[:, cs], xt[sl][:, cs], ALU.add)
                P.dma("pool", self.X3[r0:r0 + 128, :], x3[:], w=[("x3", i)])
                yield
            self.run2(NT, ebody)


def make_in_maps(inputs, S, CTXL, n_cores, phases=99):
    f = lambda a: np.ascontiguousarray(np.asarray(a, dtype=np.float32))
    moe = {}
    if phases >= 4:
        w1 = f(inputs["w1"]); w3 = f(inputs["w3"]); w2 = f(inputs["w2"])
        for l in range(2):
            wall = np.empty((NE, 128, 3, 4096), np.float32)
            wall[:, :, 0, :] = w1[l].reshape(NE, 8, 128, DE).transpose(0, 2, 1, 3).reshape(NE, 128, 4096)
            wall[:, :, 1, :] = w3[l].reshape(NE, 8, 128, DE).transpose(0, 2, 1, 3).reshape(NE, 128, 4096)
            wall[:, :, 2, :] = w2[l].reshape(NE, 4, 128, D).transpose(0, 2, 1, 3).reshape(NE, 128, 4096)
            moe["wall%d" % l] = wall.reshape(NE * 128, 3 * 4096)
    router = np.ascontiguousarray(np.concatenate([f(inputs["router_g"]), f(inputs["router_e"])], axis=-1))
    shared = {
        "c_ctx": f(inputs["c_ctx"]), "ada_w": f(inputs["ada_w"]), "ada_b": f(inputs["ada_b"]),
        "norm_mix": f(inputs["norm_mix"]), "norm_ffn": f(inputs["norm_ffn"]),
        "w_in_a": f(inputs["w_in_a"])[0], "conv_a": f(inputs["conv_a"])[0], "a_log": f(inputs["a_log_a"])[0],
        "dt_bias": f(inputs["dt_bias_a"])[0], "onorm": f(inputs["onorm_a"])[0], "w_out_a": f(inputs["w_out_a"])[0],
        "w_in_b": f(inputs["w_in_b"])[0], "conv_b": f(inputs["conv_b"])[0], "w_out_b": f(inputs["w_out_b"])[0],
        "router": router, "final_norm": f(inputs["final_norm"]),
    }
    shared.update(moe)
    x = f(inputs["x"]); c = f(inputs["c"]); ctx = f(inputs["ctx"])
    maps = []
    for b in range(n_cores):
        m = dict(shared)
        m["x"] = np.ascontiguousarray(x[b, :S])
        m["c"] = np.ascontiguousarray(c[b])
        m["ctx"] = np.ascontiguousarray(ctx[b, :CTXL])
        maps.append(m)
    return maps


def run(inputs, S, CTXL, n_cores, phases=99, debug=(), trace=False):
    bld = Builder(S, CTXL, phases=phases, debug=debug)
    nc = bld.build()
    import time as _t
    t0 = _t.time()
    maps = make_in_maps(inputs, S, CTXL, n_cores, phases)
    print("in_maps", _t.time() - t0, flush=True)
    res = run_bass_kernel_spmd(nc, maps, core_ids=list(range(n_cores)))
    print("spmd", _t.time() - t0, flush=True)
    return res, bld


def kernel(**inputs):
    res, _ = run(inputs, 8192, 256, 8)
    return np.stack([np.asarray(r["out"], dtype=np.float32) for r in res.results], axis=0)
```

```python
import numpy as np
from contextlib import ExitStack, contextmanager
import concourse.bass as bass
import concourse.mybir as mybir
from concourse.bass_utils import run_bass_kernel_spmd

F32 = mybir.dt.float32
BF16 = mybir.dt.bfloat16
I32 = mybir.dt.int32
AF = mybir.ActivationFunctionType
ALU = mybir.AluOpType
AX = mybir.AxisListType

D = 1024
NH = 8
HD = 128
NE = 32
DE = 512
EPS = 1e-6
BIG = 30000.0
GRID_W = 64


class Op:
    __slots__ = ("eng", "fn", "deps", "need", "sem", "val", "isdma", "idx")


class Prog:
    def __init__(self, nc, ndma=32):
        self.nc = nc
        self.es = ExitStack()
        self.engs = ["pe", "dve", "act", "pool", "sp"]
        self.q = {k: [] for k in self.engs}
        self.esem = {k: self.es.enter_context(nc.semaphore("s_" + k)) for k in self.engs}
        self.dsem = [self.es.enter_context(nc.semaphore("d%d" % i)) for i in range(ndma)]
        self.dlast = [None] * ndma
        self.dcount = [0] * ndma
        self.dnext = 0
        self.lastw = {}
        self.readers = {}
        self.nops = 0

    @staticmethod
    def key(x):
        if isinstance(x, (str, tuple)):
            return x
        return x.name

    def op(self, eng, fn, r=(), w=(), dma=False):
        o = Op()
        o.eng = eng
        o.fn = fn
        o.need = False
        o.isdma = dma
        o.sem = None
        o.val = None
        o.idx = self.nops
        self.nops += 1
        deps = {}
        rk = [self.key(x) for x in r]
        wk = [self.key(x) for x in w]

        def add(d):
            if d is None:
                return
            if d.isdma:
                deps[("d", id(d))] = d
            else:
                if d.eng == eng and eng == "pe" and not dma:
                    return
                cur = deps.get(("e", d.eng))
                if cur is None or cur.idx < d.idx:
                    deps[("e", d.eng)] = d

        for k in rk:
            add(self.lastw.get(k))
        for k in wk:
            add(self.lastw.get(k))
            for rd in self.readers.get(k, ()):
                add(rd)
        if dma:
            i = self.dnext
            self.dnext = (self.dnext + 1) % len(self.dsem)
            add(self.dlast[i])
            self.dcount[i] += 16
            o.sem = self.dsem[i]
            o.val = self.dcount[i]
            self.dlast[i] = o
        o.deps = list(deps.values())
        for d in o.deps:
            d.need = True
        for k in rk:
            self.readers.setdefault(k, []).append(o)
        for k in wk:
            self.lastw[k] = o
            self.readers[k] = []
        self.q[eng].append(o)
        return o

    def dma(self, eng, out, in_, r=None, w=None, **kw):
        r = [in_] if r is None else r
        w = [out] if w is None else w
        return self.op(eng, lambda e: e.dma_start(out=out, in_=in_, **kw), r=r, w=w, dma=True)

    def mm(self, out, lhsT, rhs, start=True, stop=True, r=None, w=None):
        r = [lhsT, rhs] if r is None else r
        w = [out] if w is None else w
        return self.op("pe", lambda e: e.matmul(out, lhsT, rhs, start=start, stop=stop), r=r, w=w)

    def tr(self, out, in_, ident, r=None, w=None):
        r = [in_, ident] if r is None else r
        w = [out] if w is None else w
        return self.op("pe", lambda e: e.transpose(out, in_, ident), r=r, w=w)

    def act(self, out, in_, func, bias=None, scale=None, r=None, w=None):
        rr = [in_] + ([bias] if bias is not None and not isinstance(bias, (int, float)) else [])
        r = rr if r is None else r
        w = [out] if w is None else w
        kw = {}
        if bias is not None:
            kw["bias"] = bias
        if scale is not None:
            kw["scale"] = scale
        return self.op("act", lambda e: e.activation(out=out, in_=in_, func=func, **kw), r=r, w=w)

    def tt(self, out, in0, in1, op, eng="dve", r=None, w=None):
        r = [in0, in1] if r is None else r
        w = [out] if w is None else w
        return self.op(eng, lambda e: e.tensor_tensor(out=out, in0=in0, in1=in1, op=op), r=r, w=w)

    def ts(self, out, in0, s1, op0, s2=None, op1=None, eng="dve", r=None, w=None):
        rr = [in0] + [s for s in (s1, s2) if s is not None and not isinstance(s, (int, float))]
        r = rr if r is None else r
        w = [out] if w is None else w
        if op1 is None:
            return self.op(eng, lambda e: e.tensor_scalar(out=out, in0=in0, scalar1=s1, scalar2=None, op0=op0), r=r, w=w)
        return self.op(eng, lambda e: e.tensor_scalar(out=out, in0=in0, scalar1=s1, scalar2=s2, op0=op0, op1=op1), r=r, w=w)

    def stt(self, out, in0, scalar, in1, op0, op1, r=None, w=None):
        rr = [in0, in1] + ([scalar] if not isinstance(scalar, (int, float)) else [])
        r = rr if r is None else r
        w = [out] if w is None else w
        return self.op("dve", lambda e: e.scalar_tensor_tensor(out=out, in0=in0, scalar=scalar, in1=in1, op0=op0, op1=op1), r=r, w=w)

    def copy(self, out, in_, eng="dve", r=None, w=None):
        r = [in_] if r is None else r
        w = [out] if w is None else w
        if eng == "act":
            return self.op("act", lambda e: e.copy(out=out, in_=in_), r=r, w=w)
        return self.op(eng, lambda e: e.tensor_copy(out=out, in_=in_), r=r, w=w)

    def red(self, out, in_, op, axis=AX.X, r=None, w=None):
        r = [in_] if r is None else r
        w = [out] if w is None else w
        return self.op("dve", lambda e: e.tensor_reduce(out=out, in_=in_, axis=axis, op=op), r=r, w=w)

    def memset(self, ap, val, eng="dve"):
        return self.op(eng, lambda e: e.memset(ap, val), r=[], w=[ap])

    def recip(self, out, in_, r=None, w=None):
        r = [in_] if r is None else r
        w = [out] if w is None else w
        return self.op("dve", lambda e: e.reciprocal(out=out, in_=in_), r=r, w=w)

    def barrier(self):
        lasts = [d for d in self.dlast if d is not None]
        for e in self.engs:
            for o in reversed(self.q[e]):
                if not o.isdma and o.fn is not None:
                    lasts.append(o)
                    break
        for d in lasts:
            d.need = True
        for e in self.engs:
            b = Op()
            b.eng = e
            b.fn = None
            b.need = False
            b.isdma = False
            b.sem = None
            b.val = None
            b.idx = self.nops
            self.nops += 1
            b.deps = list(lasts)
            self.q[e].append(b)
        self.lastw = {}
        self.readers = {}

    def finish(self):
        fin = Op()
        fin.eng = "sp"
        fin.fn = None
        fin.need = False
        fin.isdma = False
        fin.idx = self.nops
        deps = [d for d in self.dlast if d is not None]
        for e in self.engs:
            for o in reversed(self.q[e]):
                if not o.isdma and o.fn is not None:
                    o.need = True
                    deps.append(o)
                    break
        fin.deps = deps
        self.q["sp"].append(fin)
        for e in self.engs:
            c = 0
            for o in self.q[e]:
                if o.isdma or o.fn is None:
                    continue
                if o.need:
                    c += 1
                    o.val = c
                    o.sem = self.esem[e]
        self.counts = {e: len(self.q[e]) for e in self.engs}

        def replay(name, e):
            waited = {}
            for o in self.q[name]:
                for d in o.deps:
                    sk = id(d.sem)
                    if waited.get(sk, 0) < d.val:
                        e.wait_ge(d.sem, d.val)
                        waited[sk] = d.val
                if o.fn is None:
                    continue
                ins = o.fn(e)
                if o.isdma:
                    ins.then_inc(o.sem, 16)
                elif o.need:
                    ins.then_inc(o.sem, 1)

        nc = self.nc
        with nc.Block() as block:
            @block.tensor
            def _(e):
                replay("pe", e)

            @block.vector
            def _(e):
                replay("dve", e)

            @block.scalar
            def _(e):
                replay("act", e)

            @block.gpsimd
            def _(e):
                replay("pool", e)

            @block.sync
            def _(e):
                replay("sp", e)
        self.es.close()


class Builder:
    def __init__(self, S, CTXL, phases=99, debug=()):
        self.S = S
        self.CTXL = CTXL
        self.NT = S // 128
        self.NC = CTXL // 128
        self.T = S
        self.NBLK = (2 * S) // 128 + NE
        self.phases = phases
        self.debug = set(debug)
        nc = self.nc = bass.Bass("TRN2", target_bir_lowering=False)
        self.P = Prog(nc)
        self.top = ExitStack()
        self._n = 0
        self.regcache = {}
        self.pfi = [0]

        def din(name, shape, dt=F32):
            return nc.dram_tensor(name, list(shape), dt, kind="ExternalInput").ap()

        self.x = din("x", [S, D])
        self.c = din("c", [D])
        self.ctx = din("ctx", [CTXL, D])
        self.c_ctx = din("c_ctx", [D])
        self.ada_w = din("ada_w", [2, D, 6 * D])
        self.ada_b = din("ada_b", [2, 6 * D])
        self.norm_mix = din("norm_mix", [2, D])
        self.norm_ffn = din("norm_ffn", [2, D])
        self.w_in_a = din("w_in_a", [D, 4 * D + 32])
        self.conv_a = din("conv_a", [3, 3 * D])
        self.a_log = din("a_log", [2, NH])
        self.dt_bias = din("dt_bias", [2, NH])
        self.onorm = din("onorm", [HD])
        self.w_out_a = din("w_out_a", [D, D])
        self.w_in_b = din("w_in_b", [D, 3 * D])
        self.conv_b = din("conv_b", [3, D])
        self.w_out_b = din("w_out_b", [D, D])
        self.router = din("router", [2, D, 36])
        if phases >= 4:
            self.wall = [din("wall%d" % l, [NE * 128, 3 * 4096]) for l in range(2)]
        self.final_norm = din("final_norm", [D])
        self.out = nc.dram_tensor("out", [S, D], F32, kind="ExternalOutput").ap()

        TT = S + CTXL
        self.MODP = self.scr("modp", [16, 128, D])
        self.P0 = self.scr("p0", [TT, 4 * D + 32])
        self.OF = self.scr("of", [S, D])
        self.OB = self.scr("ob", [S, D])
        self.QKV = self.scr("qkv", [TT, 3 * D], BF16)
        self.X1 = self.scr("x1", [S, D])
        self.X2 = self.scr("x2", [S, D])
        self.X3 = self.scr("x3", [S, D])
        self.HF = self.scr("hfb", [S, D], BF16)
        self.XB = self.scr("xb", [self.NBLK * 128, D], BF16)
        self.YB = self.scr("yb", [self.NBLK * 128, D])
        self.U1 = self.scr("u1", [S, D])
        self.GB = self.scr("gb", [S, D])

    @contextmanager
    def scope(self):
        with ExitStack() as es:
            yield es
            self.P.barrier()

    def run2(self, n, body, lag=0):
        def stream(par):
            for i in range(par, n, 2):
                yield from body(i)
        gens = [stream(0), stream(1)]
        for _ in range(lag):
            next(gens[0])
        while gens:
            for g in list(gens):
                try:
                    next(g)
                except StopIteration:
                    gens.remove(g)

    def scr(self, name, shape, dt=F32):
        kind = "ExternalOutput" if name in self.debug else "Internal"
        return self.nc.dram_tensor(name, list(shape), dt, kind=kind).ap()

    def sb(self, es, name, shape, dt=F32):
        self._n += 1
        return es.enter_context(self.nc.sbuf_tensor("%s_%d" % (name, self._n), list(shape), dt))

    def build(self):
        nc, P = self.nc, self.P
        top = self.top
        self.pf = [top.enter_context(nc.psum_tensor("pf%d" % i, [128, 512], F32)) for i in range(6)]
        self.pt = [top.enter_context(nc.psum_tensor("pt%d" % i, [128, 8, 128], BF16)) for i in range(2)]
        self.consts()
        self.phase_ada()
        if self.phases >= 1:
            self.phase_projA()
        if self.phases >= 2:
            self.phase_qkv()
        if self.phases >= 3:
            self.phase_delta_both()
            self.phase_finish()
        if self.phases >= 4:
            self.phase_moe(0, self.X1, self.X2, final=False)
        if self.phases >= 5:
            self.phase_convD()
        if self.phases >= 6:
            self.phase_convE()
        if self.phases >= 7:
            self.phase_moe(1, self.X3, self.out, final=True)
        P.finish()
        top.close()
        return nc

    def consts(self):
        nc, P, top = self.nc, self.P, self.top
        sb = lambda n, s, d=F32: self.sb(top, n, s, d)
        rowi = sb("rowi", [128, 128])
        coli = sb("coli", [128, 128])
        P.op("pool", lambda e: e.iota(rowi[:], pattern=[[0, 128]], base=0, channel_multiplier=1,
                                       allow_small_or_imprecise_dtypes=True), w=[rowi])
        P.op("pool", lambda e: e.iota(coli[:], pattern=[[1, 128]], base=0, channel_multiplier=0,
                                       allow_small_or_imprecise_dtypes=True), w=[coli])
        self.rowi, self.coli = rowi, coli
        self.ident_f = sb("ident_f", [128, 128])
        self.ident_b = sb("ident_b", [128, 128], BF16)
        self.ones_f = sb("ones_f", [128, 128])
        self.ones_b = sb("ones_b", [128, 128], BF16)
        P.tt(self.ident_f[:], rowi[:], coli[:], ALU.is_equal)
        P.copy(self.ident_b[:], self.ident_f[:])
        P.memset(self.ones_f[:], 1.0)
        P.memset(self.ones_b[:], 1.0)
        self.U = [sb("Uf", [128, 128]), sb("Ub", [128, 128])]
        P.tt(self.U[0][:], rowi[:], coli[:], ALU.is_le)
        P.tt(self.U[1][:], rowi[:], coli[:], ALU.is_ge)
        self.SM = [sb("SMf", [128, 128]), sb("SMb", [128, 128])]
        P.tt(self.SM[0][:], rowi[:], coli[:], ALU.is_gt)
        P.tt(self.SM[1][:], rowi[:], coli[:], ALU.is_lt)
        self.BM = [sb("BMf", [128, 4, 128]), sb("BMb", [128, 4, 128])]
        for d in range(2):
            for h in range(4):
                P.ts(self.BM[d][:, h, :], self.SM[1 - d][:], BIG, ALU.mult)
        self.Us_b = sb("Us_b", [128, 128], BF16)
        P.copy(self.Us_b[:], self.SM[1][:])
        bd = {}
        colb = sb("colb", [128, 128]); rowb = sb("rowb", [128, 128])
        for sz in (8, 16, 32, 64):
            P.op("pool", lambda e, sz=sz: e.iota(colb[:], pattern=[[1, 128 // sz], [0, sz]], base=0, channel_multiplier=0,
                                                  allow_small_or_imprecise_dtypes=True), w=[colb])
            P.tr(self.pf[0][:, 0:128], colb[:], self.ident_f[:])
            P.copy(rowb[:], self.pf[0][:, 0:128])
            bd[sz] = sb("bd%d" % sz, [128, 128])
            P.tt(bd[sz][:], rowb[:], colb[:], ALU.is_equal)
        self.BD8 = sb("BD8b", [128, 128], BF16)
        P.copy(self.BD8[:], bd[8][:])
        self.MS = {}
        for sz in (8, 16, 32, 64):
            self.MS[sz] = sb("MS%d" % sz, [128, 128], BF16)
            if sz < 64:
                P.tt(self.MS[sz][:], bd[2 * sz][:], bd[sz][:], ALU.subtract)
            else:
                P.ts(self.MS[sz][:], bd[sz][:], -1.0, ALU.mult, 1.0, ALU.add)
        self.epsc = sb("epsc", [128, 1])
        P.memset(self.epsc[:], EPS)

    def evac(self, out, in_, i):
        if i % 2 == 0:
            return self.P.copy(out, in_, eng="dve")
        return self.P.copy(out, in_, eng="act")

    def load_cast(self, es, name, src_ap, ncols, chunk=512, kparts=8, qeng="sp"):
        P = self.P
        wb = self.sb(es, name, [128, kparts, ncols], BF16)
        with self.scope() as tmp:
            st = [self.sb(tmp, name + "_st", [128, kparts, chunk]) for _ in range(2)]
            src = src_ap.rearrange("(kc p) n -> p kc n", p=128)
            i = 0
            for c0 in range(0, ncols, chunk):
                cw = min(chunk, ncols - c0)
                s = st[i % 2]
                P.dma(qeng, s[:, :, 0:cw], src[:, :, c0:c0 + cw])
                if i % 2 == 0:
                    P.copy(wb[:, :, c0:c0 + cw], s[:, :, 0:cw], eng="dve")
                else:
                    P.copy(wb[:, :, c0:c0 + cw], s[:, :, 0:cw], eng="pool")
                i += 1
        return wb

    def bcast_load(self, es, name, vec_ap, n):
        t = self.sb(es, name, [128, n])
        self.P.dma("sp", t[:], vec_ap.partition_broadcast(128))
        return t

    def modulate(self, xt, A, B, sq, ss, hf, hb):
        P = self.P
        P.act(sq[:], xt[:], AF.Square)
        P.red(ss[:, 0:1], sq[:], ALU.add)
        P.ts(ss[:, 1:2], ss[:, 0:1], 1.0 / D, ALU.mult, EPS, ALU.add)
        P.act(ss[:, 2:3], ss[:, 1:2], AF.Sqrt)
        P.recip(ss[:, 3:4], ss[:, 2:3])
        P.stt(hf[:], xt[:], ss[:, 3:4], A[:], ALU.mult, ALU.mult)
        if hb is not None:
            P.tt(hb[:], hf[:], B[:], ALU.add)
        else:
            P.tt(hf[:], hf[:], B[:], ALU.add)

    def transpose8(self, dstT, src_bf, ptile, i=0, n=8):
        P = self.P
        for kc in range(n):
            P.tr(ptile[:, kc, :], src_bf[:, kc * 128:(kc + 1) * 128], self.ident_b[:])
        self.evac(dstT[:, 0:n, :], ptile[:, 0:n, :], i)

    def phase_ada(self):
        nc, P = self.nc, self.P
        with self.scope() as es:
            sb = lambda n, s, d=F32: self.sb(es, n, s, d)
            craw = sb("craw", [128, 2, 8])
            P.dma("sp", craw[:, 0, :], self.c.rearrange("(kc p) -> p kc", p=128), allow_slow_non_contiguous=True)
            P.dma("sp", craw[:, 1, :], self.c_ctx.rearrange("(kc p) -> p kc", p=128), allow_slow_non_contiguous=True)
            csil = sb("csil", [128, 2, 8])
            P.act(csil[:], craw[:], AF.Silu)
            cb = sb("cb", [128, 2, 8, 128])
            for j in range(2):
                P.copy(cb[:, j, :, :], csil[:, j, :].unsqueeze(2).to_broadcast([128, 8, 128]))
            wst = [sb("adaw", [128, 8, 512]) for _ in range(2)]
            mb = sb("mb", [128, 6 * D])
            bb = sb("bb", [128, 6 * D])
            nm = sb("nm", [128, D])
            res = [sb("res", [128, D]) for _ in range(2)]
            cnt = 0
            for (layer, j) in ((0, 0), (0, 1), (1, 0)):
                P.dma("sp", bb[:], self.ada_b[layer].partition_broadcast(128))
                for n in range(12):
                    w = wst[cnt % 2]
                    P.dma("sp", w[:], self.ada_w[layer].rearrange("(kc p) n -> p kc n", p=128)[:, :, n * 512:(n + 1) * 512])
                    ps = self.pf[cnt % 2]
                    for kc in range(8):
                        P.mm(ps[:], cb[:, j, kc, :], w[:, kc, :], start=(kc == 0), stop=(kc == 7))
                    P.tt(mb[:, n * 512:(n + 1) * 512], ps[:], bb[:, n * 512:(n + 1) * 512], ALU.add)
                    cnt += 1
                outs = []
                for sub, (nrm, base) in enumerate(((self.norm_mix, 0), (self.norm_ffn, 3))):
                    if j == 1 and sub == 1:
                        continue
                    P.dma("sp", nm[:], nrm[layer].partition_broadcast(128))
                    r0 = res[0]
                    P.stt(r0[:], mb[:, (base + 1) * D:(base + 2) * D], 1.0, nm[:], ALU.add, ALU.mult)
                    ka = (12 if j == 1 else 6 * layer + base)
                    P.dma("sp", self.MODP[ka], r0[:], w=[("modp", ka)])
                    P.dma("sp", self.MODP[ka + 1], mb[:, base * D:(base + 1) * D], w=[("modp", ka + 1)])
                    if j == 0:
                        P.dma("sp", self.MODP[ka + 2], mb[:, (base + 2) * D:(base + 3) * D], w=[("modp", ka + 2)])

    def load_mod(self, es, name, k):
        t = self.sb(es, name, [128, D])
        self.P.dma("sp", t[:], self.MODP[k], r=[("modp", k)])
        return t

    def phase_projA(self):
        nc, P = self.nc, self.P
        NCOL = 4 * D + 32
        with self.scope() as es:
            sb = lambda n, s, d=F32: self.sb(es, n, s, d)
            wb = self.load_cast(es, "wina", self.w_in_a, NCOL)
            A = [self.load_mod(es, "A", 0), self.load_mod(es, "Ac", 12)]
            B = [self.load_mod(es, "B", 1), self.load_mod(es, "Bc", 13)]
            xt = [sb("xt", [128, D]) for _ in range(2)]
            sq = sb("sq", [128, D])
            ss = sb("ss", [128, 4])
            hf = sb("hf", [128, D])
            hb = sb("hb", [128, D], BF16)
            hT = [sb("hT", [128, 8, 128], BF16) for _ in range(2)]
            po = [sb("po", [128, NCOL]) for _ in range(2)]
            tiles = [("c", i) for i in range(self.NC)] + [("l", i) for i in range(self.NT)]
            for ti, (kind, i) in enumerate(tiles):
                src = self.ctx if kind == "c" else self.x
                j = 1 if kind == "c" else 0
                row0 = i * 128 + (0 if kind == "c" else self.CTXL)
                x_ = xt[ti % 2]
                P.dma("sp", x_[:], src[i * 128:(i + 1) * 128, :])
                self.modulate(x_, A[j], B[j], sq, ss, hf, hb)
                hT_ = hT[ti % 2]
                self.transpose8(hT_, hb, self.pt[ti % 2], ti)
                po_ = po[ti % 2]
                g = 0
                for c0 in range(0, NCOL, 512):
                    cw = min(512, NCOL - c0)
                    ps = self.pf[g % 4]
                    for kc in range(8):
                        P.mm(ps[:, 0:cw], hT_[:, kc, :], wb[:, kc, c0:c0 + cw], start=(kc == 0), stop=(kc == 7))
                    self.evac(po_[:, c0:c0 + cw], ps[:, 0:cw], g)
                    g += 1
                P.dma("pool", self.P0[row0:row0 + 128, :], po_[:], w=[("p0", row0 // 128)])

    def phase_qkv(self):
        nc, P = self.nc, self.P
        CT = self.CTXL
        with self.scope() as es:
            sb = lambda n, s, dt=F32: self.sb(es, n, s, dt)
            cw = [self.bcast_load(es, "cw%d" % s, self.conv_a[s], 3 * D) for s in range(3)]
            pm_ = [sb("pm", [128, 3 * D]) for _ in range(2)]
            pc_ = [sb("pc", [128, 3 * D]) for _ in range(2)]
            pp_ = [sb("pp", [128, 3 * D]) for _ in range(2)]
            qo_ = [sb("qo", [128, 3 * D], BF16) for _ in range(2)]
            so_ = [sb("so", [128, 3 * D]) for _ in range(2)]
            ssq_ = [sb("ssq", [128, 64]) for _ in range(2)]
            bc = lambda ap: ap.unsqueeze(2).to_broadcast([128, ap.shape[1], HD])
            tiles = [("c", i) for i in range(self.NC)] + [("l", i) for i in range(self.NT)]

            def loads(ti):
                if ti >= len(tiles):
                    return
                kind, i = tiles[ti]
                pm, pc, pp = pm_[ti % 2], pc_[ti % 2], pp_[ti % 2]
                ntile = self.NC if kind == "c" else self.NT
                row0 = (0 if kind == "c" else CT) + i * 128
                P.dma("sp", pc[:], self.P0[row0:row0 + 128, 0:3 * D])
                if i == 0:
                    P.memset(pm[:], 0.0, eng="pool")
                    P.dma("sp", pm[1:128, :], self.P0[row0:row0 + 127, 0:3 * D], w=[pm])
                else:
                    P.dma("sp", pm[:], self.P0[row0 - 1:row0 + 127, 0:3 * D])
                if i == ntile - 1:
                    P.memset(pp[:], 0.0, eng="pool")
                    P.dma("sp", pp[0:127, :], self.P0[row0 + 1:row0 + 128, 0:3 * D], w=[pp])
                else:
                    P.dma("sp", pp[:], self.P0[row0 + 1:row0 + 129, 0:3 * D])

            loads(0)
            loads(1)

            def qbody(ti):
                kind, i = tiles[ti]
                pm, pc, pp, qo, ssq = pm_[ti % 2], pc_[ti % 2], pp_[ti % 2], qo_[ti % 2], ssq_[ti % 2]
                so = so_[ti % 2]
                row0 = (0 if kind == "c" else CT) + i * 128
                P.tt(pm[:], pm[:], cw[0][:], ALU.mult)
                P.tt(pc[:], pc[:], cw[1][:], ALU.mult)
                P.tt(pp[:], pp[:], cw[2][:], ALU.mult)
                yield
                for c in range(6):
                    ps = self.pf[c]
                    cs = slice(c * 512, (c + 1) * 512)
                    P.mm(ps[:], self.ident_f[:], pm[:, cs], start=True, stop=False)
                    P.mm(ps[:], self.ident_f[:], pc[:, cs], start=False, stop=False)
                    P.mm(ps[:], self.ident_f[:], pp[:, cs], start=False, stop=True)
                    P.act(so[:, cs], ps[:], AF.Silu)
                    if c % 2 == 1:
                        yield
                P.act(pm[:, 0:2 * D], so[:, 0:2 * D], AF.Square)
                yield
                P.red(ssq[:, 0:16], pm[:, 0:2 * D].rearrange("p (h e) -> p h e", e=HD), ALU.add)
                P.ts(ssq[:, 16:32], ssq[:, 0:16], EPS, ALU.add)
                P.act(ssq[:, 32:48], ssq[:, 16:32], AF.Sqrt)
                P.recip(ssq[:, 48:64], ssq[:, 32:48])
                P.ts(ssq[:, 48:56], ssq[:, 48:56], HD ** -0.5, ALU.mult)
                P.tt(qo[:, 0:D].rearrange("p (h e) -> p h e", e=HD), so[:, 0:D].rearrange("p (h e) -> p h e", e=HD),
                     bc(ssq[:, 48:56]), ALU.mult)
                P.tt(qo[:, D:2 * D].rearrange("p (h e) -> p h e", e=HD), so[:, D:2 * D].rearrange("p (h e) -> p h e", e=HD),
                     bc(ssq[:, 56:64]), ALU.mult, eng="pool")
                P.copy(qo[:, 2 * D:3 * D], so[:, 2 * D:3 * D], eng="act")
                yield
                loads(ti + 2)
                P.dma("pool", self.QKV[row0:row0 + 128, :], qo[:])
                yield
            self.run2(len(tiles), qbody)

    def phase_delta_both(self):
        with self.scope() as es:
            gens = [self.delta_gen(es, 0), self.delta_gen(es, 1)]
            while gens:
                for g in list(gens):
                    try:
                        next(g)
                    except StopIteration:
                        gens.remove(g)

    def delta_gen(self, es, d):
        nc, P = self.nc, self.P
        CT = self.CTXL
        sb = lambda n, s, dt=F32: self.sb(es, n, s, dt)
        nA = self.bcast_load(es, "nA", self.a_log[d], NH)
        P.act(nA[:], nA[:], AF.Exp)
        P.ts(nA[:], nA[:], -1.0, ALU.mult)
        dtb = self.bcast_load(es, "dtb", self.dt_bias[d], NH)
        qv = sb("qv", [128, 3 * D], BF16)
        gts = sb("gts", [128, 32])
        sml = sb("sml", [128, 16 * NH])
        kT = sb("kT", [128, NH, HD], BF16)
        qT = sb("qT", [128, NH, HD], BF16)
        qd_b = sb("qd_b", [128, NH, HD], BF16)
        qdT = sb("qdT", [128, NH, HD], BF16)
        vb = sb("vb", [128, NH, HD], BF16)
        kbe = sb("kbe", [128, NH, HD], BF16)
        kdec = sb("kdec", [128, NH, HD], BF16)
        rhsR = sb("rhsR", [128, NH, HD])
        Dm = sb("Dm", [128, NH, HD])
        As = sb("As", [128, NH, HD], BF16)
        qk_b = sb("qk_b", [128, NH, HD], BF16)
        qkT = sb("qkT", [128, NH, HD], BF16)
        L_b = sb("L_b", [128, NH, HD], BF16); N_b = sb("N_b", [128, NH, HD], BF16)
        Ld = sb("Ld", [128, NH, HD], BF16); Nd = sb("Nd", [128, NH, HD], BF16)
        P1 = sb("P1", [128, NH, HD], BF16); Q1 = sb("Q1", [128, NH, HD], BF16)
        P2 = sb("P2", [128, NH, HD], BF16); Q2 = sb("Q2", [128, NH, HD], BF16)
        Tw = [sb("Tw", [128, NH, HD], BF16) for _ in range(2)]
        Xw = [sb("Xw", [128, NH, HD], BF16) for _ in range(2)]
        u32 = sb("u32", [128, NH, HD])
        wT = sb("wT", [128, NH, HD], BF16)
        vnew = sb("vnew", [128, NH, HD], BF16)
        S32 = sb("S32", [128, NH, HD])
        Sb = sb("Sb", [128, NH, HD], BF16)
        Stmp = sb("Stmp", [128, NH, HD])
        o32 = sb("o32", [128, NH, HD])
        P.memset(S32[:], 0.0)
        P.memset(Sb[:], 0.0, eng="pool")
        OUT = self.OF if d == 0 else self.OB
        ptd = self.pt[d]
        if d == 0:
            order = [("c", i) for i in range(self.NC)] + [("l", i) for i in range(self.NT)]
        else:
            order = [("c", i) for i in reversed(range(self.NC))] + [("l", i) for i in reversed(range(self.NT))]
        bc = lambda ap: ap.unsqueeze(2).to_broadcast([128, ap.shape[1], HD])
        mb = lambda m: m[:].unsqueeze(1).to_broadcast([128, NH, HD])
        v4 = lambda ps: ps[:].rearrange("p (h e) -> p h e", e=HD)
        pfi = self.pfi

        def nextpf():
            pfi[0] += 1
            return self.pf[pfi[0] % 6]

        def mm4(lhs, rhs, hg):
            ps = nextpf()
            for hh in range(4):
                h = hg * 4 + hh
                P.mm(ps[:, hh * 128:(hh + 1) * 128], lhs[:, h, :], rhs[:, h, :])
            return ps

        for ti, (kind, i) in enumerate(order):
            row0 = (0 if kind == "c" else CT) + i * 128
            P.dma("sp", qv[:], self.QKV[row0:row0 + 128, :])
            P.dma("sp", gts[:], self.P0[row0:row0 + 128, 4 * D:4 * D + 32])
            qn_b = qv[:, 0:D].rearrange("p (h e) -> p h e", e=HD)
            kn_b = qv[:, D:2 * D].rearrange("p (h e) -> p h e", e=HD)
            v_b = qv[:, 2 * D:3 * D].rearrange("p (h e) -> p h e", e=HD)
            yield
            beta = sml[:, 0:8]
            P.act(beta, gts[:, d * 8:d * 8 + 8], AF.Sigmoid)
            xg = sml[:, 8:16]
            P.tt(xg, gts[:, 16 + d * 8:16 + d * 8 + 8], dtb[:], ALU.add)
            P.stt(sml[:, 16:24], xg, -1.0, xg, ALU.mult, ALU.max)
            P.act(sml[:, 24:32], sml[:, 16:24], AF.Exp, scale=-1.0)
            P.act(sml[:, 32:40], sml[:, 24:32], AF.Ln, bias=1.0)
            P.stt(sml[:, 40:48], xg, 0.0, sml[:, 32:40], ALU.max, ALU.add)
            g = sml[:, 48:56]
            P.tt(g, sml[:, 40:48], nA[:], ALU.mult)
            yield
            ps = nextpf()
            P.mm(ps[:, 0:8], self.U[d][:], g)
            P.mm(ps[:, 8:16], self.ones_f[:], g)
            gc = sml[:, 56:64]
            gl = sml[:, 64:72]
            P.copy(sml[:, 56:72], ps[:, 0:16])
            for (src, dst) in ((kn_b, kT), (qn_b, qT)):
                for h in range(NH):
                    P.tr(ptd[:, h, :], src[:, h, :], self.ident_b[:])
                self.evac(dst[:], ptd[:], 1)
                yield
            egc = sml[:, 72:80]
            P.act(egc, gc, AF.Exp)
            egl = sml[:, 80:88]
            P.act(egl, gl, AF.Exp)
            kdc = sml[:, 88:96]
            P.tt(kdc, gl, gc, ALU.subtract)
            P.act(kdc, kdc, AF.Exp)
            bq = sml[:, 96:104]
            P.tt(bq, beta, egc, ALU.mult)
            yield
            P.tt(vb[:], v_b, bc(beta), ALU.mult)
            P.tt(kbe[:], kn_b, bc(bq), ALU.mult)
            P.tt(kdec[:], kn_b, bc(kdc), ALU.mult)
            P.tt(qd_b[:], qn_b, bc(egc), ALU.mult)
            P.tt(rhsR[:], self.U[d][:].unsqueeze(1).to_broadcast([128, NH, HD]), bc(g), ALU.mult)
            yield
            for h in range(NH):
                P.tr(ptd[:, h, :], qd_b[:, h, :], self.ident_b[:])
            self.evac(qdT[:], ptd[:], 1)
            yield
            for hg in range(2):
                ps = nextpf()
                P.mm(ps[:], self.ones_f[:], rhsR[:, hg * 4:(hg + 1) * 4, :].rearrange("p h e -> p (h e)"), start=True, stop=False)
                P.mm(ps[:], self.ident_f[:], self.BM[d][:].rearrange("p h e -> p (h e)"), start=False, stop=True)
                for hh in range(4):
                    h = hg * 4 + hh
                    P.act(Dm[:, h, :], ps[:, hh * 128:(hh + 1) * 128], AF.Exp, bias=gc[:, h:h + 1], scale=-1.0)
                yield
            for hg in range(2):
                hs = slice(hg * 4, (hg + 1) * 4)
                P.tt(As[:, hs, :], v4(mm4(kT, kT, hg)), self.SM[d][:].unsqueeze(1).to_broadcast([128, 4, HD]), ALU.mult)
                P.tt(qk_b[:, hs, :], v4(mm4(qT, kT, hg)), Dm[:, hs, :], ALU.mult)
                yield
            for h in range(NH):
                P.stt(L_b[:, h, :], As[:, h, :], beta[:, h:h + 1], Dm[:, h, :], ALU.mult, ALU.mult)
            yield
            for (src, dst) in ((L_b, N_b), (qk_b, qkT)):
                for h in range(NH):
                    P.tr(ptd[:, h, :], src[:, h, :], self.ident_b[:])
                self.evac(dst[:], ptd[:], 1)
                yield
            P.tt(Ld[:], L_b[:], mb(self.BD8), ALU.mult)
            P.tt(Nd[:], N_b[:], mb(self.BD8), ALU.mult)
            P.tt(Tw[0][:], mb(self.ident_b), Ld[:], ALU.subtract)
            P.tt(Xw[0][:], mb(self.ident_b), Nd[:], ALU.subtract)
            yield
            cur = 0
            for (Pa, Qa, Pprev, Qprev) in ((P1, Q1, Ld, Nd), (P2, Q2, P1, Q1)):
                for hg in range(2):
                    hs = slice(hg * 4, (hg + 1) * 4)
                    self.evac(Pa[:, hs, :], v4(mm4(Qprev, Pprev, hg)), 1)
                    self.evac(Qa[:, hs, :], v4(mm4(Pprev, Qprev, hg)), 1)
                yield
                nxt = 1 - cur
                for hg in range(2):
                    hs = slice(hg * 4, (hg + 1) * 4)
                    P.tt(Tw[nxt][:, hs, :], Tw[cur][:, hs, :], v4(mm4(Qa, Tw[cur], hg)), ALU.add)
                    P.tt(Xw[nxt][:, hs, :], Xw[cur][:, hs, :], v4(mm4(Pa, Xw[cur], hg)), ALU.add)
                yield
                cur = nxt
            Cs, CTs, Zb, Zpb = Ld, Nd, P1, Q1
            for sz in (8, 16, 32, 64):
                P.tt(Cs[:], L_b[:], mb(self.MS[sz]), ALU.mult)
                P.tt(CTs[:], N_b[:], mb(self.MS[sz]), ALU.mult)
                for hg in range(2):
                    hs = slice(hg * 4, (hg + 1) * 4)
                    self.evac(Zb[:, hs, :], v4(mm4(Cs, Xw[cur], hg)), 1)
                    if sz < 64:
                        self.evac(Zpb[:, hs, :], v4(mm4(CTs, Tw[cur], hg)), 1)
                yield
                nxt = 1 - cur
                for hg in range(2):
                    hs = slice(hg * 4, (hg + 1) * 4)
                    P.tt(Xw[nxt][:, hs, :], Xw[cur][:, hs, :], v4(mm4(Tw[cur], Zb, hg)), ALU.subtract)
                    if sz < 64:
                        P.tt(Tw[nxt][:, hs, :], Tw[cur][:, hs, :], v4(mm4(Xw[cur], Zpb, hg)), ALU.subtract)
                yield
                cur = nxt
            TTm = Xw[cur]
            for hg in range(2):
                hs = slice(hg * 4, (hg + 1) * 4)
                self.evac(u32[:, hs, :], v4(mm4(TTm, vb, hg)), 1)
                self.evac(wT[:, hs, :], v4(mm4(kbe, TTm, hg)), 1)
            yield
            for hg in range(2):
                hs = slice(hg * 4, (hg + 1) * 4)
                P.tt(vnew[:, hs, :], u32[:, hs, :], v4(mm4(wT, Sb, hg)), ALU.subtract)
                yield
                if kind == "l":
                    ps2 = nextpf()
                    for hh in range(4):
                        h = hg * 4 + hh
                        o_ = ps2[:, hh * 128:(hh + 1) * 128]
                        P.mm(o_, qdT[:, h, :], Sb[:, h, :], start=True, stop=False)
                        P.mm(o_, qkT[:, h, :], vnew[:, h, :], start=False, stop=True)
                    self.evac(o32[:, hs, :], v4(ps2), 1)
                ps3 = mm4(kdec, vnew, hg)
                P.tt(Stmp[:, hs, :], S32[:, hs, :], bc(egl[:, hs]), ALU.mult)
                P.tt(S32[:, hs, :], Stmp[:, hs, :], v4(ps3), ALU.add)
                P.copy(Sb[:, hs, :], S32[:, hs, :], eng="act")
                yield
            if kind == "l":
                P.dma("pool", OUT[i * 128:(i + 1) * 128, :], o32[:].rearrange("p h e -> p (h e)"))
            yield

    def phase_finish(self):
        nc, P = self.nc, self.P
        CT = self.CTXL
        with self.scope() as es:
            sb = lambda n, s, dt=F32: self.sb(es, n, s, dt)
            onw = self.bcast_load(es, "onw", self.onorm, HD)
            wo = self.load_cast(es, "wouta", self.w_out_a, D)
            Gm = self.load_mod(es, "Gm", 2)
            of_ = [sb("of_", [128, NH, HD]) for _ in range(2)]
            ob_ = [sb("ob_", [128, NH, HD]) for _ in range(2)]
            zt_ = [sb("zt", [128, D]) for _ in range(2)]
            xt_ = [sb("xt", [128, D]) for _ in range(2)]
            osq_ = [sb("osq", [128, NH, HD]) for _ in range(2)]
            orn_ = [sb("orn", [128, 4 * NH]) for _ in range(2)]
            yg_ = [sb("yg", [128, D], BF16) for _ in range(2)]
            ygT = [sb("ygT", [128, 8, 128], BF16) for _ in range(2)]
            x1t = [sb("x1t", [128, D]) for _ in range(2)]
            bc = lambda ap: ap.unsqueeze(2).to_broadcast([128, ap.shape[1], HD])

            def loads(i):
                if i >= self.NT:
                    return
                sl = i % 2
                P.dma("sp", of_[sl][:].rearrange("p h e -> p (h e)"), self.OF[i * 128:(i + 1) * 128, :])
                P.dma("sp", ob_[sl][:].rearrange("p h e -> p (h e)"), self.OB[i * 128:(i + 1) * 128, :])
                P.dma("sp", zt_[sl][:], self.P0[CT + i * 128:CT + (i + 1) * 128, 3 * D:4 * D])
                P.dma("sp", xt_[sl][:], self.x[i * 128:(i + 1) * 128, :])

            loads(0)
            loads(1)

            def fbody(i):
                sl = i % 2
                o32, ob, zt, xt = of_[sl], ob_[sl], zt_[sl], xt_[sl]
                osq, orn, yg = osq_[sl], orn_[sl], yg_[sl]
                P.tt(o32[:], o32[:], ob[:], ALU.add)
                yield
                P.act(osq[:], o32[:], AF.Square)
                P.red(orn[:, 0:8], osq[:], ALU.add)
                P.ts(orn[:, 8:16], orn[:, 0:8], 1.0 / HD, ALU.mult, EPS, ALU.add)
                P.act(orn[:, 16:24], orn[:, 8:16], AF.Sqrt)
                P.recip(orn[:, 24:32], orn[:, 16:24])
                yield
                P.tt(osq[:], o32[:], bc(orn[:, 24:32]), ALU.mult)
                P.tt(osq[:], osq[:], onw[:].unsqueeze(1).to_broadcast([128, NH, HD]), ALU.mult)
                P.act(zt[:], zt[:], AF.Silu)
                P.tt(yg[:], osq[:].rearrange("p h e -> p (h e)"), zt[:], ALU.mult)
                yield
                self.transpose8(ygT[sl], yg, self.pt[sl], i)
                yield
                x1 = x1t[sl]
                for half in range(2):
                    ps = self.pf[(2 * i + half) % 6]
                    for kc in range(8):
                        P.mm(ps[:], ygT[sl][:, kc, :], wo[:, kc, half * 512:(half + 1) * 512], start=(kc == 0), stop=(kc == 7))
                    cs = slice(half * 512, (half + 1) * 512)
                    P.tt(x1[:, cs], ps[:], Gm[:, cs], ALU.mult)
                    P.tt(x1[:, cs], x1[:, cs], xt[:, cs], ALU.add)
                loads(i + 2)
                yield
                P.dma("pool", self.X1[i * 128:(i + 1) * 128, :], x1[:], w=[("x1", i)])
                yield
            self.run2(self.NT, fbody)

    def phase_moe(self, layer, XIN, XOUT, final):
        nc, P = self.nc, self.P
        NT, NB = self.NT, self.NBLK
        xin_key = "x1" if layer == 0 else "x3"
        with self.scope() as es:
            sb = lambda n, s, dt=F32: self.sb(es, n, s, dt)
            A = self.load_mod(es, "Af", 6 * layer + 3)
            B = self.load_mod(es, "Bf", 6 * layer + 4)
            G = self.load_mod(es, "Gf", 6 * layer + 5)
            rt = sb("rt", [128, 8, 36])
            P.dma("sp", rt[:], self.router[layer].rearrange("(kc p) n -> p kc n", p=128))
            E1 = sb("E1", [128, NT]); E2 = sb("E2", [128, NT])
            R1 = sb("R1", [128, NT]); R2 = sb("R2", [128, NT])
            W1 = sb("W1", [128, NT]); W2 = sb("W2", [128, NT])
            carry = sb("carry", [128, NE])
            P.memset(carry[:], 0.0)
            iotaE = sb("iotaE", [128, NE])
            P.copy(iotaE[:], self.coli[:, 0:NE])
            with self.scope() as e1:
                s1 = lambda n, s, dt=F32: self.sb(e1, n, s, dt)
                xt = [s1("xt", [128, D]) for _ in range(2)]
                bufs = []
                for _ in range(2):
                    bufs.append(dict(
                        sq=s1("sq", [128, D]), ss=s1("ss", [128, 4]), hf=s1("hf", [128, D]), hb=s1("hb", [128, D], BF16),
                        hT=s1("hT", [128, 8, 128]), lg=s1("lg", [128, 36]), sm=s1("sm", [128, 64]),
                        lem=s1("lem", [128, NE]), top8=s1("top8", [128, 8]), m1=s1("m1", [128, NE]),
                        m12=s1("m12", [128, NE]), m2=s1("m2", [128, NE]), m12b=s1("m12b", [128, NE], BF16),
                        pos=s1("pos", [128, NE]), tmp=s1("tmp", [128, NE])))

                def rt_body(i):
                    bb = bufs[i % 2]
                    sq, ss, hf, hb, hT, lg, sm = bb["sq"], bb["ss"], bb["hf"], bb["hb"], bb["hT"], bb["lg"], bb["sm"]
                    lem, top8, m1, m12, m2, m12b, pos, tmp = bb["lem"], bb["top8"], bb["m1"], bb["m12"], bb["m2"], bb["m12b"], bb["pos"], bb["tmp"]
                    x_ = xt[i % 2]
                    P.dma("sp", x_[:], XIN[i * 128:(i + 1) * 128, :], r=[(xin_key, i)])
                    self.modulate(x_, A, B, sq, ss, hf, None)
                    P.copy(hb[:], hf[:], eng="act")
                    P.dma("pool", self.HF[i * 128:(i + 1) * 128, :], hb[:], w=[("hf", i)])
                    yield
                    for half in range(2):
                        ps = self.pf[half]
                        for kk in range(4):
                            kc = half * 4 + kk
                            P.tr(ps[:, kk * 128:(kk + 1) * 128], hf[:, kc * 128:(kc + 1) * 128], self.ident_f[:])
                        self.evac(hT[:, half * 4:(half + 1) * 4, :], ps[:].rearrange("p (k e) -> p k e", e=128), half)
                    yield
                    ps = self.pf[2 + 2 * (i % 2)]
                    for kc in range(8):
                        P.mm(ps[:, 0:36], hT[:, kc, :], rt[:, kc, :], start=(kc == 0), stop=(kc == 7))
                    P.copy(lg[:], ps[:, 0:36])
                    yield
                    P.red(sm[:, 0:1], lg[:, 0:4], ALU.max)
                    P.ts(sm[:, 1:2], sm[:, 0:1], -1.0, ALU.mult)
                    P.act(sm[:, 4:8], lg[:, 0:4], AF.Exp, bias=sm[:, 1:2])
                    P.red(sm[:, 2:3], sm[:, 4:8], ALU.add)
                    P.recip(sm[:, 3:4], sm[:, 2:3])
                    P.ts(sm[:, 8:12], lg[:, 0:4], sm[:, 0:1], ALU.is_ge)
                    P.ts(sm[:, 12:16], sm[:, 8:12], BIG, ALU.mult, -BIG, ALU.add)
                    P.tt(lem[:].rearrange("p (g e) -> p g e", e=8), lg[:, 4:36].rearrange("p (g e) -> p g e", e=8),
                         sm[:, 12:16].unsqueeze(2).to_broadcast([128, 4, 8]), ALU.add)
                    yield
                    P.op("dve", lambda e, o=top8, i_=lem: e.max(out=o[:], in_=i_[:]), r=[lem], w=[top8])
                    P.ts(m1[:], lem[:], top8[:, 0:1], ALU.is_ge)
                    P.ts(m12[:], lem[:], top8[:, 1:2], ALU.is_ge)
                    P.tt(m2[:], m12[:], m1[:], ALU.subtract)
                    P.copy(m12b[:], m12[:], eng="act")
                    yield
                    P.tt(sm[:, 16:17], top8[:, 0:1], top8[:, 1:2], ALU.subtract)
                    P.act(sm[:, 17:18], sm[:, 16:17], AF.Sigmoid)
                    P.tt(W1[:, i:i + 1], sm[:, 17:18], sm[:, 3:4], ALU.mult)
                    P.tt(W2[:, i:i + 1], sm[:, 3:4], W1[:, i:i + 1], ALU.subtract)
                    yield
                    ps = self.pf[3 + 2 * (i % 2)]
                    P.mm(ps[:, 0:NE], self.Us_b[:], m12b[:])
                    P.mm(ps[:, NE:2 * NE], self.ones_b[:], m12b[:])
                    P.tt(pos[:], ps[:, 0:NE], carry[:], ALU.add)
                    P.tt(carry[:], carry[:], ps[:, NE:2 * NE], ALU.add)
                    for (mk, Ed, Rd) in ((m1, E1, R1), (m2, E2, R2)):
                        P.tt(tmp[:], mk[:], iotaE[:], ALU.mult)
                        P.red(Ed[:, i:i + 1], tmp[:], ALU.add)
                        P.tt(tmp[:], mk[:], pos[:], ALU.mult)
                        P.red(Rd[:, i:i + 1], tmp[:], ALU.add)
                    yield
                self.run2(NT, rt_body)
            D1i = sb("D1i", [128, NT], I32); D2i = sb("D2i", [128, NT], I32)
            WIi = sb("WIi", [128, NB], I32)
            with self.scope() as e2:
                s2 = lambda n, s, dt=F32: self.sb(e2, n, s, dt)
                NTH = 2 * NT + 1
                thr = s2("thr", [128, NTH])
                P.op("pool", lambda e: e.iota(thr[:], pattern=[[128, NTH]], base=0, channel_multiplier=0,
                                               allow_small_or_imprecise_dtypes=True), w=[thr])
                cmp = s2("cmp", [128, NE, NTH])
                P.tt(cmp[:], carry[:].unsqueeze(2).to_broadcast([128, NE, NTH]),
                     thr[:].unsqueeze(1).to_broadcast([128, NE, NTH]), ALU.is_gt)
                padded = s2("padded", [128, NE])
                P.red(padded[:], cmp[:], ALU.add)
                P.ts(padded[:], padded[:], 128.0, ALU.mult)
                le = s2("le", [128, NE, NE])
                P.tt(le[:], self.coli[:, 0:NE].unsqueeze(2).to_broadcast([128, NE, NE]),
                     self.coli[:, 0:NE].unsqueeze(1).to_broadcast([128, NE, NE]), ALU.is_ge)
                P.tt(le[:], le[:], padded[:].unsqueeze(1).to_broadcast([128, NE, NE]), ALU.mult)
                ends = s2("ends", [128, NE]); starts = s2("starts", [128, NE])
                P.red(ends[:], le[:], ALU.add)
                P.tt(starts[:], ends[:], padded[:], ALU.subtract)
                oh = s2("oh", [128, NT, NE])
                dd = s2("dd", [128, NT])
                for (Ed, Rd, Di) in ((E1, R1, D1i), (E2, R2, D2i)):
                    P.tt(oh[:], Ed[:].unsqueeze(2).to_broadcast([128, NT, NE]),
                         iotaE[:].unsqueeze(1).to_broadcast([128, NT, NE]), ALU.is_equal)
                    P.tt(oh[:], oh[:], starts[:].unsqueeze(1).to_broadcast([128, NT, NE]), ALU.mult)
                    P.red(dd[:], oh[:], ALU.add)
                    P.tt(dd[:], dd[:], Rd[:], ALU.add)
                    P.copy(Di[:], dd[:])
                bthr = s2("bthr", [128, NB])
                P.op("pool", lambda e: e.iota(bthr[:], pattern=[[128, NB]], base=0, channel_multiplier=0,
                                               allow_small_or_imprecise_dtypes=True), w=[bthr])
                cb = s2("cb", [128, NB, NE])
                P.tt(cb[:], ends[:].unsqueeze(1).to_broadcast([128, NB, NE]),
                     bthr[:].unsqueeze(2).to_broadcast([128, NB, NE]), ALU.is_le)
                be = s2("be", [128, NB + 2])
                P.memset(be[:, 0:2], -1.0)
                P.red(be[:, 2:NB + 2], cb[:], ALU.add)
                P.ts(be[:, 2:NB + 2], be[:, 2:NB + 2], float(NE - 1), ALU.min)
                need = s2("need", [128, NB])
                P.tt(need[:], be[:, 2:NB + 2], be[:, 1:NB + 1], ALU.not_equal)
                P.memset(need[:, NB // 2:NB // 2 + 1], 1.0)
                wi = s2("wi", [128, NB])
                P.ts(wi[:], be[:, 2:NB + 2], 128.0, ALU.mult, self.rowi[:, 0:1], ALU.add)
                P.ts(wi[:], wi[:], -1.0e6, ALU.add)
                P.tt(wi[:], wi[:], need[:], ALU.mult)
                P.ts(wi[:], wi[:], 1.0e6, ALU.add)
                P.copy(WIi[:], wi[:])
            with self.scope() as e3:
                s3 = lambda n, s, dt=F32: self.sb(e3, n, s, dt)
                hbt = [s3("hbt", [128, D], BF16) for _ in range(2)]
                for i in range(NT):
                    h_ = hbt[i % 2]
                    P.dma("sp", h_[:], self.HF[i * 128:(i + 1) * 128, :], r=[("hf", i)])
                    for Di in (D1i, D2i):
                        def sc(e, Di=Di, h_=h_, i=i):
                            if "bx" not in self.regcache:
                                self.regcache["bx"] = e.to_reg(self.NBLK * 128 - 1)
                            return e.indirect_dma_start(
                                out=self.XB[:, :], out_offset=bass.IndirectOffsetOnAxis(ap=Di[:, i:i + 1], axis=0),
                                in_=h_[:, :], in_offset=None, bounds_check=self.regcache["bx"], oob_is_err=False)
                        P.op("pool", sc, r=[h_, Di], w=[], dma=True)
            with self.scope() as e4:
                s4 = lambda n, s, dt=F32: self.sb(e4, n, s, dt)
                stg = [s4("stg", [128, 3 * 4096]) for _ in range(2)]
                w1b_ = [s4("w1b", [128, 8, DE], BF16) for _ in range(2)]
                w3b_ = [s4("w3b", [128, 8, DE], BF16) for _ in range(2)]
                w2b_ = [s4("w2b", [128, 4, D], BF16) for _ in range(2)]
                xbt = [s4("xbt", [128, D], BF16) for _ in range(2)]
                xT_ = [s4("xT", [128, 8, 128], BF16) for _ in range(2)]
                h1_ = [s4("h1", [128, DE]) for _ in range(2)]
                gb_ = [s4("gb", [128, DE], BF16) for _ in range(2)]
                gT_ = [s4("gT", [128, 4, 128], BF16) for _ in range(2)]
                yo = [s4("yo", [128, D]) for _ in range(2)]
                HB = NB // 2
                lane = lambda b: 0 if b < HB else 1

                def gather(b, ln):
                    if b >= NB or lane(b) != ln:
                        return
                    sl = ln

                    def gw(e, b=b, sl=sl):
                        if "bc" not in self.regcache:
                            self.regcache["bc"] = e.to_reg(NE * 128 - 1)
                        return e.indirect_dma_start(
                            out=stg[sl][:, :], out_offset=None, in_=self.wall[layer][:, :],
                            in_offset=bass.IndirectOffsetOnAxis(ap=WIi[:, b:b + 1], axis=0),
                            bounds_check=self.regcache["bc"], oob_is_err=False)
                    P.op("pool", gw, r=[WIi], w=[stg[sl]], dma=True)

                def xload(b, ln):
                    if b >= NB or lane(b) != ln:
                        return
                    P.dma("sp", xbt[ln][:], self.XB[b * 128:(b + 1) * 128, :], r=[], w=[xbt[ln]])

                gather(0, 0)
                gather(HB, 1)
                xload(0, 0)
                xload(HB, 1)
                def blk(b):
                    sl = lane(b)
                    w1b, w3b, w2b, xT, h1, gb, gT = w1b_[sl], w3b_[sl], w2b_[sl], xT_[sl], h1_[sl], gb_[sl], gT_[sl]
                    w1f = w1b[:].rearrange("p k n -> p (k n)")
                    w3f = w3b[:].rearrange("p k n -> p (k n)")
                    w2f = w2b[:].rearrange("p k n -> p (k n)")
                    for q in range(4):
                        cq = slice(q * 1024, (q + 1) * 1024)
                        P.copy(w1f[:, cq], stg[sl][:, q * 1024:(q + 1) * 1024], eng="dve")
                        P.copy(w3f[:, cq], stg[sl][:, 4096 + q * 1024:4096 + (q + 1) * 1024], eng="act")
                        P.copy(w2f[:, cq], stg[sl][:, 8192 + q * 1024:8192 + (q + 1) * 1024], eng="act" if q < 2 else "dve")
                        yield
                    gather(b + 1, sl)
                    yield
                    self.transpose8(xT, xbt[sl], self.pt[sl], b)
                    xload(b + 1, sl)
                    yield
                    p1, p3 = self.pf[2 * sl], self.pf[2 * sl + 1]
                    for kc in range(8):
                        P.mm(p1[:], xT[:, kc, :], w1b[:, kc, :], start=(kc == 0), stop=(kc == 7))
                    for kc in range(8):
                        P.mm(p3[:], xT[:, kc, :], w3b[:, kc, :], start=(kc == 0), stop=(kc == 7))
                    yield
                    P.act(h1[:], p1[:], AF.Silu)
                    P.tt(gb[:], h1[:], p3[:], ALU.mult)
                    yield
                    pt_ = self.pt[1 - sl]
                    for fc in range(4):
                        P.tr(pt_[:, fc, :], gb[:, fc * 128:(fc + 1) * 128], self.ident_b[:])
                    self.evac(gT[:], pt_[:, 0:4, :], b + 1)
                    yield
                    y_ = yo[sl]
                    for half in range(2):
                        ps = self.pf[4 + half]
                        for fc in range(4):
                            P.mm(ps[:], gT[:, fc, :], w2b[:, fc, half * 512:(half + 1) * 512], start=(fc == 0), stop=(fc == 3))
                        self.evac(y_[:, half * 512:(half + 1) * 512], ps[:], half)
                    yield
                    P.dma("pool", self.YB[b * 128:(b + 1) * 128, :], y_[:], w=["yb_all"])
                    yield
                def lane_stream(ln):
                    for b in (range(0, HB) if ln == 0 else range(HB, NB)):
                        yield from blk(b)
                gens = [lane_stream(0), lane_stream(1)]
                for _ in range(6):
                    next(gens[0])
                while gens:
                    for g_ in list(gens):
                        try:
                            next(g_)
                        except StopIteration:
                            gens.remove(g_)
            with self.scope() as e5:
                s5 = lambda n, s, dt=F32: self.sb(e5, n, s, dt)
                y1 = [s5("y1", [128, D]) for _ in range(2)]
                y2 = [s5("y2", [128, D]) for _ in range(2)]
                xt = [s5("xt", [128, D]) for _ in range(2)]
                acc_ = [s5("acc", [128, D]) for _ in range(2)]
                xo_ = [s5("xo", [128, D]) for _ in range(2)]
                if final:
                    fn = self.bcast_load(e5, "fn", self.final_norm, D)
                    sq_ = [s5("sq", [128, D]) for _ in range(2)]; ss_ = [s5("ss", [128, 4]) for _ in range(2)]
                def cload(i):
                    if i >= NT:
                        return
                    sl = i % 2
                    for (yy, Di) in ((y1, D1i), (y2, D2i)):
                        def gy(e, yy=yy, Di=Di, i=i, sl=sl):
                            if "bx" not in self.regcache:
                                self.regcache["bx"] = e.to_reg(self.NBLK * 128 - 1)
                            return e.indirect_dma_start(
                                out=yy[sl][:, :], out_offset=None, in_=self.YB[:, :],
                                in_offset=bass.IndirectOffsetOnAxis(ap=Di[:, i:i + 1], axis=0),
                                bounds_check=self.regcache["bx"], oob_is_err=False)
                        P.op("pool", gy, r=[Di], w=[yy[sl]], dma=True)
                    P.dma("sp", xt[sl][:], XIN[i * 128:(i + 1) * 128, :], r=[(xin_key, i)])

                cload(0)
                cload(1)

                def cmb(i):
                    sl = i % 2
                    acc = acc_[sl]
                    if final:
                        sq, ss = sq_[sl], ss_[sl]
                    P.ts(acc[:], y1[sl][:], W1[:, i:i + 1], ALU.mult)
                    P.stt(acc[:], y2[sl][:], W2[:, i:i + 1], acc[:], ALU.mult, ALU.add)
                    P.tt(acc[:], acc[:], G[:], ALU.mult)
                    yield
                    xo = xo_[sl]
                    P.tt(xo[:], acc[:], xt[sl][:], ALU.add)
                    cload(i + 2)
                    yield
                    if not final:
                        P.dma("act", XOUT[i * 128:(i + 1) * 128, :], xo[:], w=[("x2", i)])
                    else:
                        P.act(sq[:], xo[:], AF.Square)
                        P.red(ss[:, 0:1], sq[:], ALU.add)
                        P.ts(ss[:, 1:2], ss[:, 0:1], 1.0 / D, ALU.mult, EPS, ALU.add)
                        P.act(ss[:, 2:3], ss[:, 1:2], AF.Sqrt)
                        P.recip(ss[:, 3:4], ss[:, 2:3])
                        P.stt(sq[:], xo[:], ss[:, 3:4], fn[:], ALU.mult, ALU.mult)
                        P.dma("act", XOUT[i * 128:(i + 1) * 128, :], sq[:], w=[("out", i)])
                    yield
                self.run2(NT, cmb)

    def phase_convD(self):
        nc, P = self.nc, self.P
        with self.scope() as es:
            sb = lambda n, s, dt=F32: self.sb(es, n, s, dt)
            wb = self.load_cast(es, "winb", self.w_in_b, 3 * D)
            A = self.load_mod(es, "A1", 6)
            B = self.load_mod(es, "B1", 7)
            xt = [sb("xt", [128, D]) for _ in range(2)]
            sq_ = [sb("sq", [128, D]) for _ in range(2)]; ss_ = [sb("ss", [128, 4]) for _ in range(2)]
            hf_ = [sb("hf", [128, D]) for _ in range(2)]; hb_ = [sb("hb", [128, D], BF16) for _ in range(2)]
            hT = [sb("hT", [128, 8, 128], BF16) for _ in range(2)]
            gbt = [sb("gbt", [128, D]) for _ in range(2)]
            gcc_ = [sb("gc_", [128, D]) for _ in range(2)]
            ut = [sb("ut", [128, D]) for _ in range(2)]
            def dbody(i):
                x_ = xt[i % 2]
                sq, ss, hf, hb, gc_ = sq_[i % 2], ss_[i % 2], hf_[i % 2], hb_[i % 2], gcc_[i % 2]
                P.dma("sp", x_[:], self.X2[i * 128:(i + 1) * 128, :], r=[("x2", i)])
                self.modulate(x_, A, B, sq, ss, hf, hb)
                yield
                hT_ = hT[i % 2]
                self.transpose8(hT_, hb, self.pt[i % 2], i)
                yield
                g_, u_ = gbt[i % 2], ut[i % 2]
                for g in range(6):
                    ps = self.pf[(g + 3 * (i % 2)) % 6]
                    for kc in range(8):
                        P.mm(ps[:], hT_[:, kc, :], wb[:, kc, g * 512:(g + 1) * 512], start=(kc == 0), stop=(kc == 7))
                    cs = slice((g % 2) * 512, (g % 2 + 1) * 512)
                    if g < 2:
                        self.evac(g_[:, cs], ps[:], g)
                    elif g < 4:
                        self.evac(gc_[:, cs], ps[:], g)
                    else:
                        P.tt(u_[:, cs], ps[:], gc_[:, cs], ALU.mult)
                    if g % 2 == 1:
                        yield
                P.dma("pool", self.GB[i * 128:(i + 1) * 128, :], g_[:], w=[("gb", i)])
                P.dma("pool", self.U1[i * 128:(i + 1) * 128, :], u_[:], w=[("u1", i)])
                yield
            self.run2(self.NT, dbody)

    def phase_convE(self):
        nc, P = self.nc, self.P
        NT = self.NT
        H = D // 2
        with self.scope() as es:
            sb = lambda n, s, dt=F32: self.sb(es, n, s, dt)
            wo = self.load_cast(es, "woutb", self.w_out_b, D)
            cb = [self.bcast_load(es, "cb%d" % s, self.conv_b[s], D) for s in range(3)]
            Gm = self.load_mod(es, "Gm1", 8)
            mm_ = sb("mm_", [128, 4])
            P.ts(mm_[:, 0:1], self.rowi[:, 0:1], 64.0, ALU.is_equal)
            P.ts(mm_[:, 1:2], self.rowi[:, 0:1], 0.0, ALU.is_equal)
            P.tt(mm_[:, 0:1], mm_[:, 0:1], mm_[:, 1:2], ALU.add)
            P.ts(mm_[:, 0:1], mm_[:, 0:1], -1.0, ALU.mult, 1.0, ALU.add)
            P.ts(mm_[:, 2:3], self.rowi[:, 0:1], 63.0, ALU.is_equal)
            P.ts(mm_[:, 3:4], self.rowi[:, 0:1], 127.0, ALU.is_equal)
            P.tt(mm_[:, 2:3], mm_[:, 2:3], mm_[:, 3:4], ALU.add)
            P.ts(mm_[:, 2:3], mm_[:, 2:3], -1.0, ALU.mult, 1.0, ALU.add)
            um = [sb("um", [128, D]) for _ in range(2)]
            up = [sb("up", [128, D]) for _ in range(2)]
            uc = [sb("uc", [128, D]) for _ in range(2)]
            gbt = [sb("gbt", [128, D]) for _ in range(2)]
            xt = [sb("xt", [128, D]) for _ in range(2)]
            yc_ = [sb("yc", [128, D]) for _ in range(2)]
            vbb_ = [sb("vb_", [128, D], BF16) for _ in range(2)]
            vT_ = [sb("vT", [128, 8, 128], BF16) for _ in range(2)]
            x3t = [sb("x3t", [128, D]) for _ in range(2)]
            for t in um + up:
                P.memset(t[:], 0.0, eng="pool")
            def ebody(i):
                sl = i % 2
                r0 = i * 128
                yc, vb_, vT = yc_[sl], vbb_[sl], vT_[sl]
                um_, up_, uc_ = um[sl], up[sl], uc[sl]
                P.dma("sp", uc_[:], self.U1[r0:r0 + 128, :], r=[("u1", i)])
                P.dma("sp", gbt[sl][:], self.GB[r0:r0 + 128, :], r=[("gb", i)])
                P.dma("sp", xt[sl][:], self.X2[r0:r0 + 128, :], r=[("x2", i)])
                if i == 0:
                    P.dma("sp", um_[1:128, 0:H], self.U1[0:127, 0:H], r=[("u1", 0)], w=[um_])
                else:
                    P.dma("sp", um_[:, 0:H], self.U1[r0 - 1:r0 + 127, 0:H], r=[("u1", i), ("u1", i - 1)], w=[um_])
                if i == NT - 1:
                    P.dma("sp", up_[0:127, 0:H], self.U1[r0 + 1:r0 + 128, 0:H], r=[("u1", i)], w=[up_])
                else:
                    P.dma("sp", up_[:, 0:H], self.U1[r0 + 1:r0 + 129, 0:H], r=[("u1", i), ("u1", i + 1)], w=[up_])
                if i == 0:
                    P.memset(um_[0:64, H:D], 0.0, eng="pool")
                    P.dma("sp", um_[64:128, H:D], self.U1[0:64, H:D], r=[("u1", 0)], w=[um_])
                else:
                    P.dma("sp", um_[:, H:D], self.U1[r0 - 64:r0 + 64, H:D], r=[("u1", i), ("u1", i - 1)], w=[um_])
                if i == NT - 1:
                    P.memset(up_[64:128, H:D], 0.0, eng="pool")
                    P.dma("sp", up_[0:64, H:D], self.U1[r0 + 64:r0 + 128, H:D], r=[("u1", i)], w=[up_])
                else:
                    P.dma("sp", up_[:, H:D], self.U1[r0 + 64:r0 + 192, H:D], r=[("u1", i), ("u1", i + 1)], w=[up_])
                yield
                P.ts(um_[:, 0:H], um_[:, 0:H], mm_[:, 0:1], ALU.mult)
                P.ts(up_[:, 0:H], up_[:, 0:H], mm_[:, 2:3], ALU.mult, eng="pool")
                P.tt(um_[:], um_[:], cb[0][:], ALU.mult)
                P.tt(up_[:], up_[:], cb[2][:], ALU.mult)
                yield
                P.tt(yc[:], uc_[:], cb[1][:], ALU.mult)
                P.tt(yc[:], yc[:], um_[:], ALU.add)
                P.tt(yc[:], yc[:], up_[:], ALU.add)
                P.tt(vb_[:], yc[:], gbt[sl][:], ALU.mult)
                yield
                self.transpose8(vT, vb_, self.pt[sl], i)
                yield
                x3 = x3t[sl]
                for half in range(2):
                    ps = self.pf[half + 2 * sl]
                    for kc in range(8):
                        P.mm(ps[:], vT[:, kc, :], wo[:, kc, half * 512:(half + 1) * 512], start=(kc == 0), stop=(kc == 7))
                    cs = slice(half * 512, (half + 1) * 512)
                    P.tt(x3[:, cs], ps[:], Gm[:, cs], ALU.mult)
                    P.tt(x3[:, cs], x3[:, cs], xt[sl][:, cs], ALU.add)
                P.dma("pool", self.X3[r0:r0 + 128, :], x3[:], w=[("x3", i)])
                yield
            self.run2(NT, ebody)


def make_in_maps(inputs, S, CTXL, n_cores, phases=99):
    f = lambda a: np.ascontiguousarray(np.asarray(a, dtype=np.float32))
    moe = {}
    if phases >= 4:
        w1 = f(inputs["w1"]); w3 = f(inputs["w3"]); w2 = f(inputs["w2"])
        for l in range(2):
            wall = np.empty((NE, 128, 3, 4096), np.float32)
            wall[:, :, 0, :] = w1[l].reshape(NE, 8, 128, DE).transpose(0, 2, 1, 3).reshape(NE, 128, 4096)
            wall[:, :, 1, :] = w3[l].reshape(NE, 8, 128, DE).transpose(0, 2, 1, 3).reshape(NE, 128, 4096)
            wall[:, :, 2, :] = w2[l].reshape(NE, 4, 128, D).transpose(0, 2, 1, 3).reshape(NE, 128, 4096)
            moe["wall%d" % l] = wall.reshape(NE * 128, 3 * 4096)
    router = np.ascontiguousarray(np.concatenate([f(inputs["router_g"]), f(inputs["router_e"])], axis=-1))
    shared = {
        "c_ctx": f(inputs["c_ctx"]), "ada_w": f(inputs["ada_w"]), "ada_b": f(inputs["ada_b"]),
        "norm_mix": f(inputs["norm_mix"]), "norm_ffn": f(inputs["norm_ffn"]),
        "w_in_a": f(inputs["w_in_a"])[0], "conv_a": f(inputs["conv_a"])[0], "a_log": f(inputs["a_log_a"])[0],
        "dt_bias": f(inputs["dt_bias_a"])[0], "onorm": f(inputs["onorm_a"])[0], "w_out_a": f(inputs["w_out_a"])[0],
        "w_in_b": f(inputs["w_in_b"])[0], "conv_b": f(inputs["conv_b"])[0], "w_out_b": f(inputs["w_out_b"])[0],
        "router": router, "final_norm": f(inputs["final_norm"]),
    }
    shared.update(moe)
    x = f(inputs["x"]); c = f(inputs["c"]); ctx = f(inputs["ctx"])
    maps = []
    for b in range(n_cores):
        m = dict(shared)
        m["x"] = np.ascontiguousarray(x[b, :S])
        m["c"] = np.ascontiguousarray(c[b])
        m["ctx"] = np.ascontiguousarray(ctx[b, :CTXL])
        maps.append(m)
    return maps


def run(inputs, S, CTXL, n_cores, phases=99, debug=(), trace=False):
    bld = Builder(S, CTXL, phases=phases, debug=debug)
    nc = bld.build()
    import time as _t
    t0 = _t.time()
    maps = make_in_maps(inputs, S, CTXL, n_cores, phases)
    print("in_maps", _t.time() - t0, flush=True)
    res = run_bass_kernel_spmd(nc, maps, core_ids=list(range(n_cores)))
    print("spmd", _t.time() - t0, flush=True)
    return res, bld


def kernel(**inputs):
    res, _ = run(inputs, 8192, 256, 8)
    return np.stack([np.asarray(r["out"], dtype=np.float32) for r in res.results], axis=0)
```
